# Optimizing a Trainium2 kernel written in Bass

```python
import jax, jax.numpy as jnp
from jax import lax
import numpy as np

D_MODEL = 1024
BATCH = 16
SEQ = 4096
DEPTH = 1

GRID_W = 64
CTX_LEN = 256
MIX_WIDTH = D_MODEL
RG_WIDTH = MIX_WIDTH // 2
RG_BLOCKS = 8
RG_BLOCK = RG_WIDTH // RG_BLOCKS
RG_C = 8.0
CONV_W = 4
CONV_PAD_L = (CONV_W - 1) // 2
CONV_PAD_R = CONV_W - 1 - CONV_PAD_L
HG_WIDTH = MIX_WIDTH - RG_WIDTH
HG_HEAD = 128
HG_HEADS = HG_WIDTH // HG_HEAD
HG_CHUNK = 64
IN_SIZES = (RG_WIDTH, RG_WIDTH, HG_WIDTH, HG_WIDTH, HG_WIDTH, HG_WIDTH, HG_WIDTH)
N_IN = sum(IN_SIZES)
N_GROUPS = 4
EXPERTS_PER_GROUP = 8
N_EXPERTS = N_GROUPS * EXPERTS_PER_GROUP
TOP_K = 2
D_EXPERT = D_MODEL // 2
MOE_BLOCK = 256
ALPHA = (2.0 * DEPTH) ** 0.25
BETA = (8.0 * DEPTH) ** -0.25
LN_EPS = 1e-6

kernel_name = "hymba_rglru_hgrn2_hmoe_deepnorm_prefix"


def layer_norm(x, g, b):
    xf = x.astype(jnp.float32)
    mu = jnp.mean(xf, axis=-1, keepdims=True)
    var = jnp.mean(jnp.square(xf - mu), axis=-1, keepdims=True)
    return ((xf - mu) * lax.rsqrt(var + LN_EPS) * g.astype(jnp.float32) + b.astype(jnp.float32)).astype(x.dtype)


def modulation(cvec, w, b):
    m = jax.nn.silu(cvec) @ w + b
    return jnp.split(m, 6, axis=-1)


def short_conv(x, w, b):
    L = x.shape[-2]
    pad = [(0, 0)] * (x.ndim - 2) + [(CONV_PAD_L, CONV_PAD_R), (0, 0)]
    xp = jnp.pad(x, pad)
    out = b + xp[..., 0:L, :] * w[0]
    for j in range(1, CONV_W):
        out = out + xp[..., j:j + L, :] * w[j]
    return out


def rglru_scan(xc, gate_w, gate_b, lam, h0):
    B, L, W = xc.shape
    xf = xc.astype(jnp.float32)
    xb = xf.reshape(B, L, RG_BLOCKS, RG_BLOCK)
    gates = jnp.einsum('blnc,gncd->gblnd', xb, gate_w.astype(jnp.float32)).reshape(2, B, L, W)
    gates = gates + gate_b.astype(jnp.float32)[:, None, None, :]
    r = jax.nn.sigmoid(gates[0])
    i = jax.nn.sigmoid(gates[1])
    log_a = -RG_C * r * jax.nn.softplus(-lam.astype(jnp.float32))
    a = jnp.exp(log_a)
    u = jnp.sqrt(-jnp.expm1(2.0 * log_a)) * (i * xf)
    u = u.at[:, 0].add(a[:, 0] * h0)

    def combine(lhs, rhs):
        a1, b1 = lhs
        a2, b2 = rhs
        return a1 * a2, a2 * b1 + b2

    _, h = lax.associative_scan(combine, (a, u), axis=1)
    return h, h[:, -1]


def hgrn2_scan(q, k, v, log_f, s0):
    B, L, H, dk = q.shape
    n = L // HG_CHUNK

    def to_chunks(t):
        return t.reshape(B, n, HG_CHUNK, H, t.shape[-1]).transpose(1, 0, 3, 2, 4)

    causal = jnp.tril(jnp.ones((HG_CHUNK, HG_CHUNK), dtype=bool))[:, :, None]

    def step(S, inp):
        qc, kc, vc, gc = inp
        G = jnp.cumsum(gc, axis=-2)
        o_inter = jnp.einsum('bhtk,bhkv->bhtv', qc * jnp.exp(G), S)
        diff = G[..., :, None, :] - G[..., None, :, :]
        decay = jnp.exp(jnp.where(causal, diff, -jnp.inf))
        A = jnp.einsum('bhtk,bhtsk,bhsk->bhts', qc, decay, kc)
        o = o_inter + jnp.einsum('bhts,bhsv->bhtv', A, vc)
        G_last = G[..., -1:, :]
        S_new = jnp.exp(G_last[..., 0, :])[..., None] * S + jnp.einsum('bhsk,bhsv->bhkv', kc * jnp.exp(G_last - G), vc)
        return S_new, o

    S_fin, o = lax.scan(step, s0, (to_chunks(q), to_chunks(k), to_chunks(v), to_chunks(log_f)))
    o = o.transpose(1, 0, 3, 2, 4).reshape(B, L, H, v.shape[-1])
    return o, S_fin


def token_mixer(u, w_in, conv_w, conv_b, gate_w, gate_b, lam, lb, norm_g, w_out, states, on_grid, with_output):
    B, L, _ = u.shape
    proj = u @ w_in
    split_points = np.cumsum(IN_SIZES)[:-1].tolist()
    rg_x, rg_g, hq, hff, hfb, hi, hg = jnp.split(proj, split_points, axis=-1)
    h0f, h0b, s0f, s0b = states

    if on_grid:
        rows = L // GRID_W
        xc = short_conv(rg_x.reshape(B, rows, GRID_W, RG_WIDTH), conv_w, conv_b).reshape(B, L, RG_WIDTH)
    else:
        xc = short_conv(rg_x, conv_w, conv_b)
    hf, hf_last = rglru_scan(xc, gate_w[0], gate_b[0], lam[0], h0f)
    hb, hb_last = rglru_scan(xc[:, ::-1], gate_w[1], gate_b[1], lam[1], h0b)

    q = jax.nn.silu(hq.astype(jnp.float32)).reshape(B, L, HG_HEADS, HG_HEAD)
    v = hi.astype(jnp.float32).reshape(B, L, HG_HEADS, HG_HEAD)

    def forget(z, lb_d):
        z = z.astype(jnp.float32)
        log_f = jnp.log(lb_d + (1.0 - lb_d) * jax.nn.sigmoid(z))
        k = (1.0 - lb_d) * jax.nn.sigmoid(-z)
        return k.reshape(B, L, HG_HEADS, HG_HEAD), log_f.reshape(B, L, HG_HEADS, HG_HEAD)

    kf, lff = forget(hff, lb[0])
    kb, lfb = forget(hfb, lb[1])
    of, sf = hgrn2_scan(q, kf, v, lff, s0f)
    ob, sb = hgrn2_scan(q[:, ::-1], kb[:, ::-1], v[:, ::-1], lfb[:, ::-1], s0b)
    new_states = (hf_last, hb_last, sf, sb)
    if not with_output:
        return None, new_states

    rg_out = (hf + hb[:, ::-1]) * jax.nn.gelu(rg_g.astype(jnp.float32))
    o = of + ob[:, ::-1]
    o = o * lax.rsqrt(jnp.mean(jnp.square(o), axis=-1, keepdims=True) + LN_EPS)
    hg_out = o.reshape(B, L, HG_WIDTH) * norm_g.astype(jnp.float32) * jax.nn.silu(hg.astype(jnp.float32))
    mixed = jnp.concatenate([rg_out, hg_out], axis=-1).astype(u.dtype)
    return mixed @ w_out, new_states


def hier_moe(t, rg_w, rg_b, re_w, re_b, w1, w3, w2):
    T, D = t.shape
    p_group = jax.nn.softmax((t @ rg_w + rg_b).astype(jnp.float32), axis=-1)
    g_val, g_idx = lax.top_k(p_group, 1)
    e_logits = (t @ re_w + re_b).astype(jnp.float32).reshape(T, N_GROUPS, EXPERTS_PER_GROUP)
    in_group = jnp.take_along_axis(e_logits, g_idx[:, :, None], axis=1)[:, 0]
    e_val, e_idx = lax.top_k(in_group, TOP_K)
    weights = g_val * jax.nn.softmax(e_val, axis=-1)
    expert_id = g_idx * EXPERTS_PER_GROUP + e_idx

    N = T * TOP_K
    flat_e = expert_id.reshape(N)
    flat_tok = jnp.repeat(jnp.arange(T), TOP_K)
    flat_w = weights.reshape(N)
    order = jnp.argsort(flat_e)
    sorted_e = flat_e[order]
    sorted_tok = flat_tok[order]
    counts = jnp.bincount(flat_e, length=N_EXPERTS)
    starts = jnp.cumsum(counts) - counts
    padded = ((counts + MOE_BLOCK - 1) // MOE_BLOCK) * MOE_BLOCK
    pends = jnp.cumsum(padded)
    pstarts = pends - padded
    dest = pstarts[sorted_e] + (jnp.arange(N) - starts[sorted_e])
    n_blocks = (N + MOE_BLOCK - 1) // MOE_BLOCK + N_EXPERTS
    buf = jnp.zeros((n_blocks * MOE_BLOCK, D), t.dtype).at[dest].set(t[sorted_tok])
    block_e = jnp.minimum(jnp.searchsorted(pends, jnp.arange(n_blocks) * MOE_BLOCK, side='right'), N_EXPERTS - 1)

    def expert_block(args):
        xb, e = args
        hid = jax.nn.silu(xb @ w1[e]) * (xb @ w3[e])
        return hid @ w2[e]

    y = lax.map(expert_block, (buf.reshape(n_blocks, MOE_BLOCK, D), block_e)).reshape(n_blocks * MOE_BLOCK, D)
    y_slots = y[dest] * flat_w[order][:, None].astype(y.dtype)
    return jnp.zeros((T, D), y.dtype).at[sorted_tok].add(y_slots)


def setup_inputs(seed: int = 0) -> dict:
    key = jax.random.key(seed)
    ks = jax.random.split(key, 28)
    f32 = jnp.float32

    def nrm(k, shape, s):
        return jax.random.normal(k, shape, f32) * s

    u = jax.random.uniform(ks[11], (DEPTH, 2, RG_WIDTH), f32, 0.9, 0.999)
    s = u ** (1.0 / RG_C)
    return {
        "x": nrm(ks[0], (BATCH, SEQ, D_MODEL), 1.0),
        "c": nrm(ks[1], (BATCH, D_MODEL), 1.0),
        "ctx": nrm(ks[2], (BATCH, CTX_LEN, D_MODEL), 1.0),
        "c_ctx": nrm(ks[3], (D_MODEL,), 1.0),
        "ada_w": nrm(ks[4], (DEPTH, D_MODEL, 6 * D_MODEL), 0.5 * D_MODEL ** -0.5),
        "ada_b": nrm(ks[5], (DEPTH, 6 * D_MODEL), 0.01),
        "w_in": nrm(ks[6], (DEPTH, D_MODEL, N_IN), D_MODEL ** -0.5),
        "rg_conv_w": nrm(ks[7], (DEPTH, CONV_W, RG_WIDTH), CONV_W ** -0.5),
        "rg_conv_b": nrm(ks[8], (DEPTH, RG_WIDTH), 0.01),
        "rg_gate_w": nrm(ks[9], (DEPTH, 2, 2, RG_BLOCKS, RG_BLOCK, RG_BLOCK), RG_BLOCK ** -0.5),
        "rg_gate_b": nrm(ks[10], (DEPTH, 2, 2, RG_WIDTH), 0.01),
        "rg_lambda": jnp.log(s) - jnp.log1p(-s),
        "hg_lb_logits": nrm(ks[12], (DEPTH + 1, 2, HG_WIDTH), 0.1),
        "hg_norm_g": 1.0 + nrm(ks[13], (DEPTH, HG_WIDTH), 0.02),
        "w_out": nrm(ks[14], (DEPTH, MIX_WIDTH, D_MODEL), BETA * MIX_WIDTH ** -0.5),
        "ln1_g": 1.0 + nrm(ks[15], (DEPTH, D_MODEL), 0.02),
        "ln1_b": nrm(ks[16], (DEPTH, D_MODEL), 0.01),
        "router_g_w": nrm(ks[17], (DEPTH, D_MODEL, N_GROUPS), D_MODEL ** -0.5),
        "router_g_b": nrm(ks[18], (DEPTH, N_GROUPS), 0.01),
        "router_e_w": nrm(ks[19], (DEPTH, D_MODEL, N_EXPERTS), D_MODEL ** -0.5),
        "router_e_b": nrm(ks[20], (DEPTH, N_EXPERTS), 0.01),
        "exp_w1": nrm(ks[21], (DEPTH, N_EXPERTS, D_MODEL, D_EXPERT), D_MODEL ** -0.5),
        "exp_w3": nrm(ks[22], (DEPTH, N_EXPERTS, D_MODEL, D_EXPERT), D_MODEL ** -0.5),
        "exp_w2": nrm(ks[23], (DEPTH, N_EXPERTS, D_EXPERT, D_MODEL), BETA * D_EXPERT ** -0.5),
        "ln2_g": 1.0 + nrm(ks[24], (DEPTH, D_MODEL), 0.02),
        "ln2_b": nrm(ks[25], (DEPTH, D_MODEL), 0.01),
    }


def reference(x, c, ctx, c_ctx, ada_w, ada_b, w_in, rg_conv_w, rg_conv_b, rg_gate_w, rg_gate_b, rg_lambda,
              hg_lb_logits, hg_norm_g, w_out, ln1_g, ln1_b, router_g_w, router_g_b, router_e_w, router_e_b,
              exp_w1, exp_w3, exp_w2, ln2_g, ln2_b):
    B, L, D = x.shape
    Lc = ctx.shape[1]
    lb_all = jnp.cumsum(jax.nn.softmax(hg_lb_logits.astype(jnp.float32), axis=0), axis=0)
    h = x
    hc = ctx
    for l in range(DEPTH):
        last = l == DEPTH - 1
        sh1, sc1, g1, sh2, sc2, g2 = modulation(c[:, None, :], ada_w[l], ada_b[l])
        sh1c, sc1c, g1c, sh2c, sc2c, g2c = modulation(c_ctx[None, None, :], ada_w[l], ada_b[l])
        mix_params = (w_in[l], rg_conv_w[l], rg_conv_b[l], rg_gate_w[l], rg_gate_b[l], rg_lambda[l],
                      lb_all[l], hg_norm_g[l], w_out[l])
        zero_states = (jnp.zeros((B, RG_WIDTH), jnp.float32), jnp.zeros((B, RG_WIDTH), jnp.float32),
                       jnp.zeros((B, HG_HEADS, HG_HEAD, HG_HEAD), jnp.float32),
                       jnp.zeros((B, HG_HEADS, HG_HEAD, HG_HEAD), jnp.float32))
        yc, ctx_states = token_mixer(hc * (1.0 + sc1c) + sh1c, *mix_params, zero_states,
                                     on_grid=False, with_output=not last)
        y, _ = token_mixer(h * (1.0 + sc1) + sh1, *mix_params, ctx_states, on_grid=True, with_output=True)
        h = layer_norm(ALPHA * h + g1 * y, ln1_g[l], ln1_b[l])
        u2 = h * (1.0 + sc2) + sh2
        moe_params = (router_g_w[l], router_g_b[l], router_e_w[l], router_e_b[l], exp_w1[l], exp_w3[l], exp_w2[l])
        if last:
            f = hier_moe(u2.reshape(B * L, D), *moe_params).reshape(B, L, D)
        else:
            hc = layer_norm(ALPHA * hc + g1c * yc, ln1_g[l], ln1_b[l])
            u2c = hc * (1.0 + sc2c) + sh2c
            both = hier_moe(jnp.concatenate([u2c, u2], axis=1).reshape(B * (Lc + L), D), *moe_params)
            both = both.reshape(B, Lc + L, D)
            fc, f = both[:, :Lc], both[:, Lc:]
            hc = layer_norm(ALPHA * hc + g2c * fc, ln2_g[l], ln2_b[l])
        h = layer_norm(ALPHA * h + g2 * f, ln2_g[l], ln2_b[l])
    return h
```

```python
import os
import numpy as np
from contextlib import ExitStack
import ml_dtypes
import concourse.bass as bass
import concourse.mybir as mybir
from concourse.bass_utils import run_bass_kernel_spmd

F32 = mybir.dt.float32
BF16 = mybir.dt.bfloat16
I32 = mybir.dt.int32
AF = mybir.ActivationFunctionType
ALU = mybir.AluOpType
AX = mybir.AxisListType

SAME_ENG_SYNC = True
NCORES = 8
L = 4096
LC = 256
D = 1024
ALPHA = 2.0 ** 0.25
EPS = 1e-6
BLK = 128
NBLK = 160
NSLOT = NBLK * BLK
NWB = 2


class Buf:
    __slots__ = ("name", "w", "r", "excl")

    def __init__(self, name=""):
        self.name = name
        self.w = None
        self.r = {}
        self.excl = False


class Sched:
    ENGS = ("pe", "act", "dve", "pool", "sp")

    def __init__(self, nc, es, n_dma_sems=40):
        self.nc = nc
        self.q = {e: [] for e in self.ENGS}
        self.cnt = {e: 0 for e in self.ENGS}
        self.sem = {e: es.enter_context(nc.semaphore("s_" + e)) for e in ("pe", "act", "dve", "pool")}
        n_sw = 24
        self.dsem = [es.enter_context(nc.semaphore("d%d" % i)) for i in range(n_dma_sems + n_sw)]
        self.dcnt = [0] * (n_dma_sems + n_sw)
        self.n_hw = n_dma_sems
        self.n_sw = n_sw
        self.dnext = {"hw": 0, "sw": 0}
        self.seen = {e: {} for e in self.ENGS}

    def _deps(self, reads, writes):
        toks = []
        for b in reads:
            if b.w is not None:
                toks.append(b.w)
        for b in writes:
            if b.w is not None:
                toks.append(b.w)
            toks.extend(b.r.values())
        return toks

    def _mark(self, reads, writes, tok):
        for b in writes:
            b.w = tok
            b.r = {}
        for b in reads:
            b.r[id(tok[1])] = tok

    def _push(self, eng, fn, toks, inc):
        need = {}
        for kind, sem, val in toks:
            if kind == eng and (eng == "pe" or not SAME_ENG_SYNC):
                continue
            k = id(sem)
            if k not in need or need[k][1] < val:
                need[k] = (sem, val)
        waits = []
        seen = self.seen[eng]
        for k, (sem, val) in need.items():
            if seen.get(k, 0) >= val:
                continue
            seen[k] = val
            waits.append((sem, val))
        self.q[eng].append((fn, waits, inc))

    @staticmethod
    def _excl(reads, writes):
        ex = [b for b in reads if b.excl]
        if ex:
            reads = [b for b in reads if not b.excl]
            writes = list(writes) + [b for b in ex if b not in writes]
        return reads, writes

    def op(self, eng, fn, reads=(), writes=()):
        reads, writes = self._excl(reads, writes)
        toks = self._deps(reads, writes)
        self.cnt[eng] += 1
        tok = (eng, self.sem[eng], self.cnt[eng])
        self._push(eng, fn, toks, (self.sem[eng], 1))
        self._mark(reads, writes, tok)
        return tok

    def dma(self, eng, fn, reads=(), writes=()):
        toks = self._deps(reads, writes)
        if eng == "pool":
            i = self.n_hw + self.dnext["sw"]
            self.dnext["sw"] = (self.dnext["sw"] + 1) % self.n_sw
        else:
            i = self.dnext["hw"]
            self.dnext["hw"] = (self.dnext["hw"] + 1) % self.n_hw
        if self.dcnt[i] > 0:
            toks.append(("dma", self.dsem[i], self.dcnt[i]))
        self.dcnt[i] += 16
        tok = ("dma", self.dsem[i], self.dcnt[i])
        self._push(eng, fn, toks, (self.dsem[i], 16))
        self._mark(reads, writes, tok)
        return tok

    def barrier(self):
        toks = [(e, self.sem[e], self.cnt[e]) for e in ("pe", "act", "dve", "pool") if self.cnt[e] > 0]
        toks += [("dma", self.dsem[i], self.dcnt[i]) for i in range(len(self.dsem)) if self.dcnt[i] > 0]
        for e in self.ENGS:
            need = []
            for kind, sem, val in toks:
                if kind == e:
                    continue
                if self.seen[e].get(id(sem), 0) >= val:
                    continue
                self.seen[e][id(sem)] = val
                need.append((sem, val))
            self.q[e].append((None, need, None))

    def finish(self, eng="sp"):
        toks = [("dma", self.dsem[i], self.dcnt[i]) for i in range(len(self.dsem)) if self.dcnt[i] > 0]
        self.seen[eng] = {}
        self._push(eng, None, toks, None)

    def emit(self):
        nc = self.nc
        with nc.Block() as block:
            def mk(e):
                def body(engobj):
                    for fn, waits, inc in self.q[e]:
                        for sem, val in waits:
                            engobj.wait_ge(sem, val)
                        if fn is not None:
                            ins = fn(engobj)
                            ins.then_inc(inc[0], inc[1])
                return body
            block.tensor(mk("pe"))
            block.scalar(mk("act"))
            block.vector(mk("dve"))
            block.gpsimd(mk("pool"))
            block.sync(mk("sp"))


class T:
    def __init__(self, nc, es, name, shape, dtype, psum=False, nbuf=0):
        if psum:
            self.t = es.enter_context(nc.psum_tensor("p_" + name, shape, dtype))
        else:
            self.t = es.enter_context(nc.sbuf_tensor("t_" + name, shape, dtype))
        self.b = Buf(name)
        self.b.excl = psum
        self.bs = [Buf(name + "_" + str(i)) for i in range(nbuf)]

    def __getitem__(self, k):
        return self.t[k]


class V:
    def __init__(self, ap, name, nbuf=0):
        self.ap = ap
        self.b = Buf(name)
        self.bs = [Buf(name + "_" + str(i)) for i in range(nbuf)]

    def __getitem__(self, k):
        return self.ap[k]


class RV:
    def __init__(self, items):
        self.tiles = items
        self.i = 0

    def get(self):
        t = self.tiles[self.i % len(self.tiles)]
        self.i += 1
        return t


class Rot:
    def __init__(self, nc, es, name, shape, dtype, n, psum=False):
        self.tiles = [T(nc, es, name + str(i), shape, dtype, psum=psum) for i in range(n)]
        self.i = 0

    def get(self):
        t = self.tiles[self.i % len(self.tiles)]
        self.i += 1
        return t


def build_program(dbg=0):
    nc = bass.Bass("TRN2", target_bir_lowering=False)

    def din(name, shape, dt=F32):
        return nc.dram_tensor(name, shape, dt, kind="ExternalInput").ap()

    def dscr(name, shape, dt=F32):
        return nc.dram_tensor(name, shape, dt, kind="Internal").ap()

    x_d = din("x", [2, L, D])
    ctx_d = din("ctx", [2, LC, D])
    cT_d = din("cT", [128, 24])
    adaw_d = din("ada_w", [D, 6 * D])
    adab_d = din("ada_b", [1, 6 * D])
    win_d = din("w_in", [D, 3584])
    convw_d = din("convw", [128, 16])
    convb_d = din("convb", [128, 4])
    gw_d = din("gw", [128, 2048])
    gb_d = din("gb", [128, 16])
    lam_d = din("lam", [128, 8])
    lbl_d = din("lbl", [128, 16])
    ng_d = din("ng", [128, 4])
    wout_d = din("w_out", [D, D])
    lnp_d = din("lnp", [128, 4 * D])
    identF_d = din("identF", [128, 128])
    maskF_d = din("maskF", [128, 128])
    maskB_d = din("maskB", [128, 128])
    resetm_d = din("resetm", [128, 512])
    rw_d = din("rw", [D, 36])
    rb_d = din("rb", [128, 36])
    w1_d = din("w1r", [32 * 128, 8 * 512])
    w3_d = din("w3r", [32 * 128, 8 * 512])
    w2_d = din("w2r", [32 * 128, 4 * 1024])
    triu_d = din("triu", [128, 128])
    thr_d = din("thr", [128, 64 + NBLK])
    iota_d = din("iota", [128, 2])
    out_d = nc.dram_tensor("out", [2, L, D], F32, kind="ExternalOutput").ap()
    if dbg:
        dbg_d = nc.dram_tensor("dbg", [2 * L, D], F32, kind="ExternalOutput").ap()
        dbgs_d = nc.dram_tensor("dbgs", [128, 512], F32, kind="ExternalOutput").ap()

    m_scr = dscr("m_scr", [3, 6 * D])
    mix_scr = dscr("mix_scr", [2, 8, 128, L], BF16)
    h1_scr = dscr("h1_scr", [2 * L, D])
    u2_scr = dscr("u2_scr", [2 * L, D], BF16)
    xs_scr = dscr("xs_scr", [NSLOT, D], BF16)
    ys_scr = dscr("ys_scr", [NSLOT, D])
    zsrc = dscr("zsrc", [32, D], BF16)
    wb_scr = [dscr("w%db_scr" % i, [32 * 128, 4096], BF16) for i in range(3)]

    with ExitStack() as es:
        S = Sched(nc, es)

        def TT(name, shape, dt=F32, **kw):
            return T(nc, es, name, shape, dt, **kw)

        def RR(name, shape, dt, n, **kw):
            return Rot(nc, es, name, shape, dt, n, **kw)

        def mm(out, lhsT, rhs, start, stop, reads, writes, sgc=False):
            S.op("pe", lambda e: e.matmul(out, lhsT, rhs, start=start, stop=stop, skip_group_check=sgc), reads, writes)

        def tr(out, in_, ident, reads, writes):
            S.op("pe", lambda e: e.transpose(out, in_, ident), reads, writes)

        def act(out, in_, func, reads, writes, bias=None, scale=None):
            kw = {}
            if bias is not None:
                kw["bias"] = bias
            if scale is not None:
                kw["scale"] = scale
            S.op("act", lambda e: e.activation(out, in_, func, **kw), reads, writes)

        POOL2DVE = os.environ.get("K_POOL2DVE", "1") == "1"

        def tt(eng, out, in0, in1, op, reads, writes):
            if eng == "pool" and POOL2DVE:
                eng = "dve"
            S.op(eng, lambda e: e.tensor_tensor(out, in0, in1, op), reads, writes)

        def ts(eng, out, in0, s1, s2, op0, op1, reads, writes):
            if s2 is None:
                S.op(eng, lambda e: e.tensor_scalar(out, in0, s1, None, op0), reads, writes)
            else:
                S.op(eng, lambda e: e.tensor_scalar(out, in0, s1, s2, op0, op1), reads, writes)

        def stt(eng, out, in0, sc, in1, op0, op1, reads, writes):
            S.op(eng, lambda e: e.scalar_tensor_tensor(out, in0, sc, in1, op0, op1), reads, writes)

        def cp(eng, out, in_, reads, writes):
            if eng == "act":
                S.op("act", lambda e: e.copy(out, in_), reads, writes)
            else:
                S.op(eng, lambda e: e.tensor_copy(out, in_), reads, writes)

        def dma(eng, out, in_, reads, writes, **kw):
            S.dma(eng, lambda e: e.dma_start(out=out, in_=in_, **kw), reads, writes)

        def zip_run(gens):
            res = [None] * len(gens)
            live = list(range(len(gens)))
            while live:
                for gi in list(live):
                    try:
                        next(gens[gi])
                    except StopIteration as e_:
                        res[gi] = e_.value
                        live.remove(gi)
            return res

        identF = TT("identF", [128, 128])
        identB = TT("identB", [128, 128], BF16)
        onesF = TT("onesF", [128, 128])
        onesB = TT("onesB", [128, 128], BF16)
        resetm = TT("resetm", [128, 512])
        cT = TT("cT", [128, 24])
        scT = TT("scT", [128, 24])
        convw = TT("convw", [128, 16])
        convb = TT("convb", [128, 4])
        gb = TT("gb", [128, 16])
        lam = TT("lam", [128, 8])
        cL = TT("cL", [128, 8])
        lbl = TT("lbl", [128, 16])
        lb = TT("lb", [128, 8])
        oml = TT("oml", [128, 8])
        noml = TT("noml", [128, 8])
        ng = TT("ng", [128, 4])
        gwb = TT("gwb", [128, 2048], BF16)
        modT = TT("modT", [128, 48])
        ones3 = TT("ones3", [1, 4])

        for t_, d_ in ((identF, identF_d), (resetm, resetm_d), (cT, cT_d),
                       (convw, convw_d), (convb, convb_d), (gb, gb_d), (lam, lam_d), (lbl, lbl_d), (ng, ng_d)):
            dma("sp", t_[:], d_, [], [t_.b])
        cp("dve", identB[:], identF[:], [identF.b], [identB.b])
        S.op("dve", lambda e: e.memset(onesF[:], 1.0), [], [onesF.b])
        S.op("dve", lambda e: e.memset(onesB[:], 1.0), [], [onesB.b])
        S.op("dve", lambda e: e.memset(ones3[:], 1.0), [], [ones3.b])
        act(scT[:], cT[:], AF.Silu, [cT.b], [scT.b])
        act(cL[:], lam[:], AF.Exp, [lam.b], [cL.b], scale=-1.0)
        act(cL[:], cL[:], AF.Ln, [cL.b], [cL.b], bias=1.0)
        ts("dve", cL[:], cL[:], -8.0, None, ALU.mult, None, [cL.b], [cL.b])
        tt("dve", lb[:], lbl[:, 0:8], lbl[:, 8:16], ALU.subtract, [lbl.b], [lb.b])
        act(lb[:], lb[:], AF.Sigmoid, [lb.b], [lb.b])
        ts("dve", oml[:], lb[:], -1.0, 1.0, ALU.mult, ALU.add, [lb.b], [oml.b])
        ts("dve", noml[:], oml[:], -1.0, None, ALU.mult, None, [oml.b], [noml.b])

        ARENA_B = 100 * 1024
        BIG = es.enter_context(nc.sbuf_tensor("big", [128, ARENA_B // 4], F32))

        def fview(off, n, name, nbuf=0):
            return V(BIG[:, off // 4:off // 4 + n], name, nbuf)

        def bview(off, n, name, nbuf=0):
            return V(BIG[:, off // 4:off // 4 + n // 2].bitcast(BF16), name, nbuf)

        K64 = 65536
        uT = bview(0, 8 * L, "uT", 32)
        uTv = uT[:].rearrange("p (k t) -> p k t", k=8)
        cuT = bview(K64, 8 * LC, "cuT", 2)
        cuTv = cuT[:].rearrange("p (k t) -> p k t", k=8)
        LOC = K64 + 4096
        WKa = fview(LOC, 4096, "wka", 8)
        WKb = fview(LOC + 16384, 4096, "wkb", 8)
        PS = RR("ps", [128, 512], F32, int(os.environ.get("K_PS", "5")), psum=True)
        POR = RR("po", [128, 512], F32, 7 - int(os.environ.get("K_PS", "5")), psum=True)
        PL = TT("pl", [128, 512], F32, psum=True)

        dma("sp", WKa[:, 0:2048], gw_d, [], [WKa.b])
        cp("pool", gwb[:], WKa[:, 0:2048], [WKa.b], [gwb.b])

        tmpf_raw = es.enter_context(nc.sbuf_tensor("t_tmpf_raw", [128, 14 * 512], F32))
        tmpf = RV([V(tmpf_raw[:, i * 512:(i + 1) * 512], "tmpf%d" % i) for i in range(14)])
        tmph = RV([V(tmpf_raw[:, i * 256:(i + 1) * 256], "tmph%d" % i) for i in range(28)])
        adw = [fview(0, 4096, "adw0"), fview(16384, 4096, "adw1")]
        for j in range(12):
            wt = adw[j % 2]
            wv = wt[:].rearrange("p (k n) -> p k n", k=8)
            dma("sp", wv, adaw_d[:, j * 512:(j + 1) * 512].rearrange("(k p) n -> p k n", p=128), [], [wt.b])
            bt = tmpf.get()
            dma("sp", bt[0:1, :], adab_d[0:1, j * 512:(j + 1) * 512], [], [bt.b])
            ps = PS.get()
            for k in range(8):
                mm(ps[0:3, :], scT[:, k * 3:(k + 1) * 3], wv[:, k, :], k == 0, False, [scT.b, wt.b], [ps.b])
            mm(ps[0:3, :], ones3[0:1, 0:3], bt[0:1, :], False, True, [ones3.b, bt.b], [ps.b])
            mt = tmpf.get()
            cp("dve", mt[0:3, :], ps[0:3, :], [ps.b], [mt.b])
            dma("sp", m_scr[:, j * 512:(j + 1) * 512], mt[0:3, :], [mt.b], [])
            if j < 4:
                for q_ in range(4):
                    jj = j * 4 + q_
                    tr(PL[:, jj * 3:jj * 3 + 3], mt[0:3, q_ * 128:(q_ + 1) * 128], identF[0:3, 0:3], [mt.b, identF.b], [PL.b])
                if j == 3:
                    cp("dve", modT[:], PL[:, 0:48], [PL.b], [modT.b])
        S.barrier()

        def mscr_reads():
            return []

        ts("dve", modT[:, 24:48], modT[:, 24:48], 1.0, None, ALU.add, None, [modT.b], [modT.b])

        xin = RR("xin", [128, D], F32, 2)
        wbf = RR("wbf", [128, 8 * 128], BF16, 10)
        tmpb_raw = es.enter_context(nc.sbuf_tensor("t_tmpb_raw", [128, 10 * 512], BF16))
        tmpb = RV([V(tmpb_raw[:, i * 512:(i + 1) * 512], "tmpb%d" % i) for i in range(10)])
        tmphb = RV([V(tmpb_raw[:, i * 256:(i + 1) * 256], "tmphb%d" % i) for i in range(20)])
        small = RR("small", [128, 16], F32, 8)
        Sfin = [TT("Sfin%d" % i, [128, 128]) for i in range(2)]
        SstR = RR("Sst", [128, 9 * 128], F32, 1)
        SallR = RR("Sall", [128, 8 * 128], BF16, 1)
        UsbR = RR("Usb", [128, 8 * 128], F32, 2)
        mask4F = TT("mask4F", [128, 512])
        mask4B = TT("mask4B", [128, 512])
        for q_ in range(4):
            dma("sp", mask4F[:, q_ * 128:(q_ + 1) * 128], maskF_d, [], [mask4F.b])
            dma("sp", mask4B[:, q_ * 128:(q_ + 1) * 128], maskB_d, [], [mask4B.b])
        h0 = TT("h0", [128, 2])
        Vc = TT("Vc", [128, 2 * 128], BF16, nbuf=2)
        stats = RR("stats", [128, 12], F32, 2)

        def load_w(col):
            wb = wbf.get()
            wbv = wb[:].rearrange("p (k n) -> p k n", k=8)
            src = win_d[:, col:col + 128].rearrange("(k p) n -> p k n", p=128)
            S.dma("pool", (lambda o_, i_: (lambda e: e.dma_start(out=o_, in_=i_)))(wbv, src), [], [wb.b])
            return wb

        wq_pending = {}
        pc_state = [0]
        w_f32 = (w1_d, w3_d, w2_d)

        def precast(n_):
            for _ in range(n_):
                k_ = pc_state[0]
                if k_ >= 96:
                    return
                pc_state[0] += 1
                e_, m_ = k_ // 3, k_ % 3
                src = w_f32[m_][e_ * 128:(e_ + 1) * 128, :].rearrange("p (a n) -> p a n", n=2048)
                dst = wb_scr[m_][e_ * 128:(e_ + 1) * 128, :].rearrange("p (a n) -> p a n", n=2048)
                S.dma("pool", (lambda o_, i_: (lambda e: e.dma_start(out=o_, in_=i_)))(dst, src), [], [])

        def rg_cols(c):
            return (c * 128, 512 + c * 128)

        def hg_cols(hd):
            return (1024 + hd * 128, 1536 + hd * 128, 2048 + hd * 128, 2560 + hd * 128, 3072 + hd * 128)

        def prefetch(key, cols):
            wq_pending[key] = [load_w(c_) for c_ in cols]

        def take(key, cols):
            if key not in wq_pending:
                prefetch(key, cols)
            return wq_pending.pop(key)

        def proj_fm(wb, src_v, src_bufs, t0, n, ps):
            wv = wb[:].rearrange("p (k n) -> p k n", k=8)
            for k in range(8):
                mm(ps[:, 0:n], wv[:, k, :], src_v[:, k, t0:t0 + n], k == 0, k == 7, [wb.b] + src_bufs, [ps.b])

        def proj_tm(wb, src_v, src_bufs, t0, ps, c0):
            wv = wb[:].rearrange("p (k n) -> p k n", k=8)
            for k in range(8):
                mm(ps[:, c0:c0 + 128], src_v[:, k, t0:t0 + 128], wv[:, k, :], k == 0, k == 7, [wb.b] + src_bufs, [ps.b])

        OH1 = TT("OH1", [128, 64 * 32], BF16)
        OH2 = TT("OH2", [128, 64 * 32], BF16)
        OH1v = OH1[:].rearrange("p (i e) -> p i e", e=32)
        OH2v = OH2[:].rearrange("p (i e) -> p i e", e=32)
        W1 = TT("W1", [128, 64])
        W2 = TT("W2", [128, 64])
        rw = TT("rw", [128, 8 * 36])
        rwv = rw[:].rearrange("p (k n) -> p k n", k=8)
        dma("sp", rwv, rw_d.rearrange("(k p) n -> p k n", p=128), [], [rw.b])
        rb4 = TT("rb4", [128, 4 * 36])
        for q_ in range(4):
            dma("sp", rb4[:, q_ * 36:(q_ + 1) * 36], rb_d, [], [rb4.b])
        rt = RV([V(tmpf_raw[:, i * 160:(i + 1) * 160], "rt%d" % i) for i in range(8)])
        zrow = TT("zrow", [128, D], BF16)
        S.op("pool", lambda e: e.memset(zrow[:], 0.0), [], [zrow.b])
        zsrc_b = Buf("zsrc")
        dma("sp", zsrc, zrow[0:32, :], [zrow.b], [zsrc_b])
        xs16 = xs_scr.rearrange("(q r j) d -> q r (j d)", q=16, j=32)
        zflat = zsrc.rearrange("(o j) d -> o (j d)", o=1)
        for q_ in range(16):
            dma("sp", xs16[q_], zflat.to_broadcast([NSLOT // (16 * 32), 32 * D]), [zsrc_b], [])

        if os.environ.get('K_VERBOSE'):
            print('SBUF bytes remaining', nc.sbuf_bytes_remaining)
        for b in range(int(os.environ.get('K_NB', '2'))):
            S.barrier()
            def phaseA(src_d, ntile, dst_v, dst_t, r):
                for i in range(ntile):
                    xt = xin.get()
                    dma("sp", xt[:], src_d[i * 128:(i + 1) * 128, :], [], [xt.b])
                    for hlf in range(2):
                        ps = PS.get()
                        for kk in range(4):
                            k = hlf * 4 + kk
                            tr(ps[:, kk * 128:(kk + 1) * 128], xt[:, k * 128:(k + 1) * 128], identF[:], [xt.b, identF.b], [ps.b])
                        for kk in range(4):
                            k = hlf * 4 + kk
                            o_ = dst_v[:, k, i * 128:(i + 1) * 128]
                            i_ = ps[:, kk * 128:(kk + 1) * 128]
                            sc_ = modT[:, (8 + k) * 3 + r:(8 + k) * 3 + r + 1]
                            bi_ = modT[:, k * 3 + r:k * 3 + r + 1]
                            if hlf == 0:
                                act(o_, i_, AF.Identity, [ps.b, modT.b], [dst_t.bs[i]], bias=bi_, scale=sc_)
                            else:
                                ts("dve", o_, i_, sc_, bi_, ALU.mult, ALU.add, [ps.b, modT.b], [dst_t.bs[i]])

            phaseA(ctx_d[b], 2, cuTv, cuT, 2)
            phaseA(x_d[b], 32, uTv, uT, b)

            xc = WKa
            hf = WKb

            def rg_gates(c, dirn, xcb, n, xc_ap, xc_bufs):
                gi = dirn * 8
                pr = PS.get()
                pi = PS.get()
                mm(pr[:, 0:n], gwb[:, (gi + c) * 128:(gi + c + 1) * 128], xcb[:, 0:n], True, True, [gwb.b, xcb.b], [pr.b])
                mm(pi[:, 0:n], gwb[:, (gi + 4 + c) * 128:(gi + 4 + c + 1) * 128], xcb[:, 0:n], True, True, [gwb.b, xcb.b], [pi.b])
                r_ = tmpf.get()
                i_ = tmpf.get()
                act(r_[:, 0:n], pr[:, 0:n], AF.Sigmoid, [pr.b, gb.b], [r_.b], bias=gb[:, gi + c:gi + c + 1])
                act(i_[:, 0:n], pi[:, 0:n], AF.Sigmoid, [pi.b, gb.b], [i_.b], bias=gb[:, gi + 4 + c:gi + 4 + c + 1])
                a_ = tmpf.get()
                act(a_[:, 0:n], r_[:, 0:n], AF.Exp, [r_.b, cL.b], [a_.b], scale=cL[:, dirn * 4 + c:dirn * 4 + c + 1])
                a2 = tmpf.get()
                tt("pool", a2[:, 0:n], a_[:, 0:n], a_[:, 0:n], ALU.mult, [a_.b], [a2.b])
                act(a2[:, 0:n], a2[:, 0:n], AF.Sqrt, [a2.b], [a2.b], bias=1.0, scale=-1.0)
                tt("pool", i_[:, 0:n], i_[:, 0:n], xc_ap, ALU.mult, [i_.b] + xc_bufs, [i_.b])
                tt("dve", i_[:, 0:n], i_[:, 0:n], a2[:, 0:n], ALU.mult, [i_.b, a2.b], [i_.b])
                return a_, i_

            def conv(c, src, n, rows, dst_ap, dst_bufs):
                w_ = n // rows
                sv = src[:, 0:n].rearrange("p (r w) -> p r w", r=rows)
                dv = dst_ap.rearrange("p (r w) -> p r w", r=rows)
                wc = lambda j: convw[:, c * 4 + j:c * 4 + j + 1]
                rd = [src.b, convw.b, convb.b]
                ts("dve", dst_ap, src[:, 0:n], wc(1), convb[:, c:c + 1], ALU.mult, ALU.add, rd, dst_bufs)
                stt("dve", dv[:, :, 1:w_], sv[:, :, 0:w_ - 1], wc(0), dv[:, :, 1:w_], ALU.mult, ALU.add, rd, dst_bufs)
                stt("dve", dv[:, :, 0:w_ - 1], sv[:, :, 1:w_], wc(2), dv[:, :, 0:w_ - 1], ALU.mult, ALU.add, rd, dst_bufs)
                stt("dve", dv[:, :, 0:w_ - 2], sv[:, :, 2:w_], wc(3), dv[:, :, 0:w_ - 2], ALU.mult, ALU.add, rd, dst_bufs)

            for c in range(int(os.environ.get('K_NRG', '4'))):
                w_x, w_g = take(("rg", b, c), rg_cols(c))
                precast(6)
                if c + 1 < 4:
                    prefetch(("rg", b, c + 1), rg_cols(c + 1))
                else:
                    prefetch(("hg", b, 0), hg_cols(0))
                ps = PS.get()
                proj_fm(w_x, cuTv, cuT.bs, 0, LC, ps)
                rx = tmpf.get()
                cp("act", rx[:, 0:LC], ps[:, 0:LC], [ps.b], [rx.b])
                xcc = tmpf.get()
                conv(c, rx, LC, 1, xcc[:, 0:LC], [xcc.b])
                xcb = tmpb.get()
                cp("pool", xcb[:, 0:LC], xcc[:, 0:LC], [xcc.b], [xcb.b])
                for dirn in range(2):
                    a_, u_ = rg_gates(c, dirn, xcb, LC, xcc[:, 0:LC], [xcc.b])
                    hh = tmpf.get()
                    if dirn == 0:
                        S.op("dve", (lambda o, d0, d1: (lambda e: e.tensor_tensor_scan(o, d0, d1, 0.0, ALU.mult, ALU.add)))(
                            hh[:, 0:LC], a_[:, 0:LC], u_[:, 0:LC]), [a_.b, u_.b], [hh.b])
                        cp("dve", h0[:, 0:1], hh[:, LC - 1:LC], [hh.b], [h0.b])
                    else:
                        S.op("dve", (lambda o, d0, d1: (lambda e: e.tensor_tensor_scan(o, d0, d1, 0.0, ALU.mult, ALU.add)))(
                            hh[:, 0:LC][:, ::-1], a_[:, 0:LC][:, ::-1], u_[:, 0:LC][:, ::-1]), [a_.b, u_.b], [hh.b])
                        cp("dve", h0[:, 1:2], hh[:, 0:1], [hh.b], [h0.b])
                def rg_gates_g(dirn, xcb, n, xc_ap, xc_bufs):
                    gi = dirn * 8
                    pr = PS.get()
                    pi = PS.get()
                    mm(pr[:, 0:n], gwb[:, (gi + c) * 128:(gi + c + 1) * 128], xcb[:, 0:n], True, True, [gwb.b, xcb.b], [pr.b])
                    mm(pi[:, 0:n], gwb[:, (gi + 4 + c) * 128:(gi + 4 + c + 1) * 128], xcb[:, 0:n], True, True, [gwb.b, xcb.b], [pi.b])
                    yield
                    r_ = tmpf.get()
                    act(r_[:, 0:n], pr[:, 0:n], AF.Sigmoid, [pr.b, gb.b], [r_.b], bias=gb[:, gi + c:gi + c + 1])
                    yield
                    i_ = tmpf.get()
                    act(i_[:, 0:n], pi[:, 0:n], AF.Sigmoid, [pi.b, gb.b], [i_.b], bias=gb[:, gi + 4 + c:gi + 4 + c + 1])
                    yield
                    a_ = tmpf.get()
                    act(a_[:, 0:n], r_[:, 0:n], AF.Exp, [r_.b, cL.b], [a_.b], scale=cL[:, dirn * 4 + c:dirn * 4 + c + 1])
                    yield
                    a2 = tmpf.get()
                    tt("dve", a2[:, 0:n], a_[:, 0:n], a_[:, 0:n], ALU.mult, [a_.b], [a2.b])
                    tt("dve", i_[:, 0:n], i_[:, 0:n], xc_ap, ALU.mult, [i_.b] + xc_bufs, [i_.b])
                    yield
                    act(a2[:, 0:n], a2[:, 0:n], AF.Sqrt, [a2.b], [a2.b], bias=1.0, scale=-1.0)
                    yield
                    tt("dve", i_[:, 0:n], i_[:, 0:n], a2[:, 0:n], ALU.mult, [i_.b, a2.b], [i_.b])
                    return a_, i_

                def rg_fwd(s):
                    t0 = s * 512
                    ps = PS.get()
                    proj_fm(w_x, uTv, uT.bs[s * 4:(s + 1) * 4], t0, 512, ps)
                    yield
                    rx = tmpf.get()
                    cp("act", rx[:], ps[:], [ps.b], [rx.b])
                    yield
                    conv(c, rx, 512, 8, xc[:, t0:t0 + 512], [xc.bs[s]])
                    yield
                    xcb = tmpb.get()
                    cp("pool", xcb[:], xc[:, t0:t0 + 512], [xc.bs[s]], [xcb.b])
                    yield
                    a_, u_ = yield from rg_gates_g(0, xcb, 512, xc[:, t0:t0 + 512], [xc.bs[s]])
                    yield
                    init = h0[:, 0:1] if s == 0 else hf[:, t0 - 1:t0]
                    ib = [h0.b] if s == 0 else [hf.bs[s - 1]]
                    S.op("dve", (lambda o, d0, d1, ini: (lambda e: e.tensor_tensor_scan(o, d0, d1, ini, ALU.mult, ALU.add)))(
                        hf[:, t0:t0 + 512], a_[:], u_[:], init), [a_.b, u_.b] + ib, [hf.bs[s]])

                hbt = {}

                def rg_bwd(s):
                    t0 = s * 512
                    xcb = tmpb.get()
                    cp("pool", xcb[:], xc[:, t0:t0 + 512], [xc.bs[s]], [xcb.b])
                    yield
                    a_, u_ = yield from rg_gates_g(1, xcb, 512, xc[:, t0:t0 + 512], [xc.bs[s]])
                    yield
                    hb = tmpf.get()
                    hbt[s] = hb
                    init = h0[:, 1:2] if s == 7 else hbt[s + 1][:, 0:1]
                    ib = [h0.b] if s == 7 else [hbt[s + 1].b]
                    S.op("dve", (lambda o, d0, d1, ini: (lambda e: e.tensor_tensor_scan(o, d0, d1, ini, ALU.mult, ALU.add)))(
                        hb[:, ::-1], a_[:, ::-1], u_[:, ::-1], init), [a_.b, u_.b] + ib, [hb.b])
                    yield
                    ps = PS.get()
                    proj_fm(w_g, uTv, uT.bs[s * 4:(s + 1) * 4], t0, 512, ps)
                    yield
                    gl = tmpf.get()
                    act(gl[:], ps[:], AF.Gelu_apprx_tanh, [ps.b], [gl.b])
                    yield
                    hs = tmpf.get()
                    tt("dve", hs[:], hf[:, t0:t0 + 512], hb[:], ALU.add, [hf.bs[s], hb.b], [hs.b])
                    yield
                    ob = tmpb.get()
                    tt("dve", ob[:], hs[:], gl[:], ALU.mult, [hs.b, gl.b], [ob.b])
                    dma("sp", mix_scr[b, c, :, t0:t0 + 512], ob[:], [ob.b], [])

                for s in range(0, 8, 2):
                    zip_run([rg_fwd(s), rg_fwd(s + 1)])
                for s in range(7, -1, -2):
                    zip_run([rg_bwd(s), rg_bwd(s - 1)])

            S.barrier()
            of = fview(LOC, 4096, "of", 16)
            qall = bview(LOC + 16384, L, "qall", 16)
            Vall = bview(LOC + 16384 + 8192, 32 * 128, "Vall", 32)

            def hg_A(hd, dirn, src_v, src_bufs, t0, n, w_z, w_q, w_v, w_hg, with_out, Vt, Vbufs, vcol0, sidx, po, pcol, first, Usb, ucol):
                nch = n // 64
                nw = n // 128
                li = dirn * 4 + hd
                pz = PS.get()
                proj_fm(w_z, src_v, src_bufs, t0, n, pz)
                yield
                sg = tmph.get()
                act(sg[:, 0:n], pz[:, 0:n], AF.Exp, [pz.b], [sg.b], scale=-1.0)
                yield
                act(sg[:, 0:n], sg[:, 0:n], AF.Ln, [sg.b], [sg.b], bias=1.0)
                yield
                act(sg[:, 0:n], sg[:, 0:n], AF.Exp, [sg.b], [sg.b], scale=-1.0)
                yield
                lf = tmph.get()
                act(lf[:, 0:n], sg[:, 0:n], AF.Ln, [sg.b, oml.b, lb.b], [lf.b], bias=lb[:, li:li + 1], scale=oml[:, li:li + 1])
                kk_ = tmph.get()
                ts("dve", kk_[:, 0:n], sg[:, 0:n], noml[:, li:li + 1], oml[:, li:li + 1], ALU.mult, ALU.add,
                   [sg.b, oml.b, noml.b], [kk_.b])
                yield
                Gc = tmph.get()
                S.op("dve", (lambda o, d0, d1: (lambda e: e.tensor_tensor_scan(o, d0, d1, 0.0, ALU.mult, ALU.add)))(
                    Gc[:, 0:n], resetm[:, 0:n], lf[:, 0:n]), [resetm.b, lf.b], [Gc.b])
                yield
                Gv = Gc[:, 0:n].rearrange("p (c t) -> p c t", t=64)
                Glast = Gv[:, :, 63:64].to_broadcast([128, nch, 64])
                dl = small.get()
                act(dl[:, 0:nch], Gc[:, 63:n:64], AF.Exp, [Gc.b], [dl.b])
                e3 = tmph.get()
                e3v = e3[:, 0:n].rearrange("p (c t) -> p c t", t=64)
                if dirn == 0:
                    Hq = Gc
                    tt("dve", e3v, Glast, Gv, ALU.subtract, [Gc.b], [e3.b])
                else:
                    e1 = tmph.get()
                    e1v = e1[:, 0:n].rearrange("p (c t) -> p c t", t=64)
                    tt("dve", e1v, Glast, Gv, ALU.subtract, [Gc.b], [e1.b])
                    tt("pool", e3[:, 0:n], Gc[:, 0:n], lf[:, 0:n], ALU.subtract, [Gc.b, lf.b], [e3.b])
                    yield
                    tt("pool", e1[:, 0:n], e1[:, 0:n], lf[:, 0:n], ALU.add, [e1.b, lf.b], [e1.b])
                    Hq = e1
                yield
                kd = tmphb.get()
                act(e3[:, 0:n], e3[:, 0:n], AF.Exp, [e3.b], [e3.b])
                yield
                tt("pool", kd[:, 0:n], kk_[:, 0:n], e3[:, 0:n], ALU.mult, [kk_.b, e3.b], [kd.b])
                qg = None
                if with_out:
                    kg = tmphb.get()
                    en = tmph.get()
                    act(en[:, 0:n], Hq[:, 0:n], AF.Exp, [Hq.b], [en.b], scale=-1.0)
                    if dirn == 0:
                        pq = PS.get()
                        proj_fm(w_q, src_v, src_bufs, t0, n, pq)
                        yield
                        sq_ = tmph.get()
                        act(sq_[:, 0:n], pq[:, 0:n], AF.Exp, [pq.b], [sq_.b], scale=-1.0)
                        yield
                        act(sq_[:, 0:n], sq_[:, 0:n], AF.Ln, [sq_.b], [sq_.b], bias=1.0)
                        yield
                        act(sq_[:, 0:n], sq_[:, 0:n], AF.Exp, [sq_.b], [sq_.b], scale=-1.0)
                        yield
                        tt("dve", qall[:, t0:t0 + n], sq_[:, 0:n], pq[:, 0:n], ALU.mult, [sq_.b, pq.b], [qall.bs[sidx]])
                    yield
                    tt("pool", kg[:, 0:n], kk_[:, 0:n], en[:, 0:n], ALU.mult, [kk_.b, en.b], [kg.b])
                    ep = tmph.get()
                    act(ep[:, 0:n], Hq[:, 0:n], AF.Exp, [Hq.b], [ep.b])
                    yield
                    qg = tmphb.get()
                    tt("dve", qg[:, 0:n], qall[:, t0:t0 + n], ep[:, 0:n], ALU.mult, [qall.bs[sidx], ep.b], [qg.b])
                if dirn == 0:
                    pv = PS.get()
                    for w in range(nw):
                        proj_tm(w_v, src_v, src_bufs, t0 + w * 128, pv, w * 128)
                    yield
                    cp("act", Vt[:, vcol0:vcol0 + n], pv[:, 0:n], [pv.b], Vbufs)
                yield
                pk = PS.get()
                pkb = pk[:].bitcast(BF16)
                for w in range(nw):
                    tr(pkb[:, w * 128:(w + 1) * 128], kd[:, w * 128:(w + 1) * 128], identB[:], [kd.b, identB.b], [pk.b])
                yield
                kdT = tmphb.get()
                cp("act", kdT[:, 0:n], pkb[:, 0:n], [pk.b], [kdT.b])
                yield
                Uv = Usb[:, ucol:ucol + nch * 128].rearrange("p (w c k) -> p w c k", c=2, k=128)
                pus = [PS.get(), PS.get()]
                for cc in range(2):
                    pu = pus[cc]
                    for w in range(nw):
                        mm(pu[:, w * 128:(w + 1) * 128], kdT[cc * 64:(cc + 1) * 64, w * 128:(w + 1) * 128],
                           Vt[cc * 64:(cc + 1) * 64, vcol0 + w * 128:vcol0 + (w + 1) * 128], True, True, [kdT.b] + Vbufs, [pu.b])
                    yield
                for cc in range(2):
                    cp("act", Uv[:, :, cc, :], pus[cc][:, 0:nw * 128].rearrange("p (w k) -> p w k", k=128), [pus[cc].b], [Usb.b])
                    yield
                if with_out:
                    pa = PS.get()
                    for w in range(nw):
                        mm(pa[:, w * 128:(w + 1) * 128], kg[:, w * 128:(w + 1) * 128], qg[:, w * 128:(w + 1) * 128], True, True,
                           [kg.b, qg.b], [pa.b])
                    yield
                    AT = tmphb.get()
                    msk = mask4F if dirn == 0 else mask4B
                    tt("dve", AT[:, 0:n], pa[:, 0:n], msk[:, 0:n], ALU.mult, [pa.b, msk.b], [AT.b])
                    yield
                    for w in range(nw):
                        mm(po[:, pcol + w * 128:pcol + (w + 1) * 128], Vt[:, vcol0 + w * 128:vcol0 + (w + 1) * 128],
                           AT[:, w * 128:(w + 1) * 128], first and w == 0, False, Vbufs + [AT.b], [po.b], sgc=True)
                return dict(hd=hd, dirn=dirn, src_v=src_v, src_bufs=src_bufs, t0=t0, n=n, nch=nch, w_hg=w_hg, with_out=with_out,
                            dl=dl, Usb=Usb, ucol=ucol, qg=qg, po=po, pcol=pcol, sidx=sidx)

            def hg_B(c_, last_in_bank):
                dirn, n, nch, t0, hd, sidx = c_["dirn"], c_["n"], c_["nch"], c_["t0"], c_["hd"], c_["sidx"]
                dl, Usb, qg, po, pcol, ucol = c_["dl"], c_["Usb"], c_["qg"], c_["po"], c_["pcol"], c_["ucol"]
                order = list(range(nch)) if dirn == 0 else list(range(nch - 1, -1, -1))
                Sst = SstR.get()
                cp("pool", Sst[:, 0:128], Sfin[dirn][:], [Sfin[dirn].b], [Sst.b])
                yield
                for i, c in enumerate(order):
                    stt("dve", Sst[:, (i + 1) * 128:(i + 2) * 128], Sst[:, i * 128:(i + 1) * 128], dl[:, c:c + 1],
                        Usb[:, ucol + c * 128:ucol + (c + 1) * 128], ALU.mult, ALU.add, [Sst.b, dl.b, Usb.b], [Sst.b])
                    yield
                cp("pool", Sfin[dirn][:], Sst[:, nch * 128:(nch + 1) * 128], [Sst.b], [Sfin[dirn].b])
                if not c_["with_out"]:
                    return
                Sall = SallR.get()
                cp("act", Sall[:, 0:nch * 128], Sst[:, 0:nch * 128], [Sst.b], [Sall.b])
                yield
                for i, c in enumerate(order):
                    mm(po[:, pcol + c * 64:pcol + (c + 1) * 64], Sall[:, i * 128:(i + 1) * 128], qg[:, c * 64:(c + 1) * 64], False,
                       last_in_bank and i == nch - 1, [Sall.b, qg.b], [po.b], sgc=True)
                yield
                if dirn == 0:
                    cp("act", of[:, t0:t0 + n], po[:, pcol:pcol + n], [po.b], [of.bs[sidx]])
                    yield
                return

            def get512():
                if tmph.i % 2:
                    tmph.i += 1
                i_ = tmph.i % len(tmph.tiles)
                a_ = tmph.get()
                b_ = tmph.get()
                return tmpf_raw[:, i_ * 256:i_ * 256 + 512], [a_.b, b_.b]

            def get512b():
                if tmphb.i % 2:
                    tmphb.i += 1
                i_ = tmphb.i % len(tmphb.tiles)
                a_ = tmphb.get()
                b_ = tmphb.get()
                return tmpb_raw[:, i_ * 256:i_ * 256 + 512], [a_.b, b_.b]

            def hg_epi(hd, po, t0, sidxs, w_hg):
                n = 512
                src_bufs = uT.bs[t0 // 128:t0 // 128 + 4]
                ofb = [of.bs[i_] for i_ in sidxs]
                osum, ob_ = get512()
                tt("dve", osum, of[:, t0:t0 + n], po[:, 0:n], ALU.add, ofb + [po.b], ob_)
                yield
                sq, sqb = get512()
                tt("pool", sq, osum, osum, ALU.mult, ob_, sqb)
                yield
                pn = PS.get()
                mm(pn[:, 0:n], onesF[:], sq, True, True, [onesF.b] + sqb, [pn.b])
                yield
                act(sq, pn[:, 0:n], AF.Ln, [pn.b], sqb, bias=EPS, scale=1.0 / 128.0)
                yield
                act(sq, sq, AF.Exp, sqb, sqb, scale=-0.5)
                yield
                tt("pool", osum, osum, sq, ALU.mult, ob_ + sqb, ob_)
                yield
                ph = PS.get()
                proj_fm(w_hg, uTv, src_bufs, t0, n, ph)
                yield
                sl, slb = get512()
                act(sl, ph[:, 0:n], AF.Exp, [ph.b], slb, scale=-1.0)
                yield
                act(sl, sl, AF.Ln, slb, slb, bias=1.0)
                yield
                act(sl, sl, AF.Exp, slb, slb, scale=-1.0)
                yield
                tt("dve", sl, sl, ph[:, 0:n], ALU.mult, slb + [ph.b], slb)
                yield
                ob, obb = get512b()
                stt("dve", ob, sl, ng[:, hd:hd + 1], osum, ALU.mult, ALU.mult, slb + [ng.b] + ob_, obb)
                dma("sp", mix_scr[b, 4 + hd, :, t0:t0 + n], ob, obb, [])

            def zip_run(gens):
                res = [None] * len(gens)
                live = list(range(len(gens)))
                while live:
                    for gi in list(live):
                        try:
                            next(gens[gi])
                        except StopIteration as e_:
                            res[gi] = e_.value
                            live.remove(gi)
                return res

            NSUB = 16
            for hd in range(int(os.environ.get('K_NHG', '4'))):
                w_q, w_zf, w_zb, w_v, w_hg = take(("hg", b, hd), hg_cols(hd))
                precast(6)
                if hd + 1 < 4:
                    prefetch(("hg", b, hd + 1), hg_cols(hd + 1))
                for dirn in range(2):
                    S.op("pool", (lambda o: (lambda e: e.memset(o, 0.0)))(Sfin[dirn][:]), [], [Sfin[dirn].b])

                def lat(dirn, s__, po, pcol, first, ub_):
                    return hg_A(hd, dirn, uTv, uT.bs[s__ * 2:(s__ + 1) * 2], s__ * 256, 256, w_zf if dirn == 0 else w_zb, w_q, w_v, w_hg,
                                True, Vall, Vall.bs[s__ * 2:(s__ + 1) * 2], s__ * 256, s__, po, pcol, first, ub_, pcol * 2)

                pairs = []
                pairs.append(lambda: [hg_A(hd, 0, cuTv, cuT.bs, 0, LC, w_zf, w_q, w_v, w_hg, False, Vc, [Vc.b], 0, 0, None, 0, False, UsbR.get(), 0)])
                pairs.append(lambda: [hg_A(hd, 1, cuTv, cuT.bs, 0, LC, w_zb, w_q, w_v, w_hg, False, Vc, [Vc.b], 0, 0, None, 0, False, UsbR.get(), 0)])
                nsub = int(os.environ.get('K_NS', str(NSUB)))
                for p_ in range(nsub // 2):
                    def mkp(dirn, sa, sb):
                        def f():
                            po = POR.get()
                            ub_ = UsbR.get()
                            return [lat(dirn, sa, po, (sa % 2) * 256, True, ub_), lat(dirn, sb, po, (sb % 2) * 256, False, ub_)]
                        return f
                    pairs.append(mkp(0, 2 * p_, 2 * p_ + 1))
                for p_ in range(nsub // 2 - 1, -1, -1):
                    pairs.append(mkp(1, 2 * p_ + 1, 2 * p_))
                def gen_B(prev_):
                    for k_, c_ in enumerate(prev_):
                        yield from hg_B(c_, k_ == len(prev_) - 1)

                def gen_E(prev_):
                    t0_ = min(c_["t0"] for c_ in prev_)
                    yield from hg_epi(hd, prev_[0]["po"], t0_, [c_["sidx"] for c_ in prev_], w_hg)

                def needs_epi(prev_):
                    return prev_ is not None and prev_[0]["with_out"] and prev_[0]["dirn"] == 1
                prev = zip_run(pairs[0]())
                prevE = None
                for pf in pairs[1:]:
                    gens = pf()
                    extra = [gen_B(prev)] + ([gen_E(prevE)] if prevE is not None else [])
                    res = zip_run(gens + extra)
                    prevE = prev if needs_epi(prev) else None
                    prev = res[:len(gens)]
                zip_run([gen_B(prev)] + ([gen_E(prevE)] if prevE is not None else []))
                if needs_epi(prev):
                    zip_run([gen_E(prev)])

            S.barrier()
            woutb = bview(0, 8 * D, "woutb")
            woutv = woutb[:].rearrange("p (k n) -> p k n", k=8)
            wost = fview(16384, 4096, "wost")
            bcast = [fview(32768 + i * 4096, D, "bc%d" % i) for i in range(3)]
            lnp = fview(32768 + 12288, 2 * D, "lnp")
            dma("sp", lnp[:], lnp_d[:, 0:2 * D], [], [lnp.b])
            for j, lo in enumerate((2048, 3072, 4096)):
                dma("sp", bcast[j][:], m_scr[b:b + 1, lo:lo + D].partition_broadcast(128), [], [bcast[j].b])
            for hlf in range(2):
                wv = wost[:].rearrange("p (k n) -> p k n", k=8)
                dma("sp", wv, wout_d[:, hlf * 512:(hlf + 1) * 512].rearrange("(k p) n -> p k n", p=128), [], [wost.b])
                tt("pool", woutv[:, :, hlf * 512:(hlf + 1) * 512], wv,
                   bcast[0][:, hlf * 512:(hlf + 1) * 512].unsqueeze(1).to_broadcast([128, 8, 512]), ALU.mult,
                   [wost.b, bcast[0].b], [woutb.b])
            ts("dve", bcast[2][:], bcast[2][:], 1.0, None, ALU.add, None, [bcast[2].b], [bcast[2].b])
            tt("dve", bcast[0][:], lnp[:, 0:D], bcast[2][:], ALU.mult, [lnp.b, bcast[2].b, woutb.b], [bcast[0].b])
            tt("dve", bcast[2][:], lnp[:, D:2 * D], bcast[2][:], ALU.mult, [lnp.b, bcast[2].b], [bcast[2].b])
            tt("dve", bcast[1][:], bcast[1][:], bcast[2][:], ALU.add, [bcast[1].b, bcast[2].b], [bcast[1].b])
            S.barrier()

            class _R:
                def __init__(self, items):
                    self.items = items
                    self.i = 0

                def get(self):
                    t = self.items[self.i % len(self.items)]
                    self.i += 1
                    return t
            mixin = _R([bview(53248 + i * 8192, 8 * 512, "mixin%d" % i) for i in range(2)])
            zt = _R([fview(K64 + 4096 + i * 4096, D, "zt%d" % i) for i in range(6)]
                    + [fview(28672, D, "zt6"), fview(32768 + 8192, D, "zt7")])
            PSC = RV(PS.tiles + POR.tiles)
            xck = [fview(K64 + 4096 + 6 * 4096 + i * 4096, D, "xck%d" % i) for i in range(2)] + xin.tiles

            def c_load_x(i_):
                xt_ = xck[i_ % 4]
                dma("sp", xt_[:], x_d[b, i_ * 128:(i_ + 1) * 128, :], [], [xt_.b])
            for i_ in range(4):
                c_load_x(i_)

            def c_load_mix(g_):
                mi_ = mixin.items[g_ % 2]
                dma("sp", mi_[:].rearrange("p (c t) -> p c t", c=8),
                    mix_scr[b, :, :, g_ * 512:(g_ + 1) * 512].rearrange("c p t -> p c t"), [], [mi_.b])
            c_load_mix(0)
            u2b = _R([bview(16384 + i * 2048, D, "u2b%d" % i) for i in range(2)])
            u2T = _R([fview(16384 + 4096 + i * 4096, D, "u2T%d" % i) for i in range(2)])
            for g in range(8):
                mi = mixin.items[g % 2]
                miv = mi[:].rearrange("p (c t) -> p c t", c=8)
                if g + 1 < 8:
                    c_load_mix(g + 1)
                def c_tile(ii):
                    i = g * 4 + ii
                    row0 = b * L + i * 128
                    xt = xck[i % 4]
                    z = zt.get()
                    for hlf in range(2):
                        ps_ = PSC.get()
                        for c in range(8):
                            mm(ps_[:], miv[:, c, ii * 128:(ii + 1) * 128], woutv[:, c, hlf * 512:(hlf + 1) * 512],
                               c == 0, c == 7, [mi.b, woutb.b], [ps_.b])
                        yield
                        stt("dve", z[:, hlf * 512:(hlf + 1) * 512], xt[:, hlf * 512:(hlf + 1) * 512], ALPHA, ps_[:],
                            ALU.mult, ALU.add, [xt.b, ps_.b], [z.b])
                        yield
                    if i + 4 < 32:
                        c_load_x(i + 4)
                    mv = ln_stats(S, z, stats, small, act)
                    yield
                    act(z[:], z[:], AF.Identity, [z.b, mv.b], [z.b], bias=mv[:, 3:4], scale=mv[:, 2:3])
                    yield
                    u2 = zt.get()
                    tt("dve", u2[:], z[:], bcast[0][:], ALU.mult, [z.b, bcast[0].b], [u2.b])
                    yield
                    tt("dve", u2[:], u2[:], bcast[1][:], ALU.add, [u2.b, bcast[1].b], [u2.b])
                    yield
                    tt("dve", z[:], z[:], lnp[:, 0:D], ALU.mult, [z.b, lnp.b], [z.b])
                    yield
                    tt("dve", z[:], z[:], lnp[:, D:2 * D], ALU.add, [z.b, lnp.b], [z.b])
                    dma("sp", h1_scr[row0:row0 + 128, :], z[:], [z.b], [])
                    if dbg == 1:
                        dma("sp", dbg_d[row0:row0 + 128, :], z[:], [z.b], [])
                    yield
                    ub = u2b.get()
                    cp("act", ub[:], u2[:], [u2.b], [ub.b])
                    dma("sp", u2_scr[row0:row0 + 128, :], ub[:], [ub.b], [])
                    u2s[ii] = u2

                u2s = [None] * 4
                zip_run([c_tile(ii_) for ii_ in range(4)])
                for ii in range(4):
                    u2 = u2s[ii]
                    uT2 = u2T.get()
                    uT2v = uT2[:].rearrange("p (k t) -> p k t", k=8)
                    for hlf in range(2):
                        pt = PSC.get()
                        for kk in range(4):
                            k = hlf * 4 + kk
                            tr(pt[:, kk * 128:(kk + 1) * 128], u2[:, k * 128:(k + 1) * 128], identF[:], [u2.b, identF.b], [pt.b])
                        if hlf == 0:
                            cp("act", uT2[:, 0:512], pt[:], [pt.b], [uT2.b])
                        else:
                            cp("dve", uT2[:, 512:1024], pt[:], [pt.b], [uT2.b])
                    for k in range(8):
                        mm(PL[:, ii * 36:(ii + 1) * 36], uT2v[:, k, :], rwv[:, k, :], k == 0, k == 7, [uT2.b, rw.b], [PL.b])
                pl = PL
                i0 = g * 4 + b * 32
                lg = rt.get()
                tt("dve", lg[:, 0:144], pl[:, 0:144], rb4[:], ALU.add, [pl.b, rb4.b], [lg.b])
                lgv = lg[:, 0:144].rearrange("p (i n) -> p i n", n=36)
                gmax = small.get()
                S.op("dve", (lambda o, i_: (lambda e: e.tensor_reduce(o, i_, AX.X, ALU.max)))(gmax[:, 0:4], lgv[:, :, 0:4]), [lg.b], [gmax.b])
                gsh = rt.get()
                gshv = gsh[:, 0:16].rearrange("p (i n) -> p i n", n=4)
                tt("dve", gshv, lgv[:, :, 0:4], gmax[:, 0:4].unsqueeze(2).to_broadcast([128, 4, 4]), ALU.subtract, [lg.b, gmax.b], [gsh.b])
                gex = rt.get()
                act(gex[:, 0:16], gsh[:, 0:16], AF.Exp, [gsh.b], [gex.b])
                S.op("dve", (lambda o, i_: (lambda e: e.tensor_reduce(o, i_, AX.X, ALU.add)))(
                    gmax[:, 4:8], gex[:, 0:16].rearrange("p (i n) -> p i n", n=4)), [gex.b], [gmax.b])
                S.op("dve", (lambda o, i_: (lambda e: e.reciprocal(o, i_)))(gmax[:, 8:12], gmax[:, 4:8]), [gmax.b], [gmax.b])
                pen = rt.get()
                ts("dve", pen[:, 0:16], gsh[:, 0:16], 0.0, None, ALU.is_equal, None, [gsh.b], [pen.b])
                ts("dve", pen[:, 0:16], pen[:, 0:16], -1.0, 1e30, ALU.add, ALU.mult, [pen.b], [pen.b])
                elm = rt.get()
                for j in range(4):
                    tt("dve", elm[:, j * 32:(j + 1) * 32].rearrange("p (g e) -> p g e", e=8),
                       lgv[:, j, 4:36].rearrange("p (g e) -> p g e", e=8),
                       pen[:, j * 4:(j + 1) * 4].unsqueeze(2).to_broadcast([128, 4, 8]), ALU.add, [lg.b, pen.b], [elm.b])
                elmv = elm[:, 0:128].rearrange("p (i e) -> p i e", e=32)
                m12 = small.get()
                S.op("dve", (lambda o, i_: (lambda e: e.tensor_reduce(o, i_, AX.X, ALU.max)))(m12[:, 0:4], elmv), [elm.b], [m12.b])
                oh1 = rt.get()
                oh1v = oh1[:, 0:128].rearrange("p (i e) -> p i e", e=32)
                tt("dve", oh1v, elmv, m12[:, 0:4].unsqueeze(2).to_broadcast([128, 4, 32]), ALU.is_equal, [elm.b, m12.b], [oh1.b])
                elm2 = rt.get()
                stt("dve", elm2[:, 0:128], oh1[:, 0:128], -1e30, elm[:, 0:128], ALU.mult, ALU.add, [oh1.b, elm.b], [elm2.b])
                elm2v = elm2[:, 0:128].rearrange("p (i e) -> p i e", e=32)
                S.op("dve", (lambda o, i_: (lambda e: e.tensor_reduce(o, i_, AX.X, ALU.max)))(m12[:, 4:8], elm2v), [elm2.b], [m12.b])
                oh2 = rt.get()
                oh2v = oh2[:, 0:128].rearrange("p (i e) -> p i e", e=32)
                tt("dve", oh2v, elm2v, m12[:, 4:8].unsqueeze(2).to_broadcast([128, 4, 32]), ALU.is_equal, [elm2.b, m12.b], [oh2.b])
                cp("pool", OH1[:, i0 * 32:(i0 + 4) * 32], oh1[:, 0:128], [oh1.b], [OH1.b])
                cp("pool", OH2[:, i0 * 32:(i0 + 4) * 32], oh2[:, 0:128], [oh2.b], [OH2.b])
                tt("dve", m12[:, 8:12], m12[:, 0:4], m12[:, 4:8], ALU.subtract, [m12.b], [m12.b])
                act(m12[:, 8:12], m12[:, 8:12], AF.Sigmoid, [m12.b], [m12.b])
                tt("dve", W1[:, i0:i0 + 4], m12[:, 8:12], gmax[:, 8:12], ALU.mult, [m12.b, gmax.b], [W1.b])
                tt("dve", W2[:, i0:i0 + 4], gmax[:, 8:12], W1[:, i0:i0 + 4], ALU.subtract, [gmax.b, W1.b], [W2.b])

        def moe_phase():
            precast(96)
            S.barrier()
            PSM = RV(PS.tiles + POR.tiles + [PL])
            o = [0]

            def af(n, name, nbuf=0):
                v = fview(o[0], n, name, nbuf)
                o[0] += n * 4
                return v

            def ab(n, name, nbuf=0):
                v = bview(o[0], n, name, nbuf)
                o[0] += n * 2
                return v
            DST1 = V(BIG[:, o[0] // 4:o[0] // 4 + 64].bitcast(I32), "DST1")
            o[0] += 256
            DST2 = V(BIG[:, o[0] // 4:o[0] // 4 + 64].bitcast(I32), "DST2")
            o[0] += 256
            WIDX = V(BIG[:, o[0] // 4:o[0] // 4 + NBLK].bitcast(I32), "WIDX")
            o[0] += NBLK * 4
            lnp2 = af(2 * D, "lnp2")
            g2b = af(D, "g2b")
            meta_end = o[0]
            SELb = ab(2048, "SELb")
            PRE = af(2048, "PRE")
            TOT = af(2048, "TOT")
            BASE = af(2048, "BASE")
            DD = af(2048, "DD")
            CMPB = af(NBLK * 32, "CMPB")
            triuF = af(128, "triuF")
            triuB = ab(128, "triuB")
            thr = af(64 + NBLK, "thr")
            iot = af(2, "iot")
            CNT = af(32, "CNT")
            NBK = af(32, "NBK")
            PEND = af(32, "PEND")
            PST = af(32, "PST")
            ones32 = af(32, "ones32")
            D1f = af(64, "D1f")
            D2f = af(64, "D2f")
            BLKE = af(NBLK, "BLKE")
            WIDXf = af(NBLK, "WIDXf")
            SAME = af(NBLK, "SAME")
            tmp_end = o[0]

            dma("sp", triuF[:], triu_d, [], [triuF.b])
            dma("sp", thr[:], thr_d, [], [thr.b])
            dma("sp", iot[:], iota_d, [], [iot.b])
            dma("sp", lnp2[:], lnp_d[:, 2 * D:4 * D], [], [lnp2.b])
            cp("dve", triuB[:], triuF[:], [triuF.b], [triuB.b])
            S.op("dve", lambda e: e.memset(ones32[:], 1.0), [], [ones32.b])
            tt("dve", SELb[:], OH1[:], OH2[:], ALU.add, [OH1.b, OH2.b], [SELb.b])
            for j in range(4):
                pp = PSM.get()
                mm(pp[:], triuB[:], SELb[:, j * 512:(j + 1) * 512], True, True, [triuB.b, SELb.b], [pp.b])
                cp("act", PRE[:, j * 512:(j + 1) * 512], pp[:], [pp.b], [PRE.b])
                pq = PSM.get()
                mm(pq[:], onesB[:], SELb[:, j * 512:(j + 1) * 512], True, True, [onesB.b, SELb.b], [pq.b])
                cp("dve", TOT[:, j * 512:(j + 1) * 512], pq[:], [pq.b], [TOT.b])
            S.op("dve", lambda e: e.memset(BASE[:, 0:32], 0.0), [], [BASE.b])
            for i in range(1, 64):
                tt("dve", BASE[:, i * 32:(i + 1) * 32], BASE[:, (i - 1) * 32:i * 32], TOT[:, (i - 1) * 32:i * 32], ALU.add,
                   [BASE.b, TOT.b], [BASE.b])
            tt("dve", CNT[:], BASE[:, 63 * 32:64 * 32], TOT[:, 63 * 32:64 * 32], ALU.add, [BASE.b, TOT.b], [CNT.b])
            cmpv = CMPB[:, 0:32 * 64].rearrange("p (e m) -> p e m", m=64)
            tt("dve", cmpv, CNT[:].unsqueeze(2).to_broadcast([128, 32, 64]), thr[:, 0:64].unsqueeze(1).to_broadcast([128, 32, 64]),
               ALU.is_gt, [CNT.b, thr.b], [CMPB.b])
            S.op("dve", lambda e: e.tensor_reduce(NBK[:], cmpv, AX.X, ALU.add), [CMPB.b], [NBK.b])
            S.op("dve", lambda e: e.tensor_tensor_scan(PEND[:], ones32[:], NBK[:], 0.0, ALU.mult, ALU.add), [ones32.b, NBK.b], [PEND.b])
            tt("dve", PST[:], PEND[:], NBK[:], ALU.subtract, [PEND.b, NBK.b], [PST.b])
            ts("dve", PST[:], PST[:], float(BLK), None, ALU.mult, None, [PST.b], [PST.b])
            ts("dve", PEND[:], PEND[:], float(BLK), None, ALU.mult, None, [PEND.b], [PEND.b])
            tt("dve", DD[:], PRE[:], BASE[:], ALU.add, [PRE.b, BASE.b], [DD.b])
            ddv = DD[:].rearrange("p (i e) -> p i e", e=32)
            tt("dve", ddv, ddv, PST[:].unsqueeze(1).to_broadcast([128, 64, 32]), ALU.add, [DD.b, PST.b], [DD.b])
            tmpv = PRE[:].rearrange("p (i e) -> p i e", e=32)
            tt("dve", PRE[:], DD[:], OH1[:], ALU.mult, [DD.b, OH1.b], [PRE.b])
            S.op("dve", lambda e: e.tensor_reduce(D1f[:], tmpv, AX.X, ALU.add), [PRE.b], [D1f.b])
            tt("dve", PRE[:], DD[:], OH2[:], ALU.mult, [DD.b, OH2.b], [PRE.b])
            S.op("dve", lambda e: e.tensor_reduce(D2f[:], tmpv, AX.X, ALU.add), [PRE.b], [D2f.b])
            cp("dve", DST1[:], D1f[:], [D1f.b], [DST1.b])
            cp("dve", DST2[:], D2f[:], [D2f.b], [DST2.b])
            cbv = CMPB[:].rearrange("p (j e) -> p j e", e=32)
            tt("dve", cbv, PEND[:].unsqueeze(1).to_broadcast([128, NBLK, 32]),
               thr[:, 64:64 + NBLK].unsqueeze(2).to_broadcast([128, NBLK, 32]), ALU.is_le, [PEND.b, thr.b], [CMPB.b])
            S.op("dve", lambda e: e.tensor_reduce(BLKE[:], cbv, AX.X, ALU.add), [CMPB.b], [BLKE.b])
            ts("dve", BLKE[:], BLKE[:], 31.0, None, ALU.min, None, [BLKE.b], [BLKE.b])
            ts("dve", WIDXf[:], BLKE[:], 128.0, iot[:, 0:1], ALU.mult, ALU.add, [BLKE.b, iot.b], [WIDXf.b])
            S.op("dve", lambda e: e.memset(SAME[:], 0.0), [], [SAME.b])
            tt("dve", SAME[:, NWB:NBLK], BLKE[:, NWB:NBLK], BLKE[:, 0:NBLK - NWB], ALU.is_equal, [BLKE.b], [SAME.b])
            stt("dve", WIDXf[:], SAME[:], 1.0e6, WIDXf[:], ALU.mult, ALU.add, [SAME.b, WIDXf.b], [WIDXf.b])
            cp("dve", WIDX[:], WIDXf[:], [WIDXf.b], [WIDX.b])

            if dbg == 2:
                dma("sp", dbgs_d[:, 0:64], W1[:], [W1.b], [])
                dma("sp", dbgs_d[:, 64:128], W2[:], [W2.b], [])
                dma("sp", dbgs_d[:, 128:192], D1f[:], [D1f.b], [])
                dma("sp", dbgs_d[:, 192:256], D2f[:], [D2f.b], [])
                dma("sp", dbgs_d[:, 256:256 + NBLK], BLKE[:], [BLKE.b], [])
                dma("sp", dbgs_d[:, 416:448], CNT[:], [CNT.b], [])
                dma("sp", dbgs_d[:, 448:480], PST[:], [PST.b], [])
            xrow = [bview(tmp_end + i * 2048, D, "xrow%d" % i) for i in range(4)]
            for i in range(64):
                xr = xrow[i % 4]
                dma("sp", xr[:], u2_scr[i * 128:(i + 1) * 128, :], [], [xr.b])
                for dst in (DST1, DST2):
                    S.dma("pool", (lambda x_, d_, i_: (lambda e: e.indirect_dma_start(
                        out=xs_scr, out_offset=bass.IndirectOffsetOnAxis(ap=d_[:, i_:i_ + 1], axis=0),
                        in_=x_[:], in_offset=None)))(xr, dst, i), [xr.b, dst.b], [])
            S.barrier()

            wo = meta_end
            WB = []
            for i in range(NWB):
                WB.append((bview(wo, 4096, "w1b%d" % i), bview(wo + 8192, 4096, "w3b%d" % i), bview(wo + 16384, 4096, "w2b%d" % i)))
                wo += 24576
            xbk = [bview(wo + i * 2048, D, "xbk%d" % i) for i in range(3)]
            wo += 3 * 2048
            xTk = [bview(wo + i * 2048, D, "xTk%d" % i) for i in range(2)]
            wo += 2 * 2048
            hidk = [bview(wo + i * 1024, 512, "hidk%d" % i) for i in range(2)]
            wo += 2 * 1024
            sak = [fview(wo + i * 2048, 512, "sak%d" % i) for i in range(2)]
            wo += 2 * 2048
            yk = [fview(wo + i * 4096, D, "yk%d" % i) for i in range(2)]
            wo += 2 * 4096
            assert wo <= ARENA_B, wo

            regs = {}

            def load_blk_w(j):
                wb = WB[j % NWB]
                for t_, d_ in zip(wb, wb_scr):
                    def mk(t__, d__, j_):
                        def f(e):
                            if "bc" not in regs:
                                regs["bc"] = e.to_reg(4095)
                            return e.indirect_dma_start(
                                out=t__[:], out_offset=None, in_=d__,
                                in_offset=bass.IndirectOffsetOnAxis(ap=WIDX[:, j_:j_ + 1], axis=0),
                                bounds_check=regs["bc"], oob_is_err=False)
                        return f
                    S.dma("pool", mk(t_, d_, j), [WIDX.b], [t_.b])

            def load_blk_x(j):
                xb_ = xbk[j % 3]
                dma("sp", xb_[:], xs_scr[j * BLK:(j + 1) * BLK, :], [], [xb_.b])

            load_blk_w(0)
            load_blk_x(0)
            load_blk_x(1)
            for j in range(NBLK):
                if j + 1 < NBLK:
                    load_blk_w(j + 1)
                if j + 2 < NBLK:
                    load_blk_x(j + 2)
                w1b, w3b, w2b = WB[j % NWB]
                w1v = w1b[:].rearrange("p (k n) -> p k n", k=8)
                w3v = w3b[:].rearrange("p (k n) -> p k n", k=8)
                w2v = w2b[:].rearrange("p (k n) -> p k n", k=4)
                xb_ = xbk[j % 3]
                xT = xTk[j % 2]
                xTv = xT[:].rearrange("p (k t) -> p k t", k=8)
                pt = PSM.get()
                ptb = pt[:].bitcast(BF16)
                for k in range(8):
                    tr(ptb[:, k * 128:(k + 1) * 128], xb_[:, k * 128:(k + 1) * 128], identB[:], [xb_.b, identB.b], [pt.b])
                cp("act" if j % 2 == 0 else "dve", xT[:], ptb[:, 0:1024], [pt.b], [xT.b])
                pa = PSM.get()
                pb_ = PSM.get()
                for ht in range(4):
                    for k in range(8):
                        mm(pa[:, ht * 128:(ht + 1) * 128], w1v[:, k, ht * 128:(ht + 1) * 128], xTv[:, k, :], k == 0, k == 7,
                           [w1b.b, xT.b], [pa.b])
                    for k in range(8):
                        mm(pb_[:, ht * 128:(ht + 1) * 128], w3v[:, k, ht * 128:(ht + 1) * 128], xTv[:, k, :], k == 0, k == 7,
                           [w3b.b, xT.b], [pb_.b])
                sa = sak[j % 2]
                act(sa[:], pa[:], AF.Silu, [pa.b], [sa.b])
                hid = hidk[j % 2]
                tt("dve", hid[:], sa[:], pb_[:], ALU.mult, [sa.b, pb_.b], [hid.b])
                hv = hid[:].rearrange("p (k t) -> p k t", k=4)
                yy = yk[j % 2]
                for hlf in range(2):
                    py = PSM.get()
                    for ht in range(4):
                        mm(py[:], hv[:, ht, :], w2v[:, ht, hlf * 512:(hlf + 1) * 512], ht == 0, ht == 3, [hid.b, w2b.b], [py.b])
                    if hlf == 0:
                        cp("act", yy[:, 0:512], py[:], [py.b], [yy.b])
                    else:
                        cp("dve", yy[:, 512:1024], py[:], [py.b], [yy.b])
                dma("sp", ys_scr[j * BLK:(j + 1) * BLK, :], yy[:], [yy.b], [])
            S.barrier()

            fo = meta_end
            NF = 4
            y1k = [fview(fo + i * 4096, D, "y1k%d" % i) for i in range(NF)]
            y2k = [fview(fo + NF * 4096 + i * 4096, D, "y2k%d" % i) for i in range(NF)]
            h1k = [fview(fo + 2 * NF * 4096 + i * 4096, D, "h1k%d" % i) for i in range(NF)]
            assert fo + 3 * NF * 4096 <= ARENA_B
            g2bs = [g2b, fview(fo + 3 * NF * 4096, D, "g2b1")]
            assert fo + 3 * NF * 4096 + 4096 <= ARENA_B
            for bb in range(2):
                dma("sp", g2bs[bb][:], m_scr[bb:bb + 1, 5120:6144].partition_broadcast(128), [], [g2bs[bb].b])

            def fin_loads(i):
                for yt, dst in ((y1k[i % NF], DST1), (y2k[i % NF], DST2)):
                    S.dma("pool", (lambda y_, d_, i_: (lambda e: e.indirect_dma_start(
                        out=y_[:], out_offset=None, in_=ys_scr,
                        in_offset=bass.IndirectOffsetOnAxis(ap=d_[:, i_:i_ + 1], axis=0))))(yt, dst, i), [dst.b], [yt.b])
                dma("sp", h1k[i % NF][:], h1_scr[i * 128:(i + 1) * 128, :], [], [h1k[i % NF].b])

            for i in range(NF - 1):
                fin_loads(i)
            for i in range(64):
                bb = i // 32
                if i + NF - 1 < 64:
                    fin_loads(i + NF - 1)
                y1 = y1k[i % NF]
                y2 = y2k[i % NF]
                hh = h1k[i % NF]
                act(y1[:], y1[:], AF.Copy, [y1.b, W1.b], [y1.b], scale=W1[:, i:i + 1])
                stt("dve", y1[:], y2[:], W2[:, i:i + 1], y1[:], ALU.mult, ALU.add, [y2.b, W2.b, y1.b], [y1.b])
                if dbg == 2:
                    dma("sp", dbg_d[i * 128:(i + 1) * 128, :], y1[:], [y1.b], [])
                tt("dve", y1[:], y1[:], g2bs[bb][:], ALU.mult, [y1.b, g2bs[bb].b], [y1.b])
                stt("dve", hh[:], hh[:], ALPHA, y1[:], ALU.mult, ALU.add, [hh.b, y1.b], [hh.b])
                mv = ln_stats(S, hh, stats, small, act)
                act(hh[:], hh[:], AF.Identity, [hh.b, mv.b], [hh.b], bias=mv[:, 3:4], scale=mv[:, 2:3])
                tt("dve", hh[:], hh[:], lnp2[:, 0:D], ALU.mult, [hh.b, lnp2.b], [hh.b])
                tt("dve", hh[:], hh[:], lnp2[:, D:2 * D], ALU.add, [hh.b, lnp2.b], [hh.b])
                dma("sp", out_d[bb, (i % 32) * 128:(i % 32 + 1) * 128, :], hh[:], [hh.b], [])

        if dbg != 1:
            moe_phase()
        S.finish("sp")
        S.emit()
    return nc


def ln_stats(S, z, stats, small, act):
    st = stats.get()
    for hlf in range(2):
        S.op("dve", (lambda o, i_: (lambda e: e.bn_stats(o, i_)))(st[:, hlf * 6:(hlf + 1) * 6], z[:, hlf * 512:(hlf + 1) * 512]),
             [z.b], [st.b])
    mv = small.get()
    S.op("dve", (lambda o, i_: (lambda e: e.bn_aggr(o, i_)))(mv[:, 0:2], st[:, 0:12]), [st.b], [mv.b])
    act(mv[:, 2:3], mv[:, 1:2], AF.Sqrt, [mv.b], [mv.b], bias=EPS, scale=1.0)
    S.op("dve", (lambda o, i_: (lambda e: e.reciprocal(o, i_)))(mv[:, 2:3], mv[:, 2:3]), [mv.b], [mv.b])
    S.op("dve", (lambda o, a_, b_: (lambda e: e.scalar_tensor_tensor(o, a_, -1.0, b_, ALU.mult, ALU.mult)))(
        mv[:, 3:4], mv[:, 0:1], mv[:, 2:3]), [mv.b], [mv.b])
    return mv


def _host_consts():
    identF = np.eye(128, dtype=np.float32)
    s = np.arange(128)[:, None]
    t = np.arange(128)[None, :]
    same = (s // 64) == (t // 64)
    maskF = (same & (s <= t)).astype(np.float32)
    maskB = (same & (s >= t)).astype(np.float32)
    resetm = np.ones((128, 512), np.float32)
    resetm[:, ::64] = 0.0
    triu = (s < t).astype(np.float32)
    thr = np.zeros((128, 64 + NBLK), np.float32)
    thr[:, :64] = (np.arange(64) * BLK)[None, :]
    thr[:, 64:] = (np.arange(NBLK) * BLK)[None, :]
    iota = np.zeros((128, 2), np.float32)
    iota[:, 0] = np.arange(128)
    return dict(identF=identF, maskF=maskF, maskB=maskB, resetm=resetm, triu=triu, thr=thr, iota=iota)


def _prep_shared(inp):
    f = lambda a: np.ascontiguousarray(np.asarray(a, dtype=np.float32))
    sh = {}
    sh["ada_w"] = f(inp["ada_w"][0])
    sh["ada_b"] = f(inp["ada_b"][0]).reshape(1, -1)
    sh["w_in"] = f(inp["w_in"][0])
    cw = f(inp["rg_conv_w"][0])
    sh["convw"] = f(cw.reshape(4, 4, 128).transpose(2, 1, 0).reshape(128, 16))
    sh["convb"] = f(f(inp["rg_conv_b"][0]).reshape(4, 128).T)
    gwt = f(inp["rg_gate_w"][0])
    gw = np.zeros((128, 16, 128), np.float32)
    for d_ in range(2):
        for g_ in range(2):
            for c in range(4):
                for h_ in range(2):
                    gw[h_ * 64:(h_ + 1) * 64, d_ * 8 + g_ * 4 + c, h_ * 64:(h_ + 1) * 64] = gwt[d_, g_, c * 2 + h_]
    sh["gw"] = gw.reshape(128, 2048)
    gbt = f(inp["rg_gate_b"][0])
    sh["gb"] = f(gbt.reshape(2, 2, 4, 128).transpose(3, 0, 1, 2).reshape(128, 16))
    sh["lam"] = f(f(inp["rg_lambda"][0]).reshape(2, 4, 128).transpose(2, 0, 1).reshape(128, 8))
    sh["lbl"] = f(f(inp["hg_lb_logits"]).reshape(2, 2, 4, 128).transpose(3, 0, 1, 2).reshape(128, 16))
    sh["ng"] = f(f(inp["hg_norm_g"][0]).reshape(4, 128).T)
    sh["w_out"] = f(inp["w_out"][0])
    lnp = np.concatenate([f(inp["ln1_g"][0]), f(inp["ln1_b"][0]), f(inp["ln2_g"][0]), f(inp["ln2_b"][0])])
    sh["lnp"] = f(np.broadcast_to(lnp[None, :], (128, 4 * D)))
    sh["rw"] = f(np.concatenate([f(inp["router_g_w"][0]), f(inp["router_e_w"][0])], axis=1))
    rb = np.concatenate([f(inp["router_g_b"][0]), f(inp["router_e_b"][0])])
    sh["rb"] = f(np.broadcast_to(rb[None, :], (128, 36)))
    sh["w1r"] = f(f(inp["exp_w1"][0]).reshape(32, 8, 128, 512).transpose(0, 2, 1, 3).reshape(32 * 128, 8 * 512))
    sh["w3r"] = f(f(inp["exp_w3"][0]).reshape(32, 8, 128, 512).transpose(0, 2, 1, 3).reshape(32 * 128, 8 * 512))
    sh["w2r"] = f(f(inp["exp_w2"][0]).reshape(32, 4, 128, 1024).transpose(0, 2, 1, 3).reshape(32 * 128, 4 * 1024))
    sh.update(_host_consts())
    return sh


def _in_maps(inp):
    sh = _prep_shared(inp)
    x = np.asarray(inp["x"], np.float32)
    c = np.asarray(inp["c"], np.float32)
    ctx = np.asarray(inp["ctx"], np.float32)
    c_ctx = np.asarray(inp["c_ctx"], np.float32)
    maps = []
    for k in range(NCORES):
        m = dict(sh)
        m["x"] = np.ascontiguousarray(x[2 * k:2 * k + 2])
        m["ctx"] = np.ascontiguousarray(ctx[2 * k:2 * k + 2])
        cv = np.stack([c[2 * k], c[2 * k + 1], c_ctx], axis=0)
        m["cT"] = np.ascontiguousarray(cv.reshape(3, 8, 128).transpose(2, 1, 0).reshape(128, 24))
        maps.append(m)
    return maps


_NC_CACHE = {}


def kernel(**inputs):
    if "nc" not in _NC_CACHE:
        _NC_CACHE["nc"] = build_program(0)
    nc = _NC_CACHE["nc"]
    maps = _in_maps(inputs)
    res = run_bass_kernel_spmd(nc, maps, core_ids=list(range(NCORES)))
    out = np.concatenate([r["out"] for r in res.results], axis=0)
    return out.astype(np.float32)
```

```python
import os
import numpy as np
from contextlib import ExitStack
import ml_dtypes
import concourse.bass as bass
import concourse.mybir as mybir
from concourse.bass_utils import run_bass_kernel_spmd

F32 = mybir.dt.float32
BF16 = mybir.dt.bfloat16
I32 = mybir.dt.int32
AF = mybir.ActivationFunctionType
ALU = mybir.AluOpType
AX = mybir.AxisListType

SAME_ENG_SYNC = True
NCORES = 8
L = 4096
LC = 256
D = 1024
ALPHA = 2.0 ** 0.25
EPS = 1e-6
BLK = 128
NBLK = 160
NSLOT = NBLK * BLK
NWB = 2


class Buf:
    __slots__ = ("name", "w", "r", "excl")

    def __init__(self, name=""):
        self.name = name
        self.w = None
        self.r = {}
        self.excl = False


class Sched:
    ENGS = ("pe", "act", "dve", "pool", "sp")

    def __init__(self, nc, es, n_dma_sems=40):
        self.nc = nc
        self.q = {e: [] for e in self.ENGS}
        self.cnt = {e: 0 for e in self.ENGS}
        self.sem = {e: es.enter_context(nc.semaphore("s_" + e)) for e in ("pe", "act", "dve", "pool")}
        n_sw = 24
        self.dsem = [es.enter_context(nc.semaphore("d%d" % i)) for i in range(n_dma_sems + n_sw)]
        self.dcnt = [0] * (n_dma_sems + n_sw)
        self.n_hw = n_dma_sems
        self.n_sw = n_sw
        self.dnext = {"hw": 0, "sw": 0}
        self.seen = {e: {} for e in self.ENGS}

    def _deps(self, reads, writes):
        toks = []
        for b in reads:
            if b.w is not None:
                toks.append(b.w)
        for b in writes:
            if b.w is not None:
                toks.append(b.w)
            toks.extend(b.r.values())
        return toks

    def _mark(self, reads, writes, tok):
        for b in writes:
            b.w = tok
            b.r = {}
        for b in reads:
            b.r[id(tok[1])] = tok

    def _push(self, eng, fn, toks, inc):
        need = {}
        for kind, sem, val in toks:
            if kind == eng and (eng == "pe" or not SAME_ENG_SYNC):
                continue
            k = id(sem)
            if k not in need or need[k][1] < val:
                need[k] = (sem, val)
        waits = []
        seen = self.seen[eng]
        for k, (sem, val) in need.items():
            if seen.get(k, 0) >= val:
                continue
            seen[k] = val
            waits.append((sem, val))
        self.q[eng].append((fn, waits, inc))

    @staticmethod
    def _excl(reads, writes):
        ex = [b for b in reads if b.excl]
        if ex:
            reads = [b for b in reads if not b.excl]
            writes = list(writes) + [b for b in ex if b not in writes]
        return reads, writes

    def op(self, eng, fn, reads=(), writes=()):
        reads, writes = self._excl(reads, writes)
        toks = self._deps(reads, writes)
        self.cnt[eng] += 1
        tok = (eng, self.sem[eng], self.cnt[eng])
        self._push(eng, fn, toks, (self.sem[eng], 1))
        self._mark(reads, writes, tok)
        return tok

    def dma(self, eng, fn, reads=(), writes=()):
        toks = self._deps(reads, writes)
        if eng == "pool":
            i = self.n_hw + self.dnext["sw"]
            self.dnext["sw"] = (self.dnext["sw"] + 1) % self.n_sw
        else:
            i = self.dnext["hw"]
            self.dnext["hw"] = (self.dnext["hw"] + 1) % self.n_hw
        if self.dcnt[i] > 0:
            toks.append(("dma", self.dsem[i], self.dcnt[i]))
        self.dcnt[i] += 16
        tok = ("dma", self.dsem[i], self.dcnt[i])
        self._push(eng, fn, toks, (self.dsem[i], 16))
        self._mark(reads, writes, tok)
        return tok

    def barrier(self):
        toks = [(e, self.sem[e], self.cnt[e]) for e in ("pe", "act", "dve", "pool") if self.cnt[e] > 0]
        toks += [("dma", self.dsem[i], self.dcnt[i]) for i in range(len(self.dsem)) if self.dcnt[i] > 0]
        for e in self.ENGS:
            need = []
            for kind, sem, val in toks:
                if kind == e:
                    continue
                if self.seen[e].get(id(sem), 0) >= val:
                    continue
                self.seen[e][id(sem)] = val
                need.append((sem, val))
            self.q[e].append((None, need, None))

    def finish(self, eng="sp"):
        toks = [("dma", self.dsem[i], self.dcnt[i]) for i in range(len(self.dsem)) if self.dcnt[i] > 0]
        self.seen[eng] = {}
        self._push(eng, None, toks, None)

    def emit(self):
        nc = self.nc
        with nc.Block() as block:
            def mk(e):
                def body(engobj):
                    for fn, waits, inc in self.q[e]:
                        for sem, val in waits:
                            engobj.wait_ge(sem, val)
                        if fn is not None:
                            ins = fn(engobj)
                            ins.then_inc(inc[0], inc[1])
                return body
            block.tensor(mk("pe"))
            block.scalar(mk("act"))
            block.vector(mk("dve"))
            block.gpsimd(mk("pool"))
            block.sync(mk("sp"))


class T:
    def __init__(self, nc, es, name, shape, dtype, psum=False, nbuf=0):
        if psum:
            self.t = es.enter_context(nc.psum_tensor("p_" + name, shape, dtype))
        else:
            self.t = es.enter_context(nc.sbuf_tensor("t_" + name, shape, dtype))
        self.b = Buf(name)
        self.b.excl = psum
        self.bs = [Buf(name + "_" + str(i)) for i in range(nbuf)]

    def __getitem__(self, k):
        return self.t[k]


class V:
    def __init__(self, ap, name, nbuf=0):
        self.ap = ap
        self.b = Buf(name)
        self.bs = [Buf(name + "_" + str(i)) for i in range(nbuf)]

    def __getitem__(self, k):
        return self.ap[k]


class RV:
    def __init__(self, items):
        self.tiles = items
        self.i = 0

    def get(self):
        t = self.tiles[self.i % len(self.tiles)]
        self.i += 1
        return t


class Rot:
    def __init__(self, nc, es, name, shape, dtype, n, psum=False):
        self.tiles = [T(nc, es, name + str(i), shape, dtype, psum=psum) for i in range(n)]
        self.i = 0

    def get(self):
        t = self.tiles[self.i % len(self.tiles)]
        self.i += 1
        return t


def build_program(dbg=0):
    nc = bass.Bass("TRN2", target_bir_lowering=False)

    def din(name, shape, dt=F32):
        return nc.dram_tensor(name, shape, dt, kind="ExternalInput").ap()

    def dscr(name, shape, dt=F32):
        return nc.dram_tensor(name, shape, dt, kind="Internal").ap()

    x_d = din("x", [2, L, D])
    ctx_d = din("ctx", [2, LC, D])
    cT_d = din("cT", [128, 24])
    adaw_d = din("ada_w", [D, 6 * D])
    adab_d = din("ada_b", [1, 6 * D])
    win_d = din("w_in", [D, 3584])
    convw_d = din("convw", [128, 16])
    convb_d = din("convb", [128, 4])
    gw_d = din("gw", [128, 2048])
    gb_d = din("gb", [128, 16])
    lam_d = din("lam", [128, 8])
    lbl_d = din("lbl", [128, 16])
    ng_d = din("ng", [128, 4])
    wout_d = din("w_out", [D, D])
    lnp_d = din("lnp", [128, 4 * D])
    identF_d = din("identF", [128, 128])
    maskF_d = din("maskF", [128, 128])
    maskB_d = din("maskB", [128, 128])
    resetm_d = din("resetm", [128, 512])
    rw_d = din("rw", [D, 36])
    rb_d = din("rb", [128, 36])
    w1_d = din("w1r", [32 * 128, 8 * 512])
    w3_d = din("w3r", [32 * 128, 8 * 512])
    w2_d = din("w2r", [32 * 128, 4 * 1024])
    triu_d = din("triu", [128, 128])
    thr_d = din("thr", [128, 64 + NBLK])
    iota_d = din("iota", [128, 2])
    out_d = nc.dram_tensor("out", [2, L, D], F32, kind="ExternalOutput").ap()
    if dbg:
        dbg_d = nc.dram_tensor("dbg", [2 * L, D], F32, kind="ExternalOutput").ap()
        dbgs_d = nc.dram_tensor("dbgs", [128, 512], F32, kind="ExternalOutput").ap()

    m_scr = dscr("m_scr", [3, 6 * D])
    mix_scr = dscr("mix_scr", [2, 8, 128, L], BF16)
    h1_scr = dscr("h1_scr", [2 * L, D])
    u2_scr = dscr("u2_scr", [2 * L, D], BF16)
    xs_scr = dscr("xs_scr", [NSLOT, D], BF16)
    ys_scr = dscr("ys_scr", [NSLOT, D])
    zsrc = dscr("zsrc", [32, D], BF16)
    wb_scr = [dscr("w%db_scr" % i, [32 * 128, 4096], BF16) for i in range(3)]

    with ExitStack() as es:
        S = Sched(nc, es)

        def TT(name, shape, dt=F32, **kw):
            return T(nc, es, name, shape, dt, **kw)

        def RR(name, shape, dt, n, **kw):
            return Rot(nc, es, name, shape, dt, n, **kw)

        def mm(out, lhsT, rhs, start, stop, reads, writes, sgc=False):
            S.op("pe", lambda e: e.matmul(out, lhsT, rhs, start=start, stop=stop, skip_group_check=sgc), reads, writes)

        def tr(out, in_, ident, reads, writes):
            S.op("pe", lambda e: e.transpose(out, in_, ident), reads, writes)

        def act(out, in_, func, reads, writes, bias=None, scale=None):
            kw = {}
            if bias is not None:
                kw["bias"] = bias
            if scale is not None:
                kw["scale"] = scale
            S.op("act", lambda e: e.activation(out, in_, func, **kw), reads, writes)

        POOL2DVE = os.environ.get("K_POOL2DVE", "1") == "1"

        def tt(eng, out, in0, in1, op, reads, writes):
            if eng == "pool" and POOL2DVE:
                eng = "dve"
            S.op(eng, lambda e: e.tensor_tensor(out, in0, in1, op), reads, writes)

        def ts(eng, out, in0, s1, s2, op0, op1, reads, writes):
            if s2 is None:
                S.op(eng, lambda e: e.tensor_scalar(out, in0, s1, None, op0), reads, writes)
            else:
                S.op(eng, lambda e: e.tensor_scalar(out, in0, s1, s2, op0, op1), reads, writes)

        def stt(eng, out, in0, sc, in1, op0, op1, reads, writes):
            S.op(eng, lambda e: e.scalar_tensor_tensor(out, in0, sc, in1, op0, op1), reads, writes)

        def cp(eng, out, in_, reads, writes):
            if eng == "act":
                S.op("act", lambda e: e.copy(out, in_), reads, writes)
            else:
                S.op(eng, lambda e: e.tensor_copy(out, in_), reads, writes)

        def dma(eng, out, in_, reads, writes, **kw):
            S.dma(eng, lambda e: e.dma_start(out=out, in_=in_, **kw), reads, writes)

        def zip_run(gens):
            res = [None] * len(gens)
            live = list(range(len(gens)))
            while live:
                for gi in list(live):
                    try:
                        next(gens[gi])
                    except StopIteration as e_:
                        res[gi] = e_.value
                        live.remove(gi)
            return res

        identF = TT("identF", [128, 128])
        identB = TT("identB", [128, 128], BF16)
        onesF = TT("onesF", [128, 128])
        onesB = TT("onesB", [128, 128], BF16)
        resetm = TT("resetm", [128, 512])
        cT = TT("cT", [128, 24])
        scT = TT("scT", [128, 24])
        convw = TT("convw", [128, 16])
        convb = TT("convb", [128, 4])
        gb = TT("gb", [128, 16])
        lam = TT("lam", [128, 8])
        cL = TT("cL", [128, 8])
        lbl = TT("lbl", [128, 16])
        lb = TT("lb", [128, 8])
        oml = TT("oml", [128, 8])
        noml = TT("noml", [128, 8])
        ng = TT("ng", [128, 4])
        gwb = TT("gwb", [128, 2048], BF16)
        modT = TT("modT", [128, 48])
        ones3 = TT("ones3", [1, 4])

        for t_, d_ in ((identF, identF_d), (resetm, resetm_d), (cT, cT_d),
                       (convw, convw_d), (convb, convb_d), (gb, gb_d), (lam, lam_d), (lbl, lbl_d), (ng, ng_d)):
            dma("sp", t_[:], d_, [], [t_.b])
        cp("dve", identB[:], identF[:], [identF.b], [identB.b])
        S.op("dve", lambda e: e.memset(onesF[:], 1.0), [], [onesF.b])
        S.op("dve", lambda e: e.memset(onesB[:], 1.0), [], [onesB.b])
        S.op("dve", lambda e: e.memset(ones3[:], 1.0), [], [ones3.b])
        act(scT[:], cT[:], AF.Silu, [cT.b], [scT.b])
        act(cL[:], lam[:], AF.Exp, [lam.b], [cL.b], scale=-1.0)
        act(cL[:], cL[:], AF.Ln, [cL.b], [cL.b], bias=1.0)
        ts("dve", cL[:], cL[:], -8.0, None, ALU.mult, None, [cL.b], [cL.b])
        tt("dve", lb[:], lbl[:, 0:8], lbl[:, 8:16], ALU.subtract, [lbl.b], [lb.b])
        act(lb[:], lb[:], AF.Sigmoid, [lb.b], [lb.b])
        ts("dve", oml[:], lb[:], -1.0, 1.0, ALU.mult, ALU.add, [lb.b], [oml.b])
        ts("dve", noml[:], oml[:], -1.0, None, ALU.mult, None, [oml.b], [noml.b])

        ARENA_B = 100 * 1024
        BIG = es.enter_context(nc.sbuf_tensor("big", [128, ARENA_B // 4], F32))

        def fview(off, n, name, nbuf=0):
            return V(BIG[:, off // 4:off // 4 + n], name, nbuf)

        def bview(off, n, name, nbuf=0):
            return V(BIG[:, off // 4:off // 4 + n // 2].bitcast(BF16), name, nbuf)

        K64 = 65536
        uT = bview(0, 8 * L, "uT", 32)
        uTv = uT[:].rearrange("p (k t) -> p k t", k=8)
        cuT = bview(K64, 8 * LC, "cuT", 2)
        cuTv = cuT[:].rearrange("p (k t) -> p k t", k=8)
        LOC = K64 + 4096
        WKa = fview(LOC, 4096, "wka", 8)
        WKb = fview(LOC + 16384, 4096, "wkb", 8)
        PS = RR("ps", [128, 512], F32, int(os.environ.get("K_PS", "5")), psum=True)
        POR = RR("po", [128, 512], F32, 7 - int(os.environ.get("K_PS", "5")), psum=True)
        PL = TT("pl", [128, 512], F32, psum=True)

        dma("sp", WKa[:, 0:2048], gw_d, [], [WKa.b])
        cp("pool", gwb[:], WKa[:, 0:2048], [WKa.b], [gwb.b])

        tmpf_raw = es.enter_context(nc.sbuf_tensor("t_tmpf_raw", [128, 14 * 512], F32))
        tmpf = RV([V(tmpf_raw[:, i * 512:(i + 1) * 512], "tmpf%d" % i) for i in range(14)])
        tmph = RV([V(tmpf_raw[:, i * 256:(i + 1) * 256], "tmph%d" % i) for i in range(28)])
        adw = [fview(0, 4096, "adw0"), fview(16384, 4096, "adw1")]
        for j in range(12):
            wt = adw[j % 2]
            wv = wt[:].rearrange("p (k n) -> p k n", k=8)
            dma("sp", wv, adaw_d[:, j * 512:(j + 1) * 512].rearrange("(k p) n -> p k n", p=128), [], [wt.b])
            bt = tmpf.get()
            dma("sp", bt[0:1, :], adab_d[0:1, j * 512:(j + 1) * 512], [], [bt.b])
            ps = PS.get()
            for k in range(8):
                mm(ps[0:3, :], scT[:, k * 3:(k + 1) * 3], wv[:, k, :], k == 0, False, [scT.b, wt.b], [ps.b])
            mm(ps[0:3, :], ones3[0:1, 0:3], bt[0:1, :], False, True, [ones3.b, bt.b], [ps.b])
            mt = tmpf.get()
            cp("dve", mt[0:3, :], ps[0:3, :], [ps.b], [mt.b])
            dma("sp", m_scr[:, j * 512:(j + 1) * 512], mt[0:3, :], [mt.b], [])
            if j < 4:
                for q_ in range(4):
                    jj = j * 4 + q_
                    tr(PL[:, jj * 3:jj * 3 + 3], mt[0:3, q_ * 128:(q_ + 1) * 128], identF[0:3, 0:3], [mt.b, identF.b], [PL.b])
                if j == 3:
                    cp("dve", modT[:], PL[:, 0:48], [PL.b], [modT.b])
        S.barrier()

        def mscr_reads():
            return []

        ts("dve", modT[:, 24:48], modT[:, 24:48], 1.0, None, ALU.add, None, [modT.b], [modT.b])

        xin = RR("xin", [128, D], F32, 2)
        wbf = RR("wbf", [128, 8 * 128], BF16, 10)
        tmpb_raw = es.enter_context(nc.sbuf_tensor("t_tmpb_raw", [128, 10 * 512], BF16))
        tmpb = RV([V(tmpb_raw[:, i * 512:(i + 1) * 512], "tmpb%d" % i) for i in range(10)])
        tmphb = RV([V(tmpb_raw[:, i * 256:(i + 1) * 256], "tmphb%d" % i) for i in range(20)])
        small = RR("small", [128, 16], F32, 8)
        Sfin = [TT("Sfin%d" % i, [128, 128]) for i in range(2)]
        SstR = RR("Sst", [128, 9 * 128], F32, 1)
        SallR = RR("Sall", [128, 8 * 128], BF16, 1)
        UsbR = RR("Usb", [128, 8 * 128], F32, 2)
        mask4F = TT("mask4F", [128, 512])
        mask4B = TT("mask4B", [128, 512])
        for q_ in range(4):
            dma("sp", mask4F[:, q_ * 128:(q_ + 1) * 128], maskF_d, [], [mask4F.b])
            dma("sp", mask4B[:, q_ * 128:(q_ + 1) * 128], maskB_d, [], [mask4B.b])
        h0 = TT("h0", [128, 2])
        Vc = TT("Vc", [128, 2 * 128], BF16, nbuf=2)
        stats = RR("stats", [128, 12], F32, 2)

        def load_w(col):
            wb = wbf.get()
            wbv = wb[:].rearrange("p (k n) -> p k n", k=8)
            src = win_d[:, col:col + 128].rearrange("(k p) n -> p k n", p=128)
            S.dma("pool", (lambda o_, i_: (lambda e: e.dma_start(out=o_, in_=i_)))(wbv, src), [], [wb.b])
            return wb

        wq_pending = {}
        pc_state = [0]
        w_f32 = (w1_d, w3_d, w2_d)

        def precast(n_):
            for _ in range(n_):
                k_ = pc_state[0]
                if k_ >= 96:
                    return
                pc_state[0] += 1
                e_, m_ = k_ // 3, k_ % 3
                src = w_f32[m_][e_ * 128:(e_ + 1) * 128, :].rearrange("p (a n) -> p a n", n=2048)
                dst = wb_scr[m_][e_ * 128:(e_ + 1) * 128, :].rearrange("p (a n) -> p a n", n=2048)
                S.dma("pool", (lambda o_, i_: (lambda e: e.dma_start(out=o_, in_=i_)))(dst, src), [], [])

        def rg_cols(c):
            return (c * 128, 512 + c * 128)

        def hg_cols(hd):
            return (1024 + hd * 128, 1536 + hd * 128, 2048 + hd * 128, 2560 + hd * 128, 3072 + hd * 128)

        def prefetch(key, cols):
            wq_pending[key] = [load_w(c_) for c_ in cols]

        def take(key, cols):
            if key not in wq_pending:
                prefetch(key, cols)
            return wq_pending.pop(key)

        def proj_fm(wb, src_v, src_bufs, t0, n, ps):
            wv = wb[:].rearrange("p (k n) -> p k n", k=8)
            for k in range(8):
                mm(ps[:, 0:n], wv[:, k, :], src_v[:, k, t0:t0 + n], k == 0, k == 7, [wb.b] + src_bufs, [ps.b])

        def proj_tm(wb, src_v, src_bufs, t0, ps, c0):
            wv = wb[:].rearrange("p (k n) -> p k n", k=8)
            for k in range(8):
                mm(ps[:, c0:c0 + 128], src_v[:, k, t0:t0 + 128], wv[:, k, :], k == 0, k == 7, [wb.b] + src_bufs, [ps.b])

        OH1 = TT("OH1", [128, 64 * 32], BF16)
        OH2 = TT("OH2", [128, 64 * 32], BF16)
        OH1v = OH1[:].rearrange("p (i e) -> p i e", e=32)
        OH2v = OH2[:].rearrange("p (i e) -> p i e", e=32)
        W1 = TT("W1", [128, 64])
        W2 = TT("W2", [128, 64])
        rw = TT("rw", [128, 8 * 36])
        rwv = rw[:].rearrange("p (k n) -> p k n", k=8)
        dma("sp", rwv, rw_d.rearrange("(k p) n -> p k n", p=128), [], [rw.b])
        rb4 = TT("rb4", [128, 4 * 36])
        for q_ in range(4):
            dma("sp", rb4[:, q_ * 36:(q_ + 1) * 36], rb_d, [], [rb4.b])
        rt = RV([V(tmpf_raw[:, i * 160:(i + 1) * 160], "rt%d" % i) for i in range(8)])
        zrow = TT("zrow", [128, D], BF16)
        S.op("pool", lambda e: e.memset(zrow[:], 0.0), [], [zrow.b])
        zsrc_b = Buf("zsrc")
        dma("sp", zsrc, zrow[0:32, :], [zrow.b], [zsrc_b])
        xs16 = xs_scr.rearrange("(q r j) d -> q r (j d)", q=16, j=32)
        zflat = zsrc.rearrange("(o j) d -> o (j d)", o=1)
        for q_ in range(16):
            dma("sp", xs16[q_], zflat.to_broadcast([NSLOT // (16 * 32), 32 * D]), [zsrc_b], [])

        if os.environ.get('K_VERBOSE'):
            print('SBUF bytes remaining', nc.sbuf_bytes_remaining)
        for b in range(int(os.environ.get('K_NB', '2'))):
            S.barrier()
            def phaseA(src_d, ntile, dst_v, dst_t, r):
                for i in range(ntile):
                    xt = xin.get()
                    dma("sp", xt[:], src_d[i * 128:(i + 1) * 128, :], [], [xt.b])
                    for hlf in range(2):
                        ps = PS.get()
                        for kk in range(4):
                            k = hlf * 4 + kk
                            tr(ps[:, kk * 128:(kk + 1) * 128], xt[:, k * 128:(k + 1) * 128], identF[:], [xt.b, identF.b], [ps.b])
                        for kk in range(4):
                            k = hlf * 4 + kk
                            o_ = dst_v[:, k, i * 128:(i + 1) * 128]
                            i_ = ps[:, kk * 128:(kk + 1) * 128]
                            sc_ = modT[:, (8 + k) * 3 + r:(8 + k) * 3 + r + 1]
                            bi_ = modT[:, k * 3 + r:k * 3 + r + 1]
                            if hlf == 0:
                                act(o_, i_, AF.Identity, [ps.b, modT.b], [dst_t.bs[i]], bias=bi_, scale=sc_)
                            else:
                                ts("dve", o_, i_, sc_, bi_, ALU.mult, ALU.add, [ps.b, modT.b], [dst_t.bs[i]])

            phaseA(ctx_d[b], 2, cuTv, cuT, 2)
            phaseA(x_d[b], 32, uTv, uT, b)

            xc = WKa
            hf = WKb

            def rg_gates(c, dirn, xcb, n, xc_ap, xc_bufs):
                gi = dirn * 8
                pr = PS.get()
                pi = PS.get()
                mm(pr[:, 0:n], gwb[:, (gi + c) * 128:(gi + c + 1) * 128], xcb[:, 0:n], True, True, [gwb.b, xcb.b], [pr.b])
                mm(pi[:, 0:n], gwb[:, (gi + 4 + c) * 128:(gi + 4 + c + 1) * 128], xcb[:, 0:n], True, True, [gwb.b, xcb.b], [pi.b])
                r_ = tmpf.get()
                i_ = tmpf.get()
                act(r_[:, 0:n], pr[:, 0:n], AF.Sigmoid, [pr.b, gb.b], [r_.b], bias=gb[:, gi + c:gi + c + 1])
                act(i_[:, 0:n], pi[:, 0:n], AF.Sigmoid, [pi.b, gb.b], [i_.b], bias=gb[:, gi + 4 + c:gi + 4 + c + 1])
                a_ = tmpf.get()
                act(a_[:, 0:n], r_[:, 0:n], AF.Exp, [r_.b, cL.b], [a_.b], scale=cL[:, dirn * 4 + c:dirn * 4 + c + 1])
                a2 = tmpf.get()
                tt("pool", a2[:, 0:n], a_[:, 0:n], a_[:, 0:n], ALU.mult, [a_.b], [a2.b])
                act(a2[:, 0:n], a2[:, 0:n], AF.Sqrt, [a2.b], [a2.b], bias=1.0, scale=-1.0)
                tt("pool", i_[:, 0:n], i_[:, 0:n], xc_ap, ALU.mult, [i_.b] + xc_bufs, [i_.b])
                tt("dve", i_[:, 0:n], i_[:, 0:n], a2[:, 0:n], ALU.mult, [i_.b, a2.b], [i_.b])
                return a_, i_

            def conv(c, src, n, rows, dst_ap, dst_bufs):
                w_ = n // rows
                sv = src[:, 0:n].rearrange("p (r w) -> p r w", r=rows)
                dv = dst_ap.rearrange("p (r w) -> p r w", r=rows)
                wc = lambda j: convw[:, c * 4 + j:c * 4 + j + 1]
                rd = [src.b, convw.b, convb.b]
                ts("dve", dst_ap, src[:, 0:n], wc(1), convb[:, c:c + 1], ALU.mult, ALU.add, rd, dst_bufs)
                stt("dve", dv[:, :, 1:w_], sv[:, :, 0:w_ - 1], wc(0), dv[:, :, 1:w_], ALU.mult, ALU.add, rd, dst_bufs)
                stt("dve", dv[:, :, 0:w_ - 1], sv[:, :, 1:w_], wc(2), dv[:, :, 0:w_ - 1], ALU.mult, ALU.add, rd, dst_bufs)
                stt("dve", dv[:, :, 0:w_ - 2], sv[:, :, 2:w_], wc(3), dv[:, :, 0:w_ - 2], ALU.mult, ALU.add, rd, dst_bufs)

            for c in range(int(os.environ.get('K_NRG', '4'))):
                w_x, w_g = take(("rg", b, c), rg_cols(c))
                precast(6)
                if c + 1 < 4:
                    prefetch(("rg", b, c + 1), rg_cols(c + 1))
                else:
                    prefetch(("hg", b, 0), hg_cols(0))
                ps = PS.get()
                proj_fm(w_x, cuTv, cuT.bs, 0, LC, ps)
                rx = tmpf.get()
                cp("act", rx[:, 0:LC], ps[:, 0:LC], [ps.b], [rx.b])
                xcc = tmpf.get()
                conv(c, rx, LC, 1, xcc[:, 0:LC], [xcc.b])
                xcb = tmpb.get()
                cp("pool", xcb[:, 0:LC], xcc[:, 0:LC], [xcc.b], [xcb.b])
                for dirn in range(2):
                    a_, u_ = rg_gates(c, dirn, xcb, LC, xcc[:, 0:LC], [xcc.b])
                    hh = tmpf.get()
                    if dirn == 0:
                        S.op("dve", (lambda o, d0, d1: (lambda e: e.tensor_tensor_scan(o, d0, d1, 0.0, ALU.mult, ALU.add)))(
                            hh[:, 0:LC], a_[:, 0:LC], u_[:, 0:LC]), [a_.b, u_.b], [hh.b])
                        cp("dve", h0[:, 0:1], hh[:, LC - 1:LC], [hh.b], [h0.b])
                    else:
                        S.op("dve", (lambda o, d0, d1: (lambda e: e.tensor_tensor_scan(o, d0, d1, 0.0, ALU.mult, ALU.add)))(
                            hh[:, 0:LC][:, ::-1], a_[:, 0:LC][:, ::-1], u_[:, 0:LC][:, ::-1]), [a_.b, u_.b], [hh.b])
                        cp("dve", h0[:, 1:2], hh[:, 0:1], [hh.b], [h0.b])
                def rg_gates_g(dirn, xcb, n, xc_ap, xc_bufs):
                    gi = dirn * 8
                    pr = PS.get()
                    pi = PS.get()
                    mm(pr[:, 0:n], gwb[:, (gi + c) * 128:(gi + c + 1) * 128], xcb[:, 0:n], True, True, [gwb.b, xcb.b], [pr.b])
                    mm(pi[:, 0:n], gwb[:, (gi + 4 + c) * 128:(gi + 4 + c + 1) * 128], xcb[:, 0:n], True, True, [gwb.b, xcb.b], [pi.b])
                    yield
                    r_ = tmpf.get()
                    act(r_[:, 0:n], pr[:, 0:n], AF.Sigmoid, [pr.b, gb.b], [r_.b], bias=gb[:, gi + c:gi + c + 1])
                    yield
                    i_ = tmpf.get()
                    act(i_[:, 0:n], pi[:, 0:n], AF.Sigmoid, [pi.b, gb.b], [i_.b], bias=gb[:, gi + 4 + c:gi + 4 + c + 1])
                    yield
                    a_ = tmpf.get()
                    act(a_[:, 0:n], r_[:, 0:n], AF.Exp, [r_.b, cL.b], [a_.b], scale=cL[:, dirn * 4 + c:dirn * 4 + c + 1])
                    yield
                    a2 = tmpf.get()
                    tt("dve", a2[:, 0:n], a_[:, 0:n], a_[:, 0:n], ALU.mult, [a_.b], [a2.b])
                    tt("dve", i_[:, 0:n], i_[:, 0:n], xc_ap, ALU.mult, [i_.b] + xc_bufs, [i_.b])
                    yield
                    act(a2[:, 0:n], a2[:, 0:n], AF.Sqrt, [a2.b], [a2.b], bias=1.0, scale=-1.0)
                    yield
                    tt("dve", i_[:, 0:n], i_[:, 0:n], a2[:, 0:n], ALU.mult, [i_.b, a2.b], [i_.b])
                    return a_, i_

                def rg_fwd(s):
                    t0 = s * 512
                    ps = PS.get()
                    proj_fm(w_x, uTv, uT.bs[s * 4:(s + 1) * 4], t0, 512, ps)
                    yield
                    rx = tmpf.get()
                    cp("act", rx[:], ps[:], [ps.b], [rx.b])
                    yield
                    conv(c, rx, 512, 8, xc[:, t0:t0 + 512], [xc.bs[s]])
                    yield
                    xcb = tmpb.get()
                    cp("pool", xcb[:], xc[:, t0:t0 + 512], [xc.bs[s]], [xcb.b])
                    yield
                    a_, u_ = yield from rg_gates_g(0, xcb, 512, xc[:, t0:t0 + 512], [xc.bs[s]])
                    yield
                    init = h0[:, 0:1] if s == 0 else hf[:, t0 - 1:t0]
                    ib = [h0.b] if s == 0 else [hf.bs[s - 1]]
                    S.op("dve", (lambda o, d0, d1, ini: (lambda e: e.tensor_tensor_scan(o, d0, d1, ini, ALU.mult, ALU.add)))(
                        hf[:, t0:t0 + 512], a_[:], u_[:], init), [a_.b, u_.b] + ib, [hf.bs[s]])

                hbt = {}

                def rg_bwd(s):
                    t0 = s * 512
                    xcb = tmpb.get()
                    cp("pool", xcb[:], xc[:, t0:t0 + 512], [xc.bs[s]], [xcb.b])
                    yield
                    a_, u_ = yield from rg_gates_g(1, xcb, 512, xc[:, t0:t0 + 512], [xc.bs[s]])
                    yield
                    hb = tmpf.get()
                    hbt[s] = hb
                    init = h0[:, 1:2] if s == 7 else hbt[s + 1][:, 0:1]
                    ib = [h0.b] if s == 7 else [hbt[s + 1].b]
                    S.op("dve", (lambda o, d0, d1, ini: (lambda e: e.tensor_tensor_scan(o, d0, d1, ini, ALU.mult, ALU.add)))(
                        hb[:, ::-1], a_[:, ::-1], u_[:, ::-1], init), [a_.b, u_.b] + ib, [hb.b])
                    yield
                    ps = PS.get()
                    proj_fm(w_g, uTv, uT.bs[s * 4:(s + 1) * 4], t0, 512, ps)
                    yield
                    gl = tmpf.get()
                    act(gl[:], ps[:], AF.Gelu_apprx_tanh, [ps.b], [gl.b])
                    yield
                    hs = tmpf.get()
                    tt("dve", hs[:], hf[:, t0:t0 + 512], hb[:], ALU.add, [hf.bs[s], hb.b], [hs.b])
                    yield
                    ob = tmpb.get()
                    tt("dve", ob[:], hs[:], gl[:], ALU.mult, [hs.b, gl.b], [ob.b])
                    dma("sp", mix_scr[b, c, :, t0:t0 + 512], ob[:], [ob.b], [])

                for s in range(0, 8, 2):
                    zip_run([rg_fwd(s), rg_fwd(s + 1)])
                for s in range(7, -1, -2):
                    zip_run([rg_bwd(s), rg_bwd(s - 1)])

            S.barrier()
            of = fview(LOC, 4096, "of", 16)
            qall = bview(LOC + 16384, L, "qall", 16)
            Vall = bview(LOC + 16384 + 8192, 32 * 128, "Vall", 32)

            def hg_A(hd, dirn, src_v, src_bufs, t0, n, w_z, w_q, w_v, w_hg, with_out, Vt, Vbufs, vcol0, sidx, po, pcol, first, Usb, ucol):
                nch = n // 64
                nw = n // 128
                li = dirn * 4 + hd
                pz = PS.get()
                proj_fm(w_z, src_v, src_bufs, t0, n, pz)
                yield
                sg = tmph.get()
                act(sg[:, 0:n], pz[:, 0:n], AF.Exp, [pz.b], [sg.b], scale=-1.0)
                yield
                act(sg[:, 0:n], sg[:, 0:n], AF.Ln, [sg.b], [sg.b], bias=1.0)
                yield
                act(sg[:, 0:n], sg[:, 0:n], AF.Exp, [sg.b], [sg.b], scale=-1.0)
                yield
                lf = tmph.get()
                act(lf[:, 0:n], sg[:, 0:n], AF.Ln, [sg.b, oml.b, lb.b], [lf.b], bias=lb[:, li:li + 1], scale=oml[:, li:li + 1])
                kk_ = tmph.get()
                ts("dve", kk_[:, 0:n], sg[:, 0:n], noml[:, li:li + 1], oml[:, li:li + 1], ALU.mult, ALU.add,
                   [sg.b, oml.b, noml.b], [kk_.b])
                yield
                Gc = tmph.get()
                S.op("dve", (lambda o, d0, d1: (lambda e: e.tensor_tensor_scan(o, d0, d1, 0.0, ALU.mult, ALU.add)))(
                    Gc[:, 0:n], resetm[:, 0:n], lf[:, 0:n]), [resetm.b, lf.b], [Gc.b])
                yield
                Gv = Gc[:, 0:n].rearrange("p (c t) -> p c t", t=64)
                Glast = Gv[:, :, 63:64].to_broadcast([128, nch, 64])
                dl = small.get()
                act(dl[:, 0:nch], Gc[:, 63:n:64], AF.Exp, [Gc.b], [dl.b])
                e3 = tmph.get()
                e3v = e3[:, 0:n].rearrange("p (c t) -> p c t", t=64)
                if dirn == 0:
                    Hq = Gc
                    tt("dve", e3v, Glast, Gv, ALU.subtract, [Gc.b], [e3.b])
                else:
                    e1 = tmph.get()
                    e1v = e1[:, 0:n].rearrange("p (c t) -> p c t", t=64)
                    tt("dve", e1v, Glast, Gv, ALU.subtract, [Gc.b], [e1.b])
                    tt("pool", e3[:, 0:n], Gc[:, 0:n], lf[:, 0:n], ALU.subtract, [Gc.b, lf.b], [e3.b])
                    yield
                    tt("pool", e1[:, 0:n], e1[:, 0:n], lf[:, 0:n], ALU.add, [e1.b, lf.b], [e1.b])
                    Hq = e1
                yield
                kd = tmphb.get()
                act(e3[:, 0:n], e3[:, 0:n], AF.Exp, [e3.b], [e3.b])
                yield
                tt("pool", kd[:, 0:n], kk_[:, 0:n], e3[:, 0:n], ALU.mult, [kk_.b, e3.b], [kd.b])
                qg = None
                if with_out:
                    kg = tmphb.get()
                    en = tmph.get()
                    act(en[:, 0:n], Hq[:, 0:n], AF.Exp, [Hq.b], [en.b], scale=-1.0)
                    if dirn == 0:
                        pq = PS.get()
                        proj_fm(w_q, src_v, src_bufs, t0, n, pq)
                        yield
                        sq_ = tmph.get()
                        act(sq_[:, 0:n], pq[:, 0:n], AF.Exp, [pq.b], [sq_.b], scale=-1.0)
                        yield
                        act(sq_[:, 0:n], sq_[:, 0:n], AF.Ln, [sq_.b], [sq_.b], bias=1.0)
                        yield
                        act(sq_[:, 0:n], sq_[:, 0:n], AF.Exp, [sq_.b], [sq_.b], scale=-1.0)
                        yield
                        tt("dve", qall[:, t0:t0 + n], sq_[:, 0:n], pq[:, 0:n], ALU.mult, [sq_.b, pq.b], [qall.bs[sidx]])
                    yield
                    tt("pool", kg[:, 0:n], kk_[:, 0:n], en[:, 0:n], ALU.mult, [kk_.b, en.b], [kg.b])
                    ep = tmph.get()
                    act(ep[:, 0:n], Hq[:, 0:n], AF.Exp, [Hq.b], [ep.b])
                    yield
                    qg = tmphb.get()
                    tt("dve", qg[:, 0:n], qall[:, t0:t0 + n], ep[:, 0:n], ALU.mult, [qall.bs[sidx], ep.b], [qg.b])
                if dirn == 0:
                    pv = PS.get()
                    for w in range(nw):
                        proj_tm(w_v, src_v, src_bufs, t0 + w * 128, pv, w * 128)
                    yield
                    cp("act", Vt[:, vcol0:vcol0 + n], pv[:, 0:n], [pv.b], Vbufs)
                yield
                pk = PS.get()
                pkb = pk[:].bitcast(BF16)
                for w in range(nw):
                    tr(pkb[:, w * 128:(w + 1) * 128], kd[:, w * 128:(w + 1) * 128], identB[:], [kd.b, identB.b], [pk.b])
                yield
                kdT = tmphb.get()
                cp("act", kdT[:, 0:n], pkb[:, 0:n], [pk.b], [kdT.b])
                yield
                Uv = Usb[:, ucol:ucol + nch * 128].rearrange("p (w c k) -> p w c k", c=2, k=128)
                pus = [PS.get(), PS.get()]
                for cc in range(2):
                    pu = pus[cc]
                    for w in range(nw):
                        mm(pu[:, w * 128:(w + 1) * 128], kdT[cc * 64:(cc + 1) * 64, w * 128:(w + 1) * 128],
                           Vt[cc * 64:(cc + 1) * 64, vcol0 + w * 128:vcol0 + (w + 1) * 128], True, True, [kdT.b] + Vbufs, [pu.b])
                    yield
                for cc in range(2):
                    cp("act", Uv[:, :, cc, :], pus[cc][:, 0:nw * 128].rearrange("p (w k) -> p w k", k=128), [pus[cc].b], [Usb.b])
                    yield
                if with_out:
                    pa = PS.get()
                    for w in range(nw):
                        mm(pa[:, w * 128:(w + 1) * 128], kg[:, w * 128:(w + 1) * 128], qg[:, w * 128:(w + 1) * 128], True, True,
                           [kg.b, qg.b], [pa.b])
                    yield
                    AT = tmphb.get()
                    msk = mask4F if dirn == 0 else mask4B
                    tt("dve", AT[:, 0:n], pa[:, 0:n], msk[:, 0:n], ALU.mult, [pa.b, msk.b], [AT.b])
                    yield
                    for w in range(nw):
                        mm(po[:, pcol + w * 128:pcol + (w + 1) * 128], Vt[:, vcol0 + w * 128:vcol0 + (w + 1) * 128],
                           AT[:, w * 128:(w + 1) * 128], first and w == 0, False, Vbufs + [AT.b], [po.b], sgc=True)
                return dict(hd=hd, dirn=dirn, src_v=src_v, src_bufs=src_bufs, t0=t0, n=n, nch=nch, w_hg=w_hg, with_out=with_out,
                            dl=dl, Usb=Usb, ucol=ucol, qg=qg, po=po, pcol=pcol, sidx=sidx)

            def hg_B(c_, last_in_bank):
                dirn, n, nch, t0, hd, sidx = c_["dirn"], c_["n"], c_["nch"], c_["t0"], c_["hd"], c_["sidx"]
                dl, Usb, qg, po, pcol, ucol = c_["dl"], c_["Usb"], c_["qg"], c_["po"], c_["pcol"], c_["ucol"]
                order = list(range(nch)) if dirn == 0 else list(range(nch - 1, -1, -1))
                Sst = SstR.get()
                cp("pool", Sst[:, 0:128], Sfin[dirn][:], [Sfin[dirn].b], [Sst.b])
                yield
                for i, c in enumerate(order):
                    stt("dve", Sst[:, (i + 1) * 128:(i + 2) * 128], Sst[:, i * 128:(i + 1) * 128], dl[:, c:c + 1],
                        Usb[:, ucol + c * 128:ucol + (c + 1) * 128], ALU.mult, ALU.add, [Sst.b, dl.b, Usb.b], [Sst.b])
                    yield
                cp("pool", Sfin[dirn][:], Sst[:, nch * 128:(nch + 1) * 128], [Sst.b], [Sfin[dirn].b])
                if not c_["with_out"]:
                    return
                Sall = SallR.get()
                cp("act", Sall[:, 0:nch * 128], Sst[:, 0:nch * 128], [Sst.b], [Sall.b])
                yield
                for i, c in enumerate(order):
                    mm(po[:, pcol + c * 64:pcol + (c + 1) * 64], Sall[:, i * 128:(i + 1) * 128], qg[:, c * 64:(c + 1) * 64], False,
                       last_in_bank and i == nch - 1, [Sall.b, qg.b], [po.b], sgc=True)
                yield
                if dirn == 0:
                    cp("act", of[:, t0:t0 + n], po[:, pcol:pcol + n], [po.b], [of.bs[sidx]])
                    yield
                return

            def get512():
                if tmph.i % 2:
                    tmph.i += 1
                i_ = tmph.i % len(tmph.tiles)
                a_ = tmph.get()
                b_ = tmph.get()
                return tmpf_raw[:, i_ * 256:i_ * 256 + 512], [a_.b, b_.b]

            def get512b():
                if tmphb.i % 2:
                    tmphb.i += 1
                i_ = tmphb.i % len(tmphb.tiles)
                a_ = tmphb.get()
                b_ = tmphb.get()
                return tmpb_raw[:, i_ * 256:i_ * 256 + 512], [a_.b, b_.b]

            def hg_epi(hd, po, t0, sidxs, w_hg):
                n = 512
                src_bufs = uT.bs[t0 // 128:t0 // 128 + 4]
                ofb = [of.bs[i_] for i_ in sidxs]
                osum, ob_ = get512()
                tt("dve", osum, of[:, t0:t0 + n], po[:, 0:n], ALU.add, ofb + [po.b], ob_)
                yield
                sq, sqb = get512()
                tt("pool", sq, osum, osum, ALU.mult, ob_, sqb)
                yield
                pn = PS.get()
                mm(pn[:, 0:n], onesF[:], sq, True, True, [onesF.b] + sqb, [pn.b])
                yield
                act(sq, pn[:, 0:n], AF.Ln, [pn.b], sqb, bias=EPS, scale=1.0 / 128.0)
                yield
                act(sq, sq, AF.Exp, sqb, sqb, scale=-0.5)
                yield
                tt("pool", osum, osum, sq, ALU.mult, ob_ + sqb, ob_)
                yield
                ph = PS.get()
                proj_fm(w_hg, uTv, src_bufs, t0, n, ph)
                yield
                sl, slb = get512()
                act(sl, ph[:, 0:n], AF.Exp, [ph.b], slb, scale=-1.0)
                yield
                act(sl, sl, AF.Ln, slb, slb, bias=1.0)
                yield
                act(sl, sl, AF.Exp, slb, slb, scale=-1.0)
                yield
                tt("dve", sl, sl, ph[:, 0:n], ALU.mult, slb + [ph.b], slb)
                yield
                ob, obb = get512b()
                stt("dve", ob, sl, ng[:, hd:hd + 1], osum, ALU.mult, ALU.mult, slb + [ng.b] + ob_, obb)
                dma("sp", mix_scr[b, 4 + hd, :, t0:t0 + n], ob, obb, [])

            def zip_run(gens):
                res = [None] * len(gens)
                live = list(range(len(gens)))
                while live:
                    for gi in list(live):
                        try:
                            next(gens[gi])
                        except StopIteration as e_:
                            res[gi] = e_.value
                            live.remove(gi)
                return res

            NSUB = 16
            for hd in range(int(os.environ.get('K_NHG', '4'))):
                w_q, w_zf, w_zb, w_v, w_hg = take(("hg", b, hd), hg_cols(hd))
                precast(6)
                if hd + 1 < 4:
                    prefetch(("hg", b, hd + 1), hg_cols(hd + 1))
                for dirn in range(2):
                    S.op("pool", (lambda o: (lambda e: e.memset(o, 0.0)))(Sfin[dirn][:]), [], [Sfin[dirn].b])

                def lat(dirn, s__, po, pcol, first, ub_):
                    return hg_A(hd, dirn, uTv, uT.bs[s__ * 2:(s__ + 1) * 2], s__ * 256, 256, w_zf if dirn == 0 else w_zb, w_q, w_v, w_hg,
                                True, Vall, Vall.bs[s__ * 2:(s__ + 1) * 2], s__ * 256, s__, po, pcol, first, ub_, pcol * 2)

                pairs = []
                pairs.append(lambda: [hg_A(hd, 0, cuTv, cuT.bs, 0, LC, w_zf, w_q, w_v, w_hg, False, Vc, [Vc.b], 0, 0, None, 0, False, UsbR.get(), 0)])
                pairs.append(lambda: [hg_A(hd, 1, cuTv, cuT.bs, 0, LC, w_zb, w_q, w_v, w_hg, False, Vc, [Vc.b], 0, 0, None, 0, False, UsbR.get(), 0)])
                nsub = int(os.environ.get('K_NS', str(NSUB)))
                for p_ in range(nsub // 2):
                    def mkp(dirn, sa, sb):
                        def f():
                            po = POR.get()
                            ub_ = UsbR.get()
                            return [lat(dirn, sa, po, (sa % 2) * 256, True, ub_), lat(dirn, sb, po, (sb % 2) * 256, False, ub_)]
                        return f
                    pairs.append(mkp(0, 2 * p_, 2 * p_ + 1))
                for p_ in range(nsub // 2 - 1, -1, -1):
                    pairs.append(mkp(1, 2 * p_ + 1, 2 * p_))
                def gen_B(prev_):
                    for k_, c_ in enumerate(prev_):
                        yield from hg_B(c_, k_ == len(prev_) - 1)

                def gen_E(prev_):
                    t0_ = min(c_["t0"] for c_ in prev_)
                    yield from hg_epi(hd, prev_[0]["po"], t0_, [c_["sidx"] for c_ in prev_], w_hg)

                def needs_epi(prev_):
                    return prev_ is not None and prev_[0]["with_out"] and prev_[0]["dirn"] == 1
                prev = zip_run(pairs[0]())
                prevE = None
                for pf in pairs[1:]:
                    gens = pf()
                    extra = [gen_B(prev)] + ([gen_E(prevE)] if prevE is not None else [])
                    res = zip_run(gens + extra)
                    prevE = prev if needs_epi(prev) else None
                    prev = res[:len(gens)]
                zip_run([gen_B(prev)] + ([gen_E(prevE)] if prevE is not None else []))
                if needs_epi(prev):
                    zip_run([gen_E(prev)])

            S.barrier()
            woutb = bview(0, 8 * D, "woutb")
            woutv = woutb[:].rearrange("p (k n) -> p k n", k=8)
            wost = fview(16384, 4096, "wost")
            bcast = [fview(32768 + i * 4096, D, "bc%d" % i) for i in range(3)]
            lnp = fview(32768 + 12288, 2 * D, "lnp")
            dma("sp", lnp[:], lnp_d[:, 0:2 * D], [], [lnp.b])
            for j, lo in enumerate((2048, 3072, 4096)):
                dma("sp", bcast[j][:], m_scr[b:b + 1, lo:lo + D].partition_broadcast(128), [], [bcast[j].b])
            for hlf in range(2):
                wv = wost[:].rearrange("p (k n) -> p k n", k=8)
                dma("sp", wv, wout_d[:, hlf * 512:(hlf + 1) * 512].rearrange("(k p) n -> p k n", p=128), [], [wost.b])
                tt("pool", woutv[:, :, hlf * 512:(hlf + 1) * 512], wv,
                   bcast[0][:, hlf * 512:(hlf + 1) * 512].unsqueeze(1).to_broadcast([128, 8, 512]), ALU.mult,
                   [wost.b, bcast[0].b], [woutb.b])
            ts("dve", bcast[2][:], bcast[2][:], 1.0, None, ALU.add, None, [bcast[2].b], [bcast[2].b])
            tt("dve", bcast[0][:], lnp[:, 0:D], bcast[2][:], ALU.mult, [lnp.b, bcast[2].b, woutb.b], [bcast[0].b])
            tt("dve", bcast[2][:], lnp[:, D:2 * D], bcast[2][:], ALU.mult, [lnp.b, bcast[2].b], [bcast[2].b])
            tt("dve", bcast[1][:], bcast[1][:], bcast[2][:], ALU.add, [bcast[1].b, bcast[2].b], [bcast[1].b])
            S.barrier()

            class _R:
                def __init__(self, items):
                    self.items = items
                    self.i = 0

                def get(self):
                    t = self.items[self.i % len(self.items)]
                    self.i += 1
                    return t
            mixin = _R([bview(53248 + i * 8192, 8 * 512, "mixin%d" % i) for i in range(2)])
            zt = _R([fview(K64 + 4096 + i * 4096, D, "zt%d" % i) for i in range(6)]
                    + [fview(28672, D, "zt6"), fview(32768 + 8192, D, "zt7")])
            PSC = RV(PS.tiles + POR.tiles)
            xck = [fview(K64 + 4096 + 6 * 4096 + i * 4096, D, "xck%d" % i) for i in range(2)] + xin.tiles

            def c_load_x(i_):
                xt_ = xck[i_ % 4]
                dma("sp", xt_[:], x_d[b, i_ * 128:(i_ + 1) * 128, :], [], [xt_.b])
            for i_ in range(4):
                c_load_x(i_)

            def c_load_mix(g_):
                mi_ = mixin.items[g_ % 2]
                dma("sp", mi_[:].rearrange("p (c t) -> p c t", c=8),
                    mix_scr[b, :, :, g_ * 512:(g_ + 1) * 512].rearrange("c p t -> p c t"), [], [mi_.b])
            c_load_mix(0)
            u2b = _R([bview(16384 + i * 2048, D, "u2b%d" % i) for i in range(2)])
            u2T = _R([fview(16384 + 4096 + i * 4096, D, "u2T%d" % i) for i in range(2)])
            for g in range(8):
                mi = mixin.items[g % 2]
                miv = mi[:].rearrange("p (c t) -> p c t", c=8)
                if g + 1 < 8:
                    c_load_mix(g + 1)
                def c_tile(ii):
                    i = g * 4 + ii
                    row0 = b * L + i * 128
                    xt = xck[i % 4]
                    z = zt.get()
                    for hlf in range(2):
                        ps_ = PSC.get()
                        for c in range(8):
                            mm(ps_[:], miv[:, c, ii * 128:(ii + 1) * 128], woutv[:, c, hlf * 512:(hlf + 1) * 512],
                               c == 0, c == 7, [mi.b, woutb.b], [ps_.b])
                        yield
                        stt("dve", z[:, hlf * 512:(hlf + 1) * 512], xt[:, hlf * 512:(hlf + 1) * 512], ALPHA, ps_[:],
                            ALU.mult, ALU.add, [xt.b, ps_.b], [z.b])
                        yield
                    if i + 4 < 32:
                        c_load_x(i + 4)
                    mv = ln_stats(S, z, stats, small, act)
                    yield
                    act(z[:], z[:], AF.Identity, [z.b, mv.b], [z.b], bias=mv[:, 3:4], scale=mv[:, 2:3])
                    yield
                    u2 = zt.get()
                    tt("dve", u2[:], z[:], bcast[0][:], ALU.mult, [z.b, bcast[0].b], [u2.b])
                    yield
                    tt("dve", u2[:], u2[:], bcast[1][:], ALU.add, [u2.b, bcast[1].b], [u2.b])
                    yield
                    tt("dve", z[:], z[:], lnp[:, 0:D], ALU.mult, [z.b, lnp.b], [z.b])
                    yield
                    tt("dve", z[:], z[:], lnp[:, D:2 * D], ALU.add, [z.b, lnp.b], [z.b])
                    dma("sp", h1_scr[row0:row0 + 128, :], z[:], [z.b], [])
                    if dbg == 1:
                        dma("sp", dbg_d[row0:row0 + 128, :], z[:], [z.b], [])
                    yield
                    ub = u2b.get()
                    cp("act", ub[:], u2[:], [u2.b], [ub.b])
                    dma("sp", u2_scr[row0:row0 + 128, :], ub[:], [ub.b], [])
                    u2s[ii] = u2

                u2s = [None] * 4
                zip_run([c_tile(ii_) for ii_ in range(4)])
                for ii in range(4):
                    u2 = u2s[ii]
                    uT2 = u2T.get()
                    uT2v = uT2[:].rearrange("p (k t) -> p k t", k=8)
                    for hlf in range(2):
                        pt = PSC.get()
                        for kk in range(4):
                            k = hlf * 4 + kk
                            tr(pt[:, kk * 128:(kk + 1) * 128], u2[:, k * 128:(k + 1) * 128], identF[:], [u2.b, identF.b], [pt.b])
                        if hlf == 0:
                            cp("act", uT2[:, 0:512], pt[:], [pt.b], [uT2.b])
                        else:
                            cp("dve", uT2[:, 512:1024], pt[:], [pt.b], [uT2.b])
                    for k in range(8):
                        mm(PL[:, ii * 36:(ii + 1) * 36], uT2v[:, k, :], rwv[:, k, :], k == 0, k == 7, [uT2.b, rw.b], [PL.b])
                pl = PL
                i0 = g * 4 + b * 32
                lg = rt.get()
                tt("dve", lg[:, 0:144], pl[:, 0:144], rb4[:], ALU.add, [pl.b, rb4.b], [lg.b])
                lgv = lg[:, 0:144].rearrange("p (i n) -> p i n", n=36)
                gmax = small.get()
                S.op("dve", (lambda o, i_: (lambda e: e.tensor_reduce(o, i_, AX.X, ALU.max)))(gmax[:, 0:4], lgv[:, :, 0:4]), [lg.b], [gmax.b])
                gsh = rt.get()
                gshv = gsh[:, 0:16].rearrange("p (i n) -> p i n", n=4)
                tt("dve", gshv, lgv[:, :, 0:4], gmax[:, 0:4].unsqueeze(2).to_broadcast([128, 4, 4]), ALU.subtract, [lg.b, gmax.b], [gsh.b])
                gex = rt.get()
                act(gex[:, 0:16], gsh[:, 0:16], AF.Exp, [gsh.b], [gex.b])
                S.op("dve", (lambda o, i_: (lambda e: e.tensor_reduce(o, i_, AX.X, ALU.add)))(
                    gmax[:, 4:8], gex[:, 0:16].rearrange("p (i n) -> p i n", n=4)), [gex.b], [gmax.b])
                S.op("dve", (lambda o, i_: (lambda e: e.reciprocal(o, i_)))(gmax[:, 8:12], gmax[:, 4:8]), [gmax.b], [gmax.b])
                pen = rt.get()
                ts("dve", pen[:, 0:16], gsh[:, 0:16], 0.0, None, ALU.is_equal, None, [gsh.b], [pen.b])
                ts("dve", pen[:, 0:16], pen[:, 0:16], -1.0, 1e30, ALU.add, ALU.mult, [pen.b], [pen.b])
                elm = rt.get()
                for j in range(4):
                    tt("dve", elm[:, j * 32:(j + 1) * 32].rearrange("p (g e) -> p g e", e=8),
                       lgv[:, j, 4:36].rearrange("p (g e) -> p g e", e=8),
                       pen[:, j * 4:(j + 1) * 4].unsqueeze(2).to_broadcast([128, 4, 8]), ALU.add, [lg.b, pen.b], [elm.b])
                elmv = elm[:, 0:128].rearrange("p (i e) -> p i e", e=32)
                m12 = small.get()
                S.op("dve", (lambda o, i_: (lambda e: e.tensor_reduce(o, i_, AX.X, ALU.max)))(m12[:, 0:4], elmv), [elm.b], [m12.b])
                oh1 = rt.get()
                oh1v = oh1[:, 0:128].rearrange("p (i e) -> p i e", e=32)
                tt("dve", oh1v, elmv, m12[:, 0:4].unsqueeze(2).to_broadcast([128, 4, 32]), ALU.is_equal, [elm.b, m12.b], [oh1.b])
                elm2 = rt.get()
                stt("dve", elm2[:, 0:128], oh1[:, 0:128], -1e30, elm[:, 0:128], ALU.mult, ALU.add, [oh1.b, elm.b], [elm2.b])
                elm2v = elm2[:, 0:128].rearrange("p (i e) -> p i e", e=32)
                S.op("dve", (lambda o, i_: (lambda e: e.tensor_reduce(o, i_, AX.X, ALU.max)))(m12[:, 4:8], elm2v), [elm2.b], [m12.b])
                oh2 = rt.get()
                oh2v = oh2[:, 0:128].rearrange("p (i e) -> p i e", e=32)
                tt("dve", oh2v, elm2v, m12[:, 4:8].unsqueeze(2).to_broadcast([128, 4, 32]), ALU.is_equal, [elm2.b, m12.b], [oh2.b])
                cp("pool", OH1[:, i0 * 32:(i0 + 4) * 32], oh1[:, 0:128], [oh1.b], [OH1.b])
                cp("pool", OH2[:, i0 * 32:(i0 + 4) * 32], oh2[:, 0:128], [oh2.b], [OH2.b])
                tt("dve", m12[:, 8:12], m12[:, 0:4], m12[:, 4:8], ALU.subtract, [m12.b], [m12.b])
                act(m12[:, 8:12], m12[:, 8:12], AF.Sigmoid, [m12.b], [m12.b])
                tt("dve", W1[:, i0:i0 + 4], m12[:, 8:12], gmax[:, 8:12], ALU.mult, [m12.b, gmax.b], [W1.b])
                tt("dve", W2[:, i0:i0 + 4], gmax[:, 8:12], W1[:, i0:i0 + 4], ALU.subtract, [gmax.b, W1.b], [W2.b])

        def moe_phase():
            precast(96)
            S.barrier()
            PSM = RV(PS.tiles + POR.tiles + [PL])
            o = [0]

            def af(n, name, nbuf=0):
                v = fview(o[0], n, name, nbuf)
                o[0] += n * 4
                return v

            def ab(n, name, nbuf=0):
                v = bview(o[0], n, name, nbuf)
                o[0] += n * 2
                return v
            DST1 = V(BIG[:, o[0] // 4:o[0] // 4 + 64].bitcast(I32), "DST1")
            o[0] += 256
            DST2 = V(BIG[:, o[0] // 4:o[0] // 4 + 64].bitcast(I32), "DST2")
            o[0] += 256
            WIDX = V(BIG[:, o[0] // 4:o[0] // 4 + NBLK].bitcast(I32), "WIDX")
            o[0] += NBLK * 4
            lnp2 = af(2 * D, "lnp2")
            g2b = af(D, "g2b")
            meta_end = o[0]
            SELb = ab(2048, "SELb")
            PRE = af(2048, "PRE")
            TOT = af(2048, "TOT")
            BASE = af(2048, "BASE")
            DD = af(2048, "DD")
            CMPB = af(NBLK * 32, "CMPB")
            triuF = af(128, "triuF")
            triuB = ab(128, "triuB")
            thr = af(64 + NBLK, "thr")
            iot = af(2, "iot")
            CNT = af(32, "CNT")
            NBK = af(32, "NBK")
            PEND = af(32, "PEND")
            PST = af(32, "PST")
            ones32 = af(32, "ones32")
            D1f = af(64, "D1f")
            D2f = af(64, "D2f")
            BLKE = af(NBLK, "BLKE")
            WIDXf = af(NBLK, "WIDXf")
            SAME = af(NBLK, "SAME")
            tmp_end = o[0]

            dma("sp", triuF[:], triu_d, [], [triuF.b])
            dma("sp", thr[:], thr_d, [], [thr.b])
            dma("sp", iot[:], iota_d, [], [iot.b])
            dma("sp", lnp2[:], lnp_d[:, 2 * D:4 * D], [], [lnp2.b])
            cp("dve", triuB[:], triuF[:], [triuF.b], [triuB.b])
            S.op("dve", lambda e: e.memset(ones32[:], 1.0), [], [ones32.b])
            tt("dve", SELb[:], OH1[:], OH2[:], ALU.add, [OH1.b, OH2.b], [SELb.b])
            for j in range(4):
                pp = PSM.get()
                mm(pp[:], triuB[:], SELb[:, j * 512:(j + 1) * 512], True, True, [triuB.b, SELb.b], [pp.b])
                cp("act", PRE[:, j * 512:(j + 1) * 512], pp[:], [pp.b], [PRE.b])
                pq = PSM.get()
                mm(pq[:], onesB[:], SELb[:, j * 512:(j + 1) * 512], True, True, [onesB.b, SELb.b], [pq.b])
                cp("dve", TOT[:, j * 512:(j + 1) * 512], pq[:], [pq.b], [TOT.b])
            S.op("dve", lambda e: e.memset(BASE[:, 0:32], 0.0), [], [BASE.b])
            for i in range(1, 64):
                tt("dve", BASE[:, i * 32:(i + 1) * 32], BASE[:, (i - 1) * 32:i * 32], TOT[:, (i - 1) * 32:i * 32], ALU.add,
                   [BASE.b, TOT.b], [BASE.b])
            tt("dve", CNT[:], BASE[:, 63 * 32:64 * 32], TOT[:, 63 * 32:64 * 32], ALU.add, [BASE.b, TOT.b], [CNT.b])
            cmpv = CMPB[:, 0:32 * 64].rearrange("p (e m) -> p e m", m=64)
            tt("dve", cmpv, CNT[:].unsqueeze(2).to_broadcast([128, 32, 64]), thr[:, 0:64].unsqueeze(1).to_broadcast([128, 32, 64]),
               ALU.is_gt, [CNT.b, thr.b], [CMPB.b])
            S.op("dve", lambda e: e.tensor_reduce(NBK[:], cmpv, AX.X, ALU.add), [CMPB.b], [NBK.b])
            S.op("dve", lambda e: e.tensor_tensor_scan(PEND[:], ones32[:], NBK[:], 0.0, ALU.mult, ALU.add), [ones32.b, NBK.b], [PEND.b])
            tt("dve", PST[:], PEND[:], NBK[:], ALU.subtract, [PEND.b, NBK.b], [PST.b])
            ts("dve", PST[:], PST[:], float(BLK), None, ALU.mult, None, [PST.b], [PST.b])
            ts("dve", PEND[:], PEND[:], float(BLK), None, ALU.mult, None, [PEND.b], [PEND.b])
            tt("dve", DD[:], PRE[:], BASE[:], ALU.add, [PRE.b, BASE.b], [DD.b])
            ddv = DD[:].rearrange("p (i e) -> p i e", e=32)
            tt("dve", ddv, ddv, PST[:].unsqueeze(1).to_broadcast([128, 64, 32]), ALU.add, [DD.b, PST.b], [DD.b])
            tmpv = PRE[:].rearrange("p (i e) -> p i e", e=32)
            tt("dve", PRE[:], DD[:], OH1[:], ALU.mult, [DD.b, OH1.b], [PRE.b])
            S.op("dve", lambda e: e.tensor_reduce(D1f[:], tmpv, AX.X, ALU.add), [PRE.b], [D1f.b])
            tt("dve", PRE[:], DD[:], OH2[:], ALU.mult, [DD.b, OH2.b], [PRE.b])
            S.op("dve", lambda e: e.tensor_reduce(D2f[:], tmpv, AX.X, ALU.add), [PRE.b], [D2f.b])
            cp("dve", DST1[:], D1f[:], [D1f.b], [DST1.b])
            cp("dve", DST2[:], D2f[:], [D2f.b], [DST2.b])
            cbv = CMPB[:].rearrange("p (j e) -> p j e", e=32)
            tt("dve", cbv, PEND[:].unsqueeze(1).to_broadcast([128, NBLK, 32]),
               thr[:, 64:64 + NBLK].unsqueeze(2).to_broadcast([128, NBLK, 32]), ALU.is_le, [PEND.b, thr.b], [CMPB.b])
            S.op("dve", lambda e: e.tensor_reduce(BLKE[:], cbv, AX.X, ALU.add), [CMPB.b], [BLKE.b])
            ts("dve", BLKE[:], BLKE[:], 31.0, None, ALU.min, None, [BLKE.b], [BLKE.b])
            ts("dve", WIDXf[:], BLKE[:], 128.0, iot[:, 0:1], ALU.mult, ALU.add, [BLKE.b, iot.b], [WIDXf.b])
            S.op("dve", lambda e: e.memset(SAME[:], 0.0), [], [SAME.b])
            tt("dve", SAME[:, NWB:NBLK], BLKE[:, NWB:NBLK], BLKE[:, 0:NBLK - NWB], ALU.is_equal, [BLKE.b], [SAME.b])
            stt("dve", WIDXf[:], SAME[:], 1.0e6, WIDXf[:], ALU.mult, ALU.add, [SAME.b, WIDXf.b], [WIDXf.b])
            cp("dve", WIDX[:], WIDXf[:], [WIDXf.b], [WIDX.b])

            if dbg == 2:
                dma("sp", dbgs_d[:, 0:64], W1[:], [W1.b], [])
                dma("sp", dbgs_d[:, 64:128], W2[:], [W2.b], [])
                dma("sp", dbgs_d[:, 128:192], D1f[:], [D1f.b], [])
                dma("sp", dbgs_d[:, 192:256], D2f[:], [D2f.b], [])
                dma("sp", dbgs_d[:, 256:256 + NBLK], BLKE[:], [BLKE.b], [])
                dma("sp", dbgs_d[:, 416:448], CNT[:], [CNT.b], [])
                dma("sp", dbgs_d[:, 448:480], PST[:], [PST.b], [])
            xrow = [bview(tmp_end + i * 2048, D, "xrow%d" % i) for i in range(4)]
            for i in range(64):
                xr = xrow[i % 4]
                dma("sp", xr[:], u2_scr[i * 128:(i + 1) * 128, :], [], [xr.b])
                for dst in (DST1, DST2):
                    S.dma("pool", (lambda x_, d_, i_: (lambda e: e.indirect_dma_start(
                        out=xs_scr, out_offset=bass.IndirectOffsetOnAxis(ap=d_[:, i_:i_ + 1], axis=0),
                        in_=x_[:], in_offset=None)))(xr, dst, i), [xr.b, dst.b], [])
            S.barrier()

            wo = meta_end
            WB = []
            for i in range(NWB):
                WB.append((bview(wo, 4096, "w1b%d" % i), bview(wo + 8192, 4096, "w3b%d" % i), bview(wo + 16384, 4096, "w2b%d" % i)))
                wo += 24576
            xbk = [bview(wo + i * 2048, D, "xbk%d" % i) for i in range(3)]
            wo += 3 * 2048
            xTk = [bview(wo + i * 2048, D, "xTk%d" % i) for i in range(2)]
            wo += 2 * 2048
            hidk = [bview(wo + i * 1024, 512, "hidk%d" % i) for i in range(2)]
            wo += 2 * 1024
            sak = [fview(wo + i * 2048, 512, "sak%d" % i) for i in range(2)]
            wo += 2 * 2048
            yk = [fview(wo + i * 4096, D, "yk%d" % i) for i in range(2)]
            wo += 2 * 4096
            assert wo <= ARENA_B, wo

            regs = {}

            def load_blk_w(j):
                wb = WB[j % NWB]
                for t_, d_ in zip(wb, wb_scr):
                    def mk(t__, d__, j_):
                        def f(e):
                            if "bc" not in regs:
                                regs["bc"] = e.to_reg(4095)
                            return e.indirect_dma_start(
                                out=t__[:], out_offset=None, in_=d__,
                                in_offset=bass.IndirectOffsetOnAxis(ap=WIDX[:, j_:j_ + 1], axis=0),
                                bounds_check=regs["bc"], oob_is_err=False)
                        return f
                    S.dma("pool", mk(t_, d_, j), [WIDX.b], [t_.b])

            def load_blk_x(j):
                xb_ = xbk[j % 3]
                dma("sp", xb_[:], xs_scr[j * BLK:(j + 1) * BLK, :], [], [xb_.b])

            load_blk_w(0)
            load_blk_x(0)
            load_blk_x(1)
            def blk_transpose(j_):
                xb__ = xbk[j_ % 3]
                xT_ = xTk[j_ % 2]
                pt = PSM.get()
                ptb = pt[:].bitcast(BF16)
                for k in range(8):
                    tr(ptb[:, k * 128:(k + 1) * 128], xb__[:, k * 128:(k + 1) * 128], identB[:], [xb__.b, identB.b], [pt.b])
                return pt, ptb, xT_

            pend = blk_transpose(0)
            cp("act", pend[2][:], pend[1][:, 0:1024], [pend[0].b], [pend[2].b])
            for j in range(NBLK):
                if j + 1 < NBLK:
                    load_blk_w(j + 1)
                if j + 2 < NBLK:
                    load_blk_x(j + 2)
                w1b, w3b, w2b = WB[j % NWB]
                w1v = w1b[:].rearrange("p (k n) -> p k n", k=8)
                w3v = w3b[:].rearrange("p (k n) -> p k n", k=8)
                w2v = w2b[:].rearrange("p (k n) -> p k n", k=4)
                xT = xTk[j % 2]
                xTv = xT[:].rearrange("p (k t) -> p k t", k=8)
                pa = PSM.get()
                pb_ = PSM.get()
                for ht in range(4):
                    for k in range(8):
                        mm(pa[:, ht * 128:(ht + 1) * 128], w1v[:, k, ht * 128:(ht + 1) * 128], xTv[:, k, :], k == 0, k == 7,
                           [w1b.b, xT.b], [pa.b])
                    for k in range(8):
                        mm(pb_[:, ht * 128:(ht + 1) * 128], w3v[:, k, ht * 128:(ht + 1) * 128], xTv[:, k, :], k == 0, k == 7,
                           [w3b.b, xT.b], [pb_.b])
                if j + 1 < NBLK:
                    pend = blk_transpose(j + 1)
                sa = sak[j % 2]
                act(sa[:], pa[:], AF.Silu, [pa.b], [sa.b])
                hid = hidk[j % 2]
                tt("dve", hid[:], sa[:], pb_[:], ALU.mult, [sa.b, pb_.b], [hid.b])
                if j + 1 < NBLK:
                    cp("act", pend[2][:], pend[1][:, 0:1024], [pend[0].b], [pend[2].b])
                hv = hid[:].rearrange("p (k t) -> p k t", k=4)
                yy = yk[j % 2]
                for hlf in range(2):
                    py = PSM.get()
                    for ht in range(4):
                        mm(py[:], hv[:, ht, :], w2v[:, ht, hlf * 512:(hlf + 1) * 512], ht == 0, ht == 3, [hid.b, w2b.b], [py.b])
                    if hlf == 0:
                        cp("act", yy[:, 0:512], py[:], [py.b], [yy.b])
                    else:
                        cp("dve", yy[:, 512:1024], py[:], [py.b], [yy.b])
                dma("sp", ys_scr[j * BLK:(j + 1) * BLK, :], yy[:], [yy.b], [])
            S.barrier()

            fo = meta_end
            NF = 4
            y1k = [fview(fo + i * 4096, D, "y1k%d" % i) for i in range(NF)]
            y2k = [fview(fo + NF * 4096 + i * 4096, D, "y2k%d" % i) for i in range(NF)]
            h1k = [fview(fo + 2 * NF * 4096 + i * 4096, D, "h1k%d" % i) for i in range(NF)]
            assert fo + 3 * NF * 4096 <= ARENA_B
            g2bs = [g2b, fview(fo + 3 * NF * 4096, D, "g2b1")]
            assert fo + 3 * NF * 4096 + 4096 <= ARENA_B
            for bb in range(2):
                dma("sp", g2bs[bb][:], m_scr[bb:bb + 1, 5120:6144].partition_broadcast(128), [], [g2bs[bb].b])

            def fin_loads(i):
                for yt, dst in ((y1k[i % NF], DST1), (y2k[i % NF], DST2)):
                    S.dma("pool", (lambda y_, d_, i_: (lambda e: e.indirect_dma_start(
                        out=y_[:], out_offset=None, in_=ys_scr,
                        in_offset=bass.IndirectOffsetOnAxis(ap=d_[:, i_:i_ + 1], axis=0))))(yt, dst, i), [dst.b], [yt.b])
                dma("sp", h1k[i % NF][:], h1_scr[i * 128:(i + 1) * 128, :], [], [h1k[i % NF].b])

            for i in range(NF - 1):
                fin_loads(i)
            for i in range(64):
                bb = i // 32
                if i + NF - 1 < 64:
                    fin_loads(i + NF - 1)
                y1 = y1k[i % NF]
                y2 = y2k[i % NF]
                hh = h1k[i % NF]
                act(y1[:], y1[:], AF.Copy, [y1.b, W1.b], [y1.b], scale=W1[:, i:i + 1])
                stt("dve", y1[:], y2[:], W2[:, i:i + 1], y1[:], ALU.mult, ALU.add, [y2.b, W2.b, y1.b], [y1.b])
                if dbg == 2:
                    dma("sp", dbg_d[i * 128:(i + 1) * 128, :], y1[:], [y1.b], [])
                tt("dve", y1[:], y1[:], g2bs[bb][:], ALU.mult, [y1.b, g2bs[bb].b], [y1.b])
                stt("dve", hh[:], hh[:], ALPHA, y1[:], ALU.mult, ALU.add, [hh.b, y1.b], [hh.b])
                mv = ln_stats(S, hh, stats, small, act)
                act(hh[:], hh[:], AF.Identity, [hh.b, mv.b], [hh.b], bias=mv[:, 3:4], scale=mv[:, 2:3])
                tt("dve", hh[:], hh[:], lnp2[:, 0:D], ALU.mult, [hh.b, lnp2.b], [hh.b])
                tt("dve", hh[:], hh[:], lnp2[:, D:2 * D], ALU.add, [hh.b, lnp2.b], [hh.b])
                dma("sp", out_d[bb, (i % 32) * 128:(i % 32 + 1) * 128, :], hh[:], [hh.b], [])

        if dbg != 1:
            moe_phase()
        S.finish("sp")
        S.emit()
    return nc


def ln_stats(S, z, stats, small, act):
    st = stats.get()
    for hlf in range(2):
        S.op("dve", (lambda o, i_: (lambda e: e.bn_stats(o, i_)))(st[:, hlf * 6:(hlf + 1) * 6], z[:, hlf * 512:(hlf + 1) * 512]),
             [z.b], [st.b])
    mv = small.get()
    S.op("dve", (lambda o, i_: (lambda e: e.bn_aggr(o, i_)))(mv[:, 0:2], st[:, 0:12]), [st.b], [mv.b])
    act(mv[:, 2:3], mv[:, 1:2], AF.Sqrt, [mv.b], [mv.b], bias=EPS, scale=1.0)
    S.op("dve", (lambda o, i_: (lambda e: e.reciprocal(o, i_)))(mv[:, 2:3], mv[:, 2:3]), [mv.b], [mv.b])
    S.op("dve", (lambda o, a_, b_: (lambda e: e.scalar_tensor_tensor(o, a_, -1.0, b_, ALU.mult, ALU.mult)))(
        mv[:, 3:4], mv[:, 0:1], mv[:, 2:3]), [mv.b], [mv.b])
    return mv


def _host_consts():
    identF = np.eye(128, dtype=np.float32)
    s = np.arange(128)[:, None]
    t = np.arange(128)[None, :]
    same = (s // 64) == (t // 64)
    maskF = (same & (s <= t)).astype(np.float32)
    maskB = (same & (s >= t)).astype(np.float32)
    resetm = np.ones((128, 512), np.float32)
    resetm[:, ::64] = 0.0
    triu = (s < t).astype(np.float32)
    thr = np.zeros((128, 64 + NBLK), np.float32)
    thr[:, :64] = (np.arange(64) * BLK)[None, :]
    thr[:, 64:] = (np.arange(NBLK) * BLK)[None, :]
    iota = np.zeros((128, 2), np.float32)
    iota[:, 0] = np.arange(128)
    return dict(identF=identF, maskF=maskF, maskB=maskB, resetm=resetm, triu=triu, thr=thr, iota=iota)


def _prep_shared(inp):
    f = lambda a: np.ascontiguousarray(np.asarray(a, dtype=np.float32))
    sh = {}
    sh["ada_w"] = f(inp["ada_w"][0])
    sh["ada_b"] = f(inp["ada_b"][0]).reshape(1, -1)
    sh["w_in"] = f(inp["w_in"][0])
    cw = f(inp["rg_conv_w"][0])
    sh["convw"] = f(cw.reshape(4, 4, 128).transpose(2, 1, 0).reshape(128, 16))
    sh["convb"] = f(f(inp["rg_conv_b"][0]).reshape(4, 128).T)
    gwt = f(inp["rg_gate_w"][0])
    gw = np.zeros((128, 16, 128), np.float32)
    for d_ in range(2):
        for g_ in range(2):
            for c in range(4):
                for h_ in range(2):
                    gw[h_ * 64:(h_ + 1) * 64, d_ * 8 + g_ * 4 + c, h_ * 64:(h_ + 1) * 64] = gwt[d_, g_, c * 2 + h_]
    sh["gw"] = gw.reshape(128, 2048)
    gbt = f(inp["rg_gate_b"][0])
    sh["gb"] = f(gbt.reshape(2, 2, 4, 128).transpose(3, 0, 1, 2).reshape(128, 16))
    sh["lam"] = f(f(inp["rg_lambda"][0]).reshape(2, 4, 128).transpose(2, 0, 1).reshape(128, 8))
    sh["lbl"] = f(f(inp["hg_lb_logits"]).reshape(2, 2, 4, 128).transpose(3, 0, 1, 2).reshape(128, 16))
    sh["ng"] = f(f(inp["hg_norm_g"][0]).reshape(4, 128).T)
    sh["w_out"] = f(inp["w_out"][0])
    lnp = np.concatenate([f(inp["ln1_g"][0]), f(inp["ln1_b"][0]), f(inp["ln2_g"][0]), f(inp["ln2_b"][0])])
    sh["lnp"] = f(np.broadcast_to(lnp[None, :], (128, 4 * D)))
    sh["rw"] = f(np.concatenate([f(inp["router_g_w"][0]), f(inp["router_e_w"][0])], axis=1))
    rb = np.concatenate([f(inp["router_g_b"][0]), f(inp["router_e_b"][0])])
    sh["rb"] = f(np.broadcast_to(rb[None, :], (128, 36)))
    sh["w1r"] = f(f(inp["exp_w1"][0]).reshape(32, 8, 128, 512).transpose(0, 2, 1, 3).reshape(32 * 128, 8 * 512))
    sh["w3r"] = f(f(inp["exp_w3"][0]).reshape(32, 8, 128, 512).transpose(0, 2, 1, 3).reshape(32 * 128, 8 * 512))
    sh["w2r"] = f(f(inp["exp_w2"][0]).reshape(32, 4, 128, 1024).transpose(0, 2, 1, 3).reshape(32 * 128, 4 * 1024))
    sh.update(_host_consts())
    return sh


def _in_maps(inp):
    sh = _prep_shared(inp)
    x = np.asarray(inp["x"], np.float32)
    c = np.asarray(inp["c"], np.float32)
    ctx = np.asarray(inp["ctx"], np.float32)
    c_ctx = np.asarray(inp["c_ctx"], np.float32)
    maps = []
    for k in range(NCORES):
        m = dict(sh)
        m["x"] = np.ascontiguousarray(x[2 * k:2 * k + 2])
        m["ctx"] = np.ascontiguousarray(ctx[2 * k:2 * k + 2])
        cv = np.stack([c[2 * k], c[2 * k + 1], c_ctx], axis=0)
        m["cT"] = np.ascontiguousarray(cv.reshape(3, 8, 128).transpose(2, 1, 0).reshape(128, 24))
        maps.append(m)
    return maps


_NC_CACHE = {}


def kernel(**inputs):
    if "nc" not in _NC_CACHE:
        _NC_CACHE["nc"] = build_program(0)
    nc = _NC_CACHE["nc"]
    maps = _in_maps(inputs)
    res = run_bass_kernel_spmd(nc, maps, core_ids=list(range(NCORES)))
    out = np.concatenate([r["out"] for r in res.results], axis=0)
    return out.astype(np.float32)
```

```python
import os
import numpy as np
from contextlib import ExitStack
import ml_dtypes
import concourse.bass as bass
import concourse.mybir as mybir
from concourse.bass_utils import run_bass_kernel_spmd

F32 = mybir.dt.float32
BF16 = mybir.dt.bfloat16
I32 = mybir.dt.int32
AF = mybir.ActivationFunctionType
ALU = mybir.AluOpType
AX = mybir.AxisListType

SAME_ENG_SYNC = True
NCORES = 8
L = 4096
LC = 256
D = 1024
ALPHA = 2.0 ** 0.25
EPS = 1e-6
BLK = 128
NBLK = 160
NSLOT = NBLK * BLK
NWB = 2


class Buf:
    __slots__ = ("name", "w", "r", "excl")

    def __init__(self, name=""):
        self.name = name
        self.w = None
        self.r = {}
        self.excl = False


class Sched:
    ENGS = ("pe", "act", "dve", "pool", "sp")

    def __init__(self, nc, es, n_dma_sems=40):
        self.nc = nc
        self.q = {e: [] for e in self.ENGS}
        self.cnt = {e: 0 for e in self.ENGS}
        self.sem = {e: es.enter_context(nc.semaphore("s_" + e)) for e in ("pe", "act", "dve", "pool")}
        n_sw = 24
        self.dsem = [es.enter_context(nc.semaphore("d%d" % i)) for i in range(n_dma_sems + n_sw)]
        self.dcnt = [0] * (n_dma_sems + n_sw)
        self.n_hw = n_dma_sems
        self.n_sw = n_sw
        self.dnext = {"hw": 0, "sw": 0}
        self.seen = {e: {} for e in self.ENGS}

    def _deps(self, reads, writes):
        toks = []
        for b in reads:
            if b.w is not None:
                toks.append(b.w)
        for b in writes:
            if b.w is not None:
                toks.append(b.w)
            toks.extend(b.r.values())
        return toks

    def _mark(self, reads, writes, tok):
        for b in writes:
            b.w = tok
            b.r = {}
        for b in reads:
            b.r[id(tok[1])] = tok

    def _push(self, eng, fn, toks, inc):
        need = {}
        for kind, sem, val in toks:
            if kind == eng and (eng == "pe" or not SAME_ENG_SYNC):
                continue
            k = id(sem)
            if k not in need or need[k][1] < val:
                need[k] = (sem, val)
        waits = []
        seen = self.seen[eng]
        for k, (sem, val) in need.items():
            if seen.get(k, 0) >= val:
                continue
            seen[k] = val
            waits.append((sem, val))
        self.q[eng].append((fn, waits, inc))

    @staticmethod
    def _excl(reads, writes):
        ex = [b for b in reads if b.excl]
        if ex:
            reads = [b for b in reads if not b.excl]
            writes = list(writes) + [b for b in ex if b not in writes]
        return reads, writes

    def op(self, eng, fn, reads=(), writes=()):
        reads, writes = self._excl(reads, writes)
        toks = self._deps(reads, writes)
        self.cnt[eng] += 1
        tok = (eng, self.sem[eng], self.cnt[eng])
        self._push(eng, fn, toks, (self.sem[eng], 1))
        self._mark(reads, writes, tok)
        return tok

    def dma(self, eng, fn, reads=(), writes=()):
        toks = self._deps(reads, writes)
        if eng == "pool":
            i = self.n_hw + self.dnext["sw"]
            self.dnext["sw"] = (self.dnext["sw"] + 1) % self.n_sw
        else:
            i = self.dnext["hw"]
            self.dnext["hw"] = (self.dnext["hw"] + 1) % self.n_hw
        if self.dcnt[i] > 0:
            toks.append(("dma", self.dsem[i], self.dcnt[i]))
        self.dcnt[i] += 16
        tok = ("dma", self.dsem[i], self.dcnt[i])
        self._push(eng, fn, toks, (self.dsem[i], 16))
        self._mark(reads, writes, tok)
        return tok

    def barrier(self):
        toks = [(e, self.sem[e], self.cnt[e]) for e in ("pe", "act", "dve", "pool") if self.cnt[e] > 0]
        toks += [("dma", self.dsem[i], self.dcnt[i]) for i in range(len(self.dsem)) if self.dcnt[i] > 0]
        for e in self.ENGS:
            need = []
            for kind, sem, val in toks:
                if kind == e:
                    continue
                if self.seen[e].get(id(sem), 0) >= val:
                    continue
                self.seen[e][id(sem)] = val
                need.append((sem, val))
            self.q[e].append((None, need, None))

    def finish(self, eng="sp"):
        toks = [("dma", self.dsem[i], self.dcnt[i]) for i in range(len(self.dsem)) if self.dcnt[i] > 0]
        self.seen[eng] = {}
        self._push(eng, None, toks, None)

    def emit(self):
        nc = self.nc
        with nc.Block() as block:
            def mk(e):
                def body(engobj):
                    for fn, waits, inc in self.q[e]:
                        for sem, val in waits:
                            engobj.wait_ge(sem, val)
                        if fn is not None:
                            ins = fn(engobj)
                            ins.then_inc(inc[0], inc[1])
                return body
            block.tensor(mk("pe"))
            block.scalar(mk("act"))
            block.vector(mk("dve"))
            block.gpsimd(mk("pool"))
            block.sync(mk("sp"))


class T:
    def __init__(self, nc, es, name, shape, dtype, psum=False, nbuf=0):
        if psum:
            self.t = es.enter_context(nc.psum_tensor("p_" + name, shape, dtype))
        else:
            self.t = es.enter_context(nc.sbuf_tensor("t_" + name, shape, dtype))
        self.b = Buf(name)
        self.b.excl = psum
        self.bs = [Buf(name + "_" + str(i)) for i in range(nbuf)]

    def __getitem__(self, k):
        return self.t[k]


class V:
    def __init__(self, ap, name, nbuf=0):
        self.ap = ap
        self.b = Buf(name)
        self.bs = [Buf(name + "_" + str(i)) for i in range(nbuf)]

    def __getitem__(self, k):
        return self.ap[k]


class RV:
    def __init__(self, items):
        self.tiles = items
        self.i = 0

    def get(self):
        t = self.tiles[self.i % len(self.tiles)]
        self.i += 1
        return t


class Rot:
    def __init__(self, nc, es, name, shape, dtype, n, psum=False):
        self.tiles = [T(nc, es, name + str(i), shape, dtype, psum=psum) for i in range(n)]
        self.i = 0

    def get(self):
        t = self.tiles[self.i % len(self.tiles)]
        self.i += 1
        return t


def build_program(dbg=0):
    nc = bass.Bass("TRN2", target_bir_lowering=False)

    def din(name, shape, dt=F32):
        return nc.dram_tensor(name, shape, dt, kind="ExternalInput").ap()

    def dscr(name, shape, dt=F32):
        return nc.dram_tensor(name, shape, dt, kind="Internal").ap()

    x_d = din("x", [2, L, D])
    ctx_d = din("ctx", [2, LC, D])
    cT_d = din("cT", [128, 24])
    adaw_d = din("ada_w", [D, 6 * D])
    adab_d = din("ada_b", [1, 6 * D])
    win_d = din("w_in", [D, 3584])
    convw_d = din("convw", [128, 16])
    convb_d = din("convb", [128, 4])
    gw_d = din("gw", [128, 2048])
    gb_d = din("gb", [128, 16])
    lam_d = din("lam", [128, 8])
    lbl_d = din("lbl", [128, 16])
    ng_d = din("ng", [128, 4])
    wout_d = din("w_out", [D, D])
    lnp_d = din("lnp", [128, 4 * D])
    identF_d = din("identF", [128, 128])
    maskF_d = din("maskF", [128, 128])
    maskB_d = din("maskB", [128, 128])
    resetm_d = din("resetm", [128, 512])
    rw_d = din("rw", [D, 36])
    rb_d = din("rb", [128, 36])
    w1_d = din("w1r", [32 * 128, 8 * 512])
    w3_d = din("w3r", [32 * 128, 8 * 512])
    w2_d = din("w2r", [32 * 128, 4 * 1024])
    triu_d = din("triu", [128, 128])
    thr_d = din("thr", [128, 64 + NBLK])
    iota_d = din("iota", [128, 2])
    out_d = nc.dram_tensor("out", [2, L, D], F32, kind="ExternalOutput").ap()
    if dbg:
        dbg_d = nc.dram_tensor("dbg", [2 * L, D], F32, kind="ExternalOutput").ap()
        dbgs_d = nc.dram_tensor("dbgs", [128, 512], F32, kind="ExternalOutput").ap()

    m_scr = dscr("m_scr", [3, 6 * D])
    mix_scr = dscr("mix_scr", [2, 8, 128, L], BF16)
    h1_scr = dscr("h1_scr", [2 * L, D])
    u2_scr = dscr("u2_scr", [2 * L, D], BF16)
    xs_scr = dscr("xs_scr", [NSLOT, D], BF16)
    ys_scr = dscr("ys_scr", [NSLOT, D])
    zsrc = dscr("zsrc", [32, D], BF16)
    wb_scr = [dscr("w%db_scr" % i, [32 * 128, 4096], BF16) for i in range(3)]

    with ExitStack() as es:
        S = Sched(nc, es)

        def TT(name, shape, dt=F32, **kw):
            return T(nc, es, name, shape, dt, **kw)

        def RR(name, shape, dt, n, **kw):
            return Rot(nc, es, name, shape, dt, n, **kw)

        def mm(out, lhsT, rhs, start, stop, reads, writes, sgc=False):
            S.op("pe", lambda e: e.matmul(out, lhsT, rhs, start=start, stop=stop, skip_group_check=sgc), reads, writes)

        def tr(out, in_, ident, reads, writes):
            S.op("pe", lambda e: e.transpose(out, in_, ident), reads, writes)

        def act(out, in_, func, reads, writes, bias=None, scale=None):
            kw = {}
            if bias is not None:
                kw["bias"] = bias
            if scale is not None:
                kw["scale"] = scale
            S.op("act", lambda e: e.activation(out, in_, func, **kw), reads, writes)

        POOL2DVE = os.environ.get("K_POOL2DVE", "1") == "1"

        def tt(eng, out, in0, in1, op, reads, writes):
            if eng == "pool" and POOL2DVE:
                eng = "dve"
            S.op(eng, lambda e: e.tensor_tensor(out, in0, in1, op), reads, writes)

        def ts(eng, out, in0, s1, s2, op0, op1, reads, writes):
            if s2 is None:
                S.op(eng, lambda e: e.tensor_scalar(out, in0, s1, None, op0), reads, writes)
            else:
                S.op(eng, lambda e: e.tensor_scalar(out, in0, s1, s2, op0, op1), reads, writes)

        def stt(eng, out, in0, sc, in1, op0, op1, reads, writes):
            S.op(eng, lambda e: e.scalar_tensor_tensor(out, in0, sc, in1, op0, op1), reads, writes)

        def cp(eng, out, in_, reads, writes):
            if eng == "act":
                S.op("act", lambda e: e.copy(out, in_), reads, writes)
            else:
                S.op(eng, lambda e: e.tensor_copy(out, in_), reads, writes)

        def dma(eng, out, in_, reads, writes, **kw):
            S.dma(eng, lambda e: e.dma_start(out=out, in_=in_, **kw), reads, writes)

        def zip_run(gens):
            res = [None] * len(gens)
            live = list(range(len(gens)))
            while live:
                for gi in list(live):
                    try:
                        next(gens[gi])
                    except StopIteration as e_:
                        res[gi] = e_.value
                        live.remove(gi)
            return res

        identF = TT("identF", [128, 128])
        identB = TT("identB", [128, 128], BF16)
        onesF = TT("onesF", [128, 128])
        onesB = TT("onesB", [128, 128], BF16)
        resetm = TT("resetm", [128, 512])
        cT = TT("cT", [128, 24])
        scT = TT("scT", [128, 24])
        convw = TT("convw", [128, 16])
        convb = TT("convb", [128, 4])
        gb = TT("gb", [128, 16])
        lam = TT("lam", [128, 8])
        cL = TT("cL", [128, 8])
        lbl = TT("lbl", [128, 16])
        lb = TT("lb", [128, 8])
        oml = TT("oml", [128, 8])
        noml = TT("noml", [128, 8])
        ng = TT("ng", [128, 4])
        gwb = TT("gwb", [128, 2048], BF16)
        modT = TT("modT", [128, 48])
        ones3 = TT("ones3", [1, 4])

        for t_, d_ in ((identF, identF_d), (resetm, resetm_d), (cT, cT_d),
                       (convw, convw_d), (convb, convb_d), (gb, gb_d), (lam, lam_d), (lbl, lbl_d), (ng, ng_d)):
            dma("sp", t_[:], d_, [], [t_.b])
        cp("dve", identB[:], identF[:], [identF.b], [identB.b])
        S.op("dve", lambda e: e.memset(onesF[:], 1.0), [], [onesF.b])
        S.op("dve", lambda e: e.memset(onesB[:], 1.0), [], [onesB.b])
        S.op("dve", lambda e: e.memset(ones3[:], 1.0), [], [ones3.b])
        act(scT[:], cT[:], AF.Silu, [cT.b], [scT.b])
        act(cL[:], lam[:], AF.Exp, [lam.b], [cL.b], scale=-1.0)
        act(cL[:], cL[:], AF.Ln, [cL.b], [cL.b], bias=1.0)
        ts("dve", cL[:], cL[:], -8.0, None, ALU.mult, None, [cL.b], [cL.b])
        tt("dve", lb[:], lbl[:, 0:8], lbl[:, 8:16], ALU.subtract, [lbl.b], [lb.b])
        act(lb[:], lb[:], AF.Sigmoid, [lb.b], [lb.b])
        ts("dve", oml[:], lb[:], -1.0, 1.0, ALU.mult, ALU.add, [lb.b], [oml.b])
        ts("dve", noml[:], oml[:], -1.0, None, ALU.mult, None, [oml.b], [noml.b])

        ARENA_B = 100 * 1024
        BIG = es.enter_context(nc.sbuf_tensor("big", [128, ARENA_B // 4], F32))

        def fview(off, n, name, nbuf=0):
            return V(BIG[:, off // 4:off // 4 + n], name, nbuf)

        def bview(off, n, name, nbuf=0):
            return V(BIG[:, off // 4:off // 4 + n // 2].bitcast(BF16), name, nbuf)

        K64 = 65536
        uT = bview(0, 8 * L, "uT", 32)
        uTv = uT[:].rearrange("p (k t) -> p k t", k=8)
        cuT = bview(K64, 8 * LC, "cuT", 2)
        cuTv = cuT[:].rearrange("p (k t) -> p k t", k=8)
        LOC = K64 + 4096
        WKa = fview(LOC, 4096, "wka", 8)
        WKb = fview(LOC + 16384, 4096, "wkb", 8)
        PS = RR("ps", [128, 512], F32, int(os.environ.get("K_PS", "5")), psum=True)
        POR = RR("po", [128, 512], F32, 7 - int(os.environ.get("K_PS", "5")), psum=True)
        PL = TT("pl", [128, 512], F32, psum=True)

        dma("sp", WKa[:, 0:2048], gw_d, [], [WKa.b])
        cp("pool", gwb[:], WKa[:, 0:2048], [WKa.b], [gwb.b])

        tmpf_raw = es.enter_context(nc.sbuf_tensor("t_tmpf_raw", [128, 14 * 512], F32))
        tmpf = RV([V(tmpf_raw[:, i * 512:(i + 1) * 512], "tmpf%d" % i) for i in range(14)])
        tmph = RV([V(tmpf_raw[:, i * 256:(i + 1) * 256], "tmph%d" % i) for i in range(28)])
        adw = [fview(i * 16384, 4096, "adw%d" % i) for i in range(4)]
        for j in range(12):
            wt = adw[j % 4]
            wv = wt[:].rearrange("p (k n) -> p k n", k=8)
            dma("sp", wv, adaw_d[:, j * 512:(j + 1) * 512].rearrange("(k p) n -> p k n", p=128), [], [wt.b])
            bt = tmpf.get()
            dma("sp", bt[0:1, :], adab_d[0:1, j * 512:(j + 1) * 512], [], [bt.b])
            ps = PS.get()
            for k in range(8):
                mm(ps[0:3, :], scT[:, k * 3:(k + 1) * 3], wv[:, k, :], k == 0, False, [scT.b, wt.b], [ps.b])
            mm(ps[0:3, :], ones3[0:1, 0:3], bt[0:1, :], False, True, [ones3.b, bt.b], [ps.b])
            mt = tmpf.get()
            cp("dve", mt[0:3, :], ps[0:3, :], [ps.b], [mt.b])
            dma("sp", m_scr[:, j * 512:(j + 1) * 512], mt[0:3, :], [mt.b], [])
            if j < 4:
                for q_ in range(4):
                    jj = j * 4 + q_
                    tr(PL[:, jj * 3:jj * 3 + 3], mt[0:3, q_ * 128:(q_ + 1) * 128], identF[0:3, 0:3], [mt.b, identF.b], [PL.b])
                if j == 3:
                    cp("dve", modT[:], PL[:, 0:48], [PL.b], [modT.b])
        S.barrier()

        def mscr_reads():
            return []

        ts("dve", modT[:, 24:48], modT[:, 24:48], 1.0, None, ALU.add, None, [modT.b], [modT.b])

        xin = RR("xin", [128, D], F32, 2)
        wbf = RR("wbf", [128, 8 * 128], BF16, 10)
        tmpb_raw = es.enter_context(nc.sbuf_tensor("t_tmpb_raw", [128, 10 * 512], BF16))
        tmpb = RV([V(tmpb_raw[:, i * 512:(i + 1) * 512], "tmpb%d" % i) for i in range(10)])
        tmphb = RV([V(tmpb_raw[:, i * 256:(i + 1) * 256], "tmphb%d" % i) for i in range(20)])
        small = RR("small", [128, 16], F32, 8)
        Sfin = [TT("Sfin%d" % i, [128, 128]) for i in range(2)]
        SstR = RR("Sst", [128, 9 * 128], F32, 1)
        SallR = RR("Sall", [128, 8 * 128], BF16, 1)
        UsbR = RR("Usb", [128, 8 * 128], F32, 2)
        mask4F = TT("mask4F", [128, 512])
        mask4B = TT("mask4B", [128, 512])
        for q_ in range(4):
            dma("sp", mask4F[:, q_ * 128:(q_ + 1) * 128], maskF_d, [], [mask4F.b])
            dma("sp", mask4B[:, q_ * 128:(q_ + 1) * 128], maskB_d, [], [mask4B.b])
        h0 = TT("h0", [128, 2])
        Vc = TT("Vc", [128, 2 * 128], BF16, nbuf=2)
        stats = RR("stats", [128, 12], F32, 2)

        def load_w(col):
            wb = wbf.get()
            wbv = wb[:].rearrange("p (k n) -> p k n", k=8)
            src = win_d[:, col:col + 128].rearrange("(k p) n -> p k n", p=128)
            S.dma("pool", (lambda o_, i_: (lambda e: e.dma_start(out=o_, in_=i_)))(wbv, src), [], [wb.b])
            return wb

        wq_pending = {}
        pc_state = [0]
        w_f32 = (w1_d, w3_d, w2_d)

        def precast(n_):
            for _ in range(n_):
                k_ = pc_state[0]
                if k_ >= 96:
                    return
                pc_state[0] += 1
                e_, m_ = k_ // 3, k_ % 3
                src = w_f32[m_][e_ * 128:(e_ + 1) * 128, :].rearrange("p (a n) -> p a n", n=2048)
                dst = wb_scr[m_][e_ * 128:(e_ + 1) * 128, :].rearrange("p (a n) -> p a n", n=2048)
                S.dma("pool", (lambda o_, i_: (lambda e: e.dma_start(out=o_, in_=i_)))(dst, src), [], [])

        def rg_cols(c):
            return (c * 128, 512 + c * 128)

        def hg_cols(hd):
            return (1024 + hd * 128, 1536 + hd * 128, 2048 + hd * 128, 2560 + hd * 128, 3072 + hd * 128)

        def prefetch(key, cols):
            wq_pending[key] = [load_w(c_) for c_ in cols]

        def take(key, cols):
            if key not in wq_pending:
                prefetch(key, cols)
            return wq_pending.pop(key)

        def proj_fm(wb, src_v, src_bufs, t0, n, ps):
            wv = wb[:].rearrange("p (k n) -> p k n", k=8)
            for k in range(8):
                mm(ps[:, 0:n], wv[:, k, :], src_v[:, k, t0:t0 + n], k == 0, k == 7, [wb.b] + src_bufs, [ps.b])

        def proj_tm(wb, src_v, src_bufs, t0, ps, c0):
            wv = wb[:].rearrange("p (k n) -> p k n", k=8)
            for k in range(8):
                mm(ps[:, c0:c0 + 128], src_v[:, k, t0:t0 + 128], wv[:, k, :], k == 0, k == 7, [wb.b] + src_bufs, [ps.b])

        OH1 = TT("OH1", [128, 64 * 32], BF16)
        OH2 = TT("OH2", [128, 64 * 32], BF16)
        OH1v = OH1[:].rearrange("p (i e) -> p i e", e=32)
        OH2v = OH2[:].rearrange("p (i e) -> p i e", e=32)
        W1 = TT("W1", [128, 64])
        W2 = TT("W2", [128, 64])
        rw = TT("rw", [128, 8 * 36])
        rwv = rw[:].rearrange("p (k n) -> p k n", k=8)
        dma("sp", rwv, rw_d.rearrange("(k p) n -> p k n", p=128), [], [rw.b])
        rb4 = TT("rb4", [128, 4 * 36])
        for q_ in range(4):
            dma("sp", rb4[:, q_ * 36:(q_ + 1) * 36], rb_d, [], [rb4.b])
        rt = RV([V(tmpf_raw[:, i * 160:(i + 1) * 160], "rt%d" % i) for i in range(8)])
        zrow = TT("zrow", [128, D], BF16)
        S.op("pool", lambda e: e.memset(zrow[:], 0.0), [], [zrow.b])
        zsrc_b = Buf("zsrc")
        dma("sp", zsrc, zrow[0:32, :], [zrow.b], [zsrc_b])
        xs16 = xs_scr.rearrange("(q r j) d -> q r (j d)", q=16, j=32)
        zflat = zsrc.rearrange("(o j) d -> o (j d)", o=1)
        for q_ in range(16):
            dma("sp", xs16[q_], zflat.to_broadcast([NSLOT // (16 * 32), 32 * D]), [zsrc_b], [])

        if os.environ.get('K_VERBOSE'):
            print('SBUF bytes remaining', nc.sbuf_bytes_remaining)
        for b in range(int(os.environ.get('K_NB', '2'))):
            S.barrier()
            def phaseA(src_d, ntile, dst_v, dst_t, r):
                for i in range(ntile):
                    xt = xin.get()
                    dma("sp", xt[:], src_d[i * 128:(i + 1) * 128, :], [], [xt.b])
                    for hlf in range(2):
                        ps = PS.get()
                        for kk in range(4):
                            k = hlf * 4 + kk
                            tr(ps[:, kk * 128:(kk + 1) * 128], xt[:, k * 128:(k + 1) * 128], identF[:], [xt.b, identF.b], [ps.b])
                        for kk in range(4):
                            k = hlf * 4 + kk
                            o_ = dst_v[:, k, i * 128:(i + 1) * 128]
                            i_ = ps[:, kk * 128:(kk + 1) * 128]
                            sc_ = modT[:, (8 + k) * 3 + r:(8 + k) * 3 + r + 1]
                            bi_ = modT[:, k * 3 + r:k * 3 + r + 1]
                            if hlf == 0:
                                act(o_, i_, AF.Identity, [ps.b, modT.b], [dst_t.bs[i]], bias=bi_, scale=sc_)
                            else:
                                ts("dve", o_, i_, sc_, bi_, ALU.mult, ALU.add, [ps.b, modT.b], [dst_t.bs[i]])

            phaseA(ctx_d[b], 2, cuTv, cuT, 2)
            phaseA(x_d[b], 32, uTv, uT, b)

            xc = WKa
            hf = WKb

            def rg_gates(c, dirn, xcb, n, xc_ap, xc_bufs):
                gi = dirn * 8
                pr = PS.get()
                pi = PS.get()
                mm(pr[:, 0:n], gwb[:, (gi + c) * 128:(gi + c + 1) * 128], xcb[:, 0:n], True, True, [gwb.b, xcb.b], [pr.b])
                mm(pi[:, 0:n], gwb[:, (gi + 4 + c) * 128:(gi + 4 + c + 1) * 128], xcb[:, 0:n], True, True, [gwb.b, xcb.b], [pi.b])
                r_ = tmpf.get()
                i_ = tmpf.get()
                act(r_[:, 0:n], pr[:, 0:n], AF.Sigmoid, [pr.b, gb.b], [r_.b], bias=gb[:, gi + c:gi + c + 1])
                act(i_[:, 0:n], pi[:, 0:n], AF.Sigmoid, [pi.b, gb.b], [i_.b], bias=gb[:, gi + 4 + c:gi + 4 + c + 1])
                a_ = tmpf.get()
                act(a_[:, 0:n], r_[:, 0:n], AF.Exp, [r_.b, cL.b], [a_.b], scale=cL[:, dirn * 4 + c:dirn * 4 + c + 1])
                a2 = tmpf.get()
                tt("pool", a2[:, 0:n], a_[:, 0:n], a_[:, 0:n], ALU.mult, [a_.b], [a2.b])
                act(a2[:, 0:n], a2[:, 0:n], AF.Sqrt, [a2.b], [a2.b], bias=1.0, scale=-1.0)
                tt("pool", i_[:, 0:n], i_[:, 0:n], xc_ap, ALU.mult, [i_.b] + xc_bufs, [i_.b])
                tt("dve", i_[:, 0:n], i_[:, 0:n], a2[:, 0:n], ALU.mult, [i_.b, a2.b], [i_.b])
                return a_, i_

            def conv(c, src, n, rows, dst_ap, dst_bufs):
                w_ = n // rows
                sv = src[:, 0:n].rearrange("p (r w) -> p r w", r=rows)
                dv = dst_ap.rearrange("p (r w) -> p r w", r=rows)
                wc = lambda j: convw[:, c * 4 + j:c * 4 + j + 1]
                rd = [src.b, convw.b, convb.b]
                ts("dve", dst_ap, src[:, 0:n], wc(1), convb[:, c:c + 1], ALU.mult, ALU.add, rd, dst_bufs)
                stt("dve", dv[:, :, 1:w_], sv[:, :, 0:w_ - 1], wc(0), dv[:, :, 1:w_], ALU.mult, ALU.add, rd, dst_bufs)
                stt("dve", dv[:, :, 0:w_ - 1], sv[:, :, 1:w_], wc(2), dv[:, :, 0:w_ - 1], ALU.mult, ALU.add, rd, dst_bufs)
                stt("dve", dv[:, :, 0:w_ - 2], sv[:, :, 2:w_], wc(3), dv[:, :, 0:w_ - 2], ALU.mult, ALU.add, rd, dst_bufs)

            for c in range(int(os.environ.get('K_NRG', '4'))):
                w_x, w_g = take(("rg", b, c), rg_cols(c))
                precast(6)
                if c + 1 < 4:
                    prefetch(("rg", b, c + 1), rg_cols(c + 1))
                else:
                    prefetch(("hg", b, 0), hg_cols(0))
                ps = PS.get()
                proj_fm(w_x, cuTv, cuT.bs, 0, LC, ps)
                rx = tmpf.get()
                cp("act", rx[:, 0:LC], ps[:, 0:LC], [ps.b], [rx.b])
                xcc = tmpf.get()
                conv(c, rx, LC, 1, xcc[:, 0:LC], [xcc.b])
                xcb = tmpb.get()
                cp("pool", xcb[:, 0:LC], xcc[:, 0:LC], [xcc.b], [xcb.b])
                for dirn in range(2):
                    a_, u_ = rg_gates(c, dirn, xcb, LC, xcc[:, 0:LC], [xcc.b])
                    hh = tmpf.get()
                    if dirn == 0:
                        S.op("dve", (lambda o, d0, d1: (lambda e: e.tensor_tensor_scan(o, d0, d1, 0.0, ALU.mult, ALU.add)))(
                            hh[:, 0:LC], a_[:, 0:LC], u_[:, 0:LC]), [a_.b, u_.b], [hh.b])
                        cp("dve", h0[:, 0:1], hh[:, LC - 1:LC], [hh.b], [h0.b])
                    else:
                        S.op("dve", (lambda o, d0, d1: (lambda e: e.tensor_tensor_scan(o, d0, d1, 0.0, ALU.mult, ALU.add)))(
                            hh[:, 0:LC][:, ::-1], a_[:, 0:LC][:, ::-1], u_[:, 0:LC][:, ::-1]), [a_.b, u_.b], [hh.b])
                        cp("dve", h0[:, 1:2], hh[:, 0:1], [hh.b], [h0.b])
                def rg_gates_g(dirn, xcb, n, xc_ap, xc_bufs):
                    gi = dirn * 8
                    pr = PS.get()
                    pi = PS.get()
                    mm(pr[:, 0:n], gwb[:, (gi + c) * 128:(gi + c + 1) * 128], xcb[:, 0:n], True, True, [gwb.b, xcb.b], [pr.b])
                    mm(pi[:, 0:n], gwb[:, (gi + 4 + c) * 128:(gi + 4 + c + 1) * 128], xcb[:, 0:n], True, True, [gwb.b, xcb.b], [pi.b])
                    yield
                    r_ = tmpf.get()
                    act(r_[:, 0:n], pr[:, 0:n], AF.Sigmoid, [pr.b, gb.b], [r_.b], bias=gb[:, gi + c:gi + c + 1])
                    yield
                    i_ = tmpf.get()
                    act(i_[:, 0:n], pi[:, 0:n], AF.Sigmoid, [pi.b, gb.b], [i_.b], bias=gb[:, gi + 4 + c:gi + 4 + c + 1])
                    yield
                    a_ = tmpf.get()
                    act(a_[:, 0:n], r_[:, 0:n], AF.Exp, [r_.b, cL.b], [a_.b], scale=cL[:, dirn * 4 + c:dirn * 4 + c + 1])
                    yield
                    a2 = tmpf.get()
                    tt("dve", a2[:, 0:n], a_[:, 0:n], a_[:, 0:n], ALU.mult, [a_.b], [a2.b])
                    tt("dve", i_[:, 0:n], i_[:, 0:n], xc_ap, ALU.mult, [i_.b] + xc_bufs, [i_.b])
                    yield
                    act(a2[:, 0:n], a2[:, 0:n], AF.Sqrt, [a2.b], [a2.b], bias=1.0, scale=-1.0)
                    yield
                    tt("dve", i_[:, 0:n], i_[:, 0:n], a2[:, 0:n], ALU.mult, [i_.b, a2.b], [i_.b])
                    return a_, i_

                def rg_fwd(s):
                    t0 = s * 512
                    ps = PS.get()
                    proj_fm(w_x, uTv, uT.bs[s * 4:(s + 1) * 4], t0, 512, ps)
                    yield
                    rx = tmpf.get()
                    cp("act", rx[:], ps[:], [ps.b], [rx.b])
                    yield
                    conv(c, rx, 512, 8, xc[:, t0:t0 + 512], [xc.bs[s]])
                    yield
                    xcb = tmpb.get()
                    cp("pool", xcb[:], xc[:, t0:t0 + 512], [xc.bs[s]], [xcb.b])
                    yield
                    a_, u_ = yield from rg_gates_g(0, xcb, 512, xc[:, t0:t0 + 512], [xc.bs[s]])
                    yield
                    init = h0[:, 0:1] if s == 0 else hf[:, t0 - 1:t0]
                    ib = [h0.b] if s == 0 else [hf.bs[s - 1]]
                    S.op("dve", (lambda o, d0, d1, ini: (lambda e: e.tensor_tensor_scan(o, d0, d1, ini, ALU.mult, ALU.add)))(
                        hf[:, t0:t0 + 512], a_[:], u_[:], init), [a_.b, u_.b] + ib, [hf.bs[s]])

                hbt = {}

                def rg_bwd(s):
                    t0 = s * 512
                    xcb = tmpb.get()
                    cp("pool", xcb[:], xc[:, t0:t0 + 512], [xc.bs[s]], [xcb.b])
                    yield
                    a_, u_ = yield from rg_gates_g(1, xcb, 512, xc[:, t0:t0 + 512], [xc.bs[s]])
                    yield
                    hb = tmpf.get()
                    hbt[s] = hb
                    init = h0[:, 1:2] if s == 7 else hbt[s + 1][:, 0:1]
                    ib = [h0.b] if s == 7 else [hbt[s + 1].b]
                    S.op("dve", (lambda o, d0, d1, ini: (lambda e: e.tensor_tensor_scan(o, d0, d1, ini, ALU.mult, ALU.add)))(
                        hb[:, ::-1], a_[:, ::-1], u_[:, ::-1], init), [a_.b, u_.b] + ib, [hb.b])
                    yield
                    ps = PS.get()
                    proj_fm(w_g, uTv, uT.bs[s * 4:(s + 1) * 4], t0, 512, ps)
                    yield
                    gl = tmpf.get()
                    act(gl[:], ps[:], AF.Gelu_apprx_tanh, [ps.b], [gl.b])
                    yield
                    hs = tmpf.get()
                    tt("dve", hs[:], hf[:, t0:t0 + 512], hb[:], ALU.add, [hf.bs[s], hb.b], [hs.b])
                    yield
                    ob = tmpb.get()
                    tt("dve", ob[:], hs[:], gl[:], ALU.mult, [hs.b, gl.b], [ob.b])
                    dma("sp", mix_scr[b, c, :, t0:t0 + 512], ob[:], [ob.b], [])

                for s in range(0, 8, 2):
                    zip_run([rg_fwd(s), rg_fwd(s + 1)])
                for s in range(7, -1, -2):
                    zip_run([rg_bwd(s), rg_bwd(s - 1)])

            S.barrier()
            of = fview(LOC, 4096, "of", 16)
            qall = bview(LOC + 16384, L, "qall", 16)
            Vall = bview(LOC + 16384 + 8192, 32 * 128, "Vall", 32)

            def hg_A(hd, dirn, src_v, src_bufs, t0, n, w_z, w_q, w_v, w_hg, with_out, Vt, Vbufs, vcol0, sidx, po, pcol, first, Usb, ucol):
                nch = n // 64
                nw = n // 128
                li = dirn * 4 + hd
                pz = PS.get()
                proj_fm(w_z, src_v, src_bufs, t0, n, pz)
                yield
                sg = tmph.get()
                act(sg[:, 0:n], pz[:, 0:n], AF.Exp, [pz.b], [sg.b], scale=-1.0)
                yield
                act(sg[:, 0:n], sg[:, 0:n], AF.Ln, [sg.b], [sg.b], bias=1.0)
                yield
                act(sg[:, 0:n], sg[:, 0:n], AF.Exp, [sg.b], [sg.b], scale=-1.0)
                yield
                lf = tmph.get()
                act(lf[:, 0:n], sg[:, 0:n], AF.Ln, [sg.b, oml.b, lb.b], [lf.b], bias=lb[:, li:li + 1], scale=oml[:, li:li + 1])
                kk_ = tmph.get()
                ts("dve", kk_[:, 0:n], sg[:, 0:n], noml[:, li:li + 1], oml[:, li:li + 1], ALU.mult, ALU.add,
                   [sg.b, oml.b, noml.b], [kk_.b])
                yield
                Gc = tmph.get()
                S.op("dve", (lambda o, d0, d1: (lambda e: e.tensor_tensor_scan(o, d0, d1, 0.0, ALU.mult, ALU.add)))(
                    Gc[:, 0:n], resetm[:, 0:n], lf[:, 0:n]), [resetm.b, lf.b], [Gc.b])
                yield
                Gv = Gc[:, 0:n].rearrange("p (c t) -> p c t", t=64)
                Glast = Gv[:, :, 63:64].to_broadcast([128, nch, 64])
                dl = small.get()
                act(dl[:, 0:nch], Gc[:, 63:n:64], AF.Exp, [Gc.b], [dl.b])
                e3 = tmph.get()
                e3v = e3[:, 0:n].rearrange("p (c t) -> p c t", t=64)
                if dirn == 0:
                    Hq = Gc
                    tt("dve", e3v, Glast, Gv, ALU.subtract, [Gc.b], [e3.b])
                else:
                    e1 = tmph.get()
                    e1v = e1[:, 0:n].rearrange("p (c t) -> p c t", t=64)
                    tt("dve", e1v, Glast, Gv, ALU.subtract, [Gc.b], [e1.b])
                    tt("pool", e3[:, 0:n], Gc[:, 0:n], lf[:, 0:n], ALU.subtract, [Gc.b, lf.b], [e3.b])
                    yield
                    tt("pool", e1[:, 0:n], e1[:, 0:n], lf[:, 0:n], ALU.add, [e1.b, lf.b], [e1.b])
                    Hq = e1
                yield
                kd = tmphb.get()
                act(e3[:, 0:n], e3[:, 0:n], AF.Exp, [e3.b], [e3.b])
                yield
                tt("pool", kd[:, 0:n], kk_[:, 0:n], e3[:, 0:n], ALU.mult, [kk_.b, e3.b], [kd.b])
                qg = None
                if with_out:
                    kg = tmphb.get()
                    en = tmph.get()
                    act(en[:, 0:n], Hq[:, 0:n], AF.Exp, [Hq.b], [en.b], scale=-1.0)
                    if dirn == 0:
                        pq = PS.get()
                        proj_fm(w_q, src_v, src_bufs, t0, n, pq)
                        yield
                        sq_ = tmph.get()
                        act(sq_[:, 0:n], pq[:, 0:n], AF.Exp, [pq.b], [sq_.b], scale=-1.0)
                        yield
                        act(sq_[:, 0:n], sq_[:, 0:n], AF.Ln, [sq_.b], [sq_.b], bias=1.0)
                        yield
                        act(sq_[:, 0:n], sq_[:, 0:n], AF.Exp, [sq_.b], [sq_.b], scale=-1.0)
                        yield
                        tt("dve", qall[:, t0:t0 + n], sq_[:, 0:n], pq[:, 0:n], ALU.mult, [sq_.b, pq.b], [qall.bs[sidx]])
                    yield
                    tt("pool", kg[:, 0:n], kk_[:, 0:n], en[:, 0:n], ALU.mult, [kk_.b, en.b], [kg.b])
                    ep = tmph.get()
                    act(ep[:, 0:n], Hq[:, 0:n], AF.Exp, [Hq.b], [ep.b])
                    yield
                    qg = tmphb.get()
                    tt("dve", qg[:, 0:n], qall[:, t0:t0 + n], ep[:, 0:n], ALU.mult, [qall.bs[sidx], ep.b], [qg.b])
                if dirn == 0:
                    pv = PS.get()
                    for w in range(nw):
                        proj_tm(w_v, src_v, src_bufs, t0 + w * 128, pv, w * 128)
                    yield
                    cp("act", Vt[:, vcol0:vcol0 + n], pv[:, 0:n], [pv.b], Vbufs)
                yield
                pk = PS.get()
                pkb = pk[:].bitcast(BF16)
                for w in range(nw):
                    tr(pkb[:, w * 128:(w + 1) * 128], kd[:, w * 128:(w + 1) * 128], identB[:], [kd.b, identB.b], [pk.b])
                yield
                kdT = tmphb.get()
                cp("act", kdT[:, 0:n], pkb[:, 0:n], [pk.b], [kdT.b])
                yield
                Uv = Usb[:, ucol:ucol + nch * 128].rearrange("p (w c k) -> p w c k", c=2, k=128)
                pus = [PS.get(), PS.get()]
                for cc in range(2):
                    pu = pus[cc]
                    for w in range(nw):
                        mm(pu[:, w * 128:(w + 1) * 128], kdT[cc * 64:(cc + 1) * 64, w * 128:(w + 1) * 128],
                           Vt[cc * 64:(cc + 1) * 64, vcol0 + w * 128:vcol0 + (w + 1) * 128], True, True, [kdT.b] + Vbufs, [pu.b])
                    yield
                for cc in range(2):
                    cp("act", Uv[:, :, cc, :], pus[cc][:, 0:nw * 128].rearrange("p (w k) -> p w k", k=128), [pus[cc].b], [Usb.b])
                    yield
                if with_out:
                    pa = PS.get()
                    for w in range(nw):
                        mm(pa[:, w * 128:(w + 1) * 128], kg[:, w * 128:(w + 1) * 128], qg[:, w * 128:(w + 1) * 128], True, True,
                           [kg.b, qg.b], [pa.b])
                    yield
                    AT = tmphb.get()
                    msk = mask4F if dirn == 0 else mask4B
                    tt("dve", AT[:, 0:n], pa[:, 0:n], msk[:, 0:n], ALU.mult, [pa.b, msk.b], [AT.b])
                    yield
                    for w in range(nw):
                        mm(po[:, pcol + w * 128:pcol + (w + 1) * 128], Vt[:, vcol0 + w * 128:vcol0 + (w + 1) * 128],
                           AT[:, w * 128:(w + 1) * 128], first and w == 0, False, Vbufs + [AT.b], [po.b], sgc=True)
                return dict(hd=hd, dirn=dirn, src_v=src_v, src_bufs=src_bufs, t0=t0, n=n, nch=nch, w_hg=w_hg, with_out=with_out,
                            dl=dl, Usb=Usb, ucol=ucol, qg=qg, po=po, pcol=pcol, sidx=sidx)

            def hg_B(c_, last_in_bank):
                dirn, n, nch, t0, hd, sidx = c_["dirn"], c_["n"], c_["nch"], c_["t0"], c_["hd"], c_["sidx"]
                dl, Usb, qg, po, pcol, ucol = c_["dl"], c_["Usb"], c_["qg"], c_["po"], c_["pcol"], c_["ucol"]
                order = list(range(nch)) if dirn == 0 else list(range(nch - 1, -1, -1))
                Sst = SstR.get()
                cp("pool", Sst[:, 0:128], Sfin[dirn][:], [Sfin[dirn].b], [Sst.b])
                yield
                for i, c in enumerate(order):
                    stt("dve", Sst[:, (i + 1) * 128:(i + 2) * 128], Sst[:, i * 128:(i + 1) * 128], dl[:, c:c + 1],
                        Usb[:, ucol + c * 128:ucol + (c + 1) * 128], ALU.mult, ALU.add, [Sst.b, dl.b, Usb.b], [Sst.b])
                    yield
                cp("pool", Sfin[dirn][:], Sst[:, nch * 128:(nch + 1) * 128], [Sst.b], [Sfin[dirn].b])
                if not c_["with_out"]:
                    return
                Sall = SallR.get()
                cp("act", Sall[:, 0:nch * 128], Sst[:, 0:nch * 128], [Sst.b], [Sall.b])
                yield
                for i, c in enumerate(order):
                    mm(po[:, pcol + c * 64:pcol + (c + 1) * 64], Sall[:, i * 128:(i + 1) * 128], qg[:, c * 64:(c + 1) * 64], False,
                       last_in_bank and i == nch - 1, [Sall.b, qg.b], [po.b], sgc=True)
                yield
                if dirn == 0:
                    cp("act", of[:, t0:t0 + n], po[:, pcol:pcol + n], [po.b], [of.bs[sidx]])
                    yield
                return

            def get512():
                if tmph.i % 2:
                    tmph.i += 1
                i_ = tmph.i % len(tmph.tiles)
                a_ = tmph.get()
                b_ = tmph.get()
                return tmpf_raw[:, i_ * 256:i_ * 256 + 512], [a_.b, b_.b]

            def get512b():
                if tmphb.i % 2:
                    tmphb.i += 1
                i_ = tmphb.i % len(tmphb.tiles)
                a_ = tmphb.get()
                b_ = tmphb.get()
                return tmpb_raw[:, i_ * 256:i_ * 256 + 512], [a_.b, b_.b]

            def hg_epi(hd, po, t0, sidxs, w_hg):
                n = 512
                src_bufs = uT.bs[t0 // 128:t0 // 128 + 4]
                ofb = [of.bs[i_] for i_ in sidxs]
                osum, ob_ = get512()
                tt("dve", osum, of[:, t0:t0 + n], po[:, 0:n], ALU.add, ofb + [po.b], ob_)
                yield
                sq, sqb = get512()
                tt("pool", sq, osum, osum, ALU.mult, ob_, sqb)
                yield
                pn = PS.get()
                mm(pn[:, 0:n], onesF[:], sq, True, True, [onesF.b] + sqb, [pn.b])
                yield
                act(sq, pn[:, 0:n], AF.Ln, [pn.b], sqb, bias=EPS, scale=1.0 / 128.0)
                yield
                act(sq, sq, AF.Exp, sqb, sqb, scale=-0.5)
                yield
                tt("pool", osum, osum, sq, ALU.mult, ob_ + sqb, ob_)
                yield
                ph = PS.get()
                proj_fm(w_hg, uTv, src_bufs, t0, n, ph)
                yield
                sl, slb = get512()
                act(sl, ph[:, 0:n], AF.Exp, [ph.b], slb, scale=-1.0)
                yield
                act(sl, sl, AF.Ln, slb, slb, bias=1.0)
                yield
                act(sl, sl, AF.Exp, slb, slb, scale=-1.0)
                yield
                tt("dve", sl, sl, ph[:, 0:n], ALU.mult, slb + [ph.b], slb)
                yield
                ob, obb = get512b()
                stt("dve", ob, sl, ng[:, hd:hd + 1], osum, ALU.mult, ALU.mult, slb + [ng.b] + ob_, obb)
                dma("sp", mix_scr[b, 4 + hd, :, t0:t0 + n], ob, obb, [])

            def zip_run(gens):
                res = [None] * len(gens)
                live = list(range(len(gens)))
                while live:
                    for gi in list(live):
                        try:
                            next(gens[gi])
                        except StopIteration as e_:
                            res[gi] = e_.value
                            live.remove(gi)
                return res

            NSUB = 16
            for hd in range(int(os.environ.get('K_NHG', '4'))):
                w_q, w_zf, w_zb, w_v, w_hg = take(("hg", b, hd), hg_cols(hd))
                precast(6)
                if hd + 1 < 4:
                    prefetch(("hg", b, hd + 1), hg_cols(hd + 1))
                for dirn in range(2):
                    S.op("pool", (lambda o: (lambda e: e.memset(o, 0.0)))(Sfin[dirn][:]), [], [Sfin[dirn].b])

                def lat(dirn, s__, po, pcol, first, ub_):
                    return hg_A(hd, dirn, uTv, uT.bs[s__ * 2:(s__ + 1) * 2], s__ * 256, 256, w_zf if dirn == 0 else w_zb, w_q, w_v, w_hg,
                                True, Vall, Vall.bs[s__ * 2:(s__ + 1) * 2], s__ * 256, s__, po, pcol, first, ub_, pcol * 2)

                pairs = []
                pairs.append(lambda: [hg_A(hd, 0, cuTv, cuT.bs, 0, LC, w_zf, w_q, w_v, w_hg, False, Vc, [Vc.b], 0, 0, None, 0, False, UsbR.get(), 0)])
                pairs.append(lambda: [hg_A(hd, 1, cuTv, cuT.bs, 0, LC, w_zb, w_q, w_v, w_hg, False, Vc, [Vc.b], 0, 0, None, 0, False, UsbR.get(), 0)])
                nsub = int(os.environ.get('K_NS', str(NSUB)))
                for p_ in range(nsub // 2):
                    def mkp(dirn, sa, sb):
                        def f():
                            po = POR.get()
                            ub_ = UsbR.get()
                            return [lat(dirn, sa, po, (sa % 2) * 256, True, ub_), lat(dirn, sb, po, (sb % 2) * 256, False, ub_)]
                        return f
                    pairs.append(mkp(0, 2 * p_, 2 * p_ + 1))
                for p_ in range(nsub // 2 - 1, -1, -1):
                    pairs.append(mkp(1, 2 * p_ + 1, 2 * p_))
                def gen_B(prev_):
                    for k_, c_ in enumerate(prev_):
                        yield from hg_B(c_, k_ == len(prev_) - 1)

                def gen_E(prev_):
                    t0_ = min(c_["t0"] for c_ in prev_)
                    yield from hg_epi(hd, prev_[0]["po"], t0_, [c_["sidx"] for c_ in prev_], w_hg)

                def needs_epi(prev_):
                    return prev_ is not None and prev_[0]["with_out"] and prev_[0]["dirn"] == 1
                prev = zip_run(pairs[0]())
                prevE = None
                for pf in pairs[1:]:
                    gens = pf()
                    extra = [gen_B(prev)] + ([gen_E(prevE)] if prevE is not None else [])
                    res = zip_run(gens + extra)
                    prevE = prev if needs_epi(prev) else None
                    prev = res[:len(gens)]
                zip_run([gen_B(prev)] + ([gen_E(prevE)] if prevE is not None else []))
                if needs_epi(prev):
                    zip_run([gen_E(prev)])

            S.barrier()
            woutb = bview(0, 8 * D, "woutb")
            woutv = woutb[:].rearrange("p (k n) -> p k n", k=8)
            wost = fview(16384, 4096, "wost")
            bcast = [fview(32768 + i * 4096, D, "bc%d" % i) for i in range(3)]
            lnp = fview(32768 + 12288, 2 * D, "lnp")
            dma("sp", lnp[:], lnp_d[:, 0:2 * D], [], [lnp.b])
            for j, lo in enumerate((2048, 3072, 4096)):
                dma("sp", bcast[j][:], m_scr[b:b + 1, lo:lo + D].partition_broadcast(128), [], [bcast[j].b])
            for hlf in range(2):
                wv = wost[:].rearrange("p (k n) -> p k n", k=8)
                dma("sp", wv, wout_d[:, hlf * 512:(hlf + 1) * 512].rearrange("(k p) n -> p k n", p=128), [], [wost.b])
                tt("pool", woutv[:, :, hlf * 512:(hlf + 1) * 512], wv,
                   bcast[0][:, hlf * 512:(hlf + 1) * 512].unsqueeze(1).to_broadcast([128, 8, 512]), ALU.mult,
                   [wost.b, bcast[0].b], [woutb.b])
            ts("dve", bcast[2][:], bcast[2][:], 1.0, None, ALU.add, None, [bcast[2].b], [bcast[2].b])
            tt("dve", bcast[0][:], lnp[:, 0:D], bcast[2][:], ALU.mult, [lnp.b, bcast[2].b, woutb.b], [bcast[0].b])
            tt("dve", bcast[2][:], lnp[:, D:2 * D], bcast[2][:], ALU.mult, [lnp.b, bcast[2].b], [bcast[2].b])
            tt("dve", bcast[1][:], bcast[1][:], bcast[2][:], ALU.add, [bcast[1].b, bcast[2].b], [bcast[1].b])
            S.barrier()

            class _R:
                def __init__(self, items):
                    self.items = items
                    self.i = 0

                def get(self):
                    t = self.items[self.i % len(self.items)]
                    self.i += 1
                    return t
            mixin = _R([bview(53248 + i * 8192, 8 * 512, "mixin%d" % i) for i in range(2)])
            zt = _R([fview(K64 + 4096 + i * 4096, D, "zt%d" % i) for i in range(6)]
                    + [fview(28672, D, "zt6"), fview(32768 + 8192, D, "zt7")])
            PSC = RV(PS.tiles + POR.tiles)
            xck = [fview(K64 + 4096 + 6 * 4096 + i * 4096, D, "xck%d" % i) for i in range(2)] + xin.tiles

            def c_load_x(i_):
                xt_ = xck[i_ % 4]
                dma("sp", xt_[:], x_d[b, i_ * 128:(i_ + 1) * 128, :], [], [xt_.b])
            for i_ in range(4):
                c_load_x(i_)

            def c_load_mix(g_):
                mi_ = mixin.items[g_ % 2]
                dma("sp", mi_[:].rearrange("p (c t) -> p c t", c=8),
                    mix_scr[b, :, :, g_ * 512:(g_ + 1) * 512].rearrange("c p t -> p c t"), [], [mi_.b])
            c_load_mix(0)
            u2b = _R([bview(16384 + i * 2048, D, "u2b%d" % i) for i in range(2)])
            u2T = _R([fview(16384 + 4096 + i * 4096, D, "u2T%d" % i) for i in range(2)])
            for g in range(8):
                mi = mixin.items[g % 2]
                miv = mi[:].rearrange("p (c t) -> p c t", c=8)
                if g + 1 < 8:
                    c_load_mix(g + 1)
                def c_tile(ii):
                    i = g * 4 + ii
                    row0 = b * L + i * 128
                    xt = xck[i % 4]
                    z = zt.get()
                    for hlf in range(2):
                        ps_ = PSC.get()
                        for c in range(8):
                            mm(ps_[:], miv[:, c, ii * 128:(ii + 1) * 128], woutv[:, c, hlf * 512:(hlf + 1) * 512],
                               c == 0, c == 7, [mi.b, woutb.b], [ps_.b])
                        yield
                        stt("dve", z[:, hlf * 512:(hlf + 1) * 512], xt[:, hlf * 512:(hlf + 1) * 512], ALPHA, ps_[:],
                            ALU.mult, ALU.add, [xt.b, ps_.b], [z.b])
                        yield
                    if i + 4 < 32:
                        c_load_x(i + 4)
                    mv = ln_stats(S, z, stats, small, act)
                    yield
                    act(z[:], z[:], AF.Identity, [z.b, mv.b], [z.b], bias=mv[:, 3:4], scale=mv[:, 2:3])
                    yield
                    u2 = zt.get()
                    tt("dve", u2[:], z[:], bcast[0][:], ALU.mult, [z.b, bcast[0].b], [u2.b])
                    yield
                    tt("dve", u2[:], u2[:], bcast[1][:], ALU.add, [u2.b, bcast[1].b], [u2.b])
                    yield
                    tt("dve", z[:], z[:], lnp[:, 0:D], ALU.mult, [z.b, lnp.b], [z.b])
                    yield
                    tt("dve", z[:], z[:], lnp[:, D:2 * D], ALU.add, [z.b, lnp.b], [z.b])
                    dma("sp", h1_scr[row0:row0 + 128, :], z[:], [z.b], [])
                    if dbg == 1:
                        dma("sp", dbg_d[row0:row0 + 128, :], z[:], [z.b], [])
                    yield
                    ub = u2b.get()
                    cp("act", ub[:], u2[:], [u2.b], [ub.b])
                    dma("sp", u2_scr[row0:row0 + 128, :], ub[:], [ub.b], [])
                    u2s[ii] = u2

                u2s = [None] * 4
                zip_run([c_tile(ii_) for ii_ in range(4)])
                for ii in range(4):
                    u2 = u2s[ii]
                    uT2 = u2T.get()
                    uT2v = uT2[:].rearrange("p (k t) -> p k t", k=8)
                    for hlf in range(2):
                        pt = PSC.get()
                        for kk in range(4):
                            k = hlf * 4 + kk
                            tr(pt[:, kk * 128:(kk + 1) * 128], u2[:, k * 128:(k + 1) * 128], identF[:], [u2.b, identF.b], [pt.b])
                        if hlf == 0:
                            cp("act", uT2[:, 0:512], pt[:], [pt.b], [uT2.b])
                        else:
                            cp("dve", uT2[:, 512:1024], pt[:], [pt.b], [uT2.b])
                    for k in range(8):
                        mm(PL[:, ii * 36:(ii + 1) * 36], uT2v[:, k, :], rwv[:, k, :], k == 0, k == 7, [uT2.b, rw.b], [PL.b])
                pl = PL
                i0 = g * 4 + b * 32
                lg = rt.get()
                tt("dve", lg[:, 0:144], pl[:, 0:144], rb4[:], ALU.add, [pl.b, rb4.b], [lg.b])
                lgv = lg[:, 0:144].rearrange("p (i n) -> p i n", n=36)
                gmax = small.get()
                S.op("dve", (lambda o, i_: (lambda e: e.tensor_reduce(o, i_, AX.X, ALU.max)))(gmax[:, 0:4], lgv[:, :, 0:4]), [lg.b], [gmax.b])
                gsh = rt.get()
                gshv = gsh[:, 0:16].rearrange("p (i n) -> p i n", n=4)
                tt("dve", gshv, lgv[:, :, 0:4], gmax[:, 0:4].unsqueeze(2).to_broadcast([128, 4, 4]), ALU.subtract, [lg.b, gmax.b], [gsh.b])
                gex = rt.get()
                act(gex[:, 0:16], gsh[:, 0:16], AF.Exp, [gsh.b], [gex.b])
                S.op("dve", (lambda o, i_: (lambda e: e.tensor_reduce(o, i_, AX.X, ALU.add)))(
                    gmax[:, 4:8], gex[:, 0:16].rearrange("p (i n) -> p i n", n=4)), [gex.b], [gmax.b])
                S.op("dve", (lambda o, i_: (lambda e: e.reciprocal(o, i_)))(gmax[:, 8:12], gmax[:, 4:8]), [gmax.b], [gmax.b])
                pen = rt.get()
                ts("dve", pen[:, 0:16], gsh[:, 0:16], 0.0, None, ALU.is_equal, None, [gsh.b], [pen.b])
                ts("dve", pen[:, 0:16], pen[:, 0:16], -1.0, 1e30, ALU.add, ALU.mult, [pen.b], [pen.b])
                elm = rt.get()
                for j in range(4):
                    tt("dve", elm[:, j * 32:(j + 1) * 32].rearrange("p (g e) -> p g e", e=8),
                       lgv[:, j, 4:36].rearrange("p (g e) -> p g e", e=8),
                       pen[:, j * 4:(j + 1) * 4].unsqueeze(2).to_broadcast([128, 4, 8]), ALU.add, [lg.b, pen.b], [elm.b])
                elmv = elm[:, 0:128].rearrange("p (i e) -> p i e", e=32)
                m12 = small.get()
                S.op("dve", (lambda o, i_: (lambda e: e.tensor_reduce(o, i_, AX.X, ALU.max)))(m12[:, 0:4], elmv), [elm.b], [m12.b])
                oh1 = rt.get()
                oh1v = oh1[:, 0:128].rearrange("p (i e) -> p i e", e=32)
                tt("dve", oh1v, elmv, m12[:, 0:4].unsqueeze(2).to_broadcast([128, 4, 32]), ALU.is_equal, [elm.b, m12.b], [oh1.b])
                elm2 = rt.get()
                stt("dve", elm2[:, 0:128], oh1[:, 0:128], -1e30, elm[:, 0:128], ALU.mult, ALU.add, [oh1.b, elm.b], [elm2.b])
                elm2v = elm2[:, 0:128].rearrange("p (i e) -> p i e", e=32)
                S.op("dve", (lambda o, i_: (lambda e: e.tensor_reduce(o, i_, AX.X, ALU.max)))(m12[:, 4:8], elm2v), [elm2.b], [m12.b])
                oh2 = rt.get()
                oh2v = oh2[:, 0:128].rearrange("p (i e) -> p i e", e=32)
                tt("dve", oh2v, elm2v, m12[:, 4:8].unsqueeze(2).to_broadcast([128, 4, 32]), ALU.is_equal, [elm2.b, m12.b], [oh2.b])
                cp("pool", OH1[:, i0 * 32:(i0 + 4) * 32], oh1[:, 0:128], [oh1.b], [OH1.b])
                cp("pool", OH2[:, i0 * 32:(i0 + 4) * 32], oh2[:, 0:128], [oh2.b], [OH2.b])
                tt("dve", m12[:, 8:12], m12[:, 0:4], m12[:, 4:8], ALU.subtract, [m12.b], [m12.b])
                act(m12[:, 8:12], m12[:, 8:12], AF.Sigmoid, [m12.b], [m12.b])
                tt("dve", W1[:, i0:i0 + 4], m12[:, 8:12], gmax[:, 8:12], ALU.mult, [m12.b, gmax.b], [W1.b])
                tt("dve", W2[:, i0:i0 + 4], gmax[:, 8:12], W1[:, i0:i0 + 4], ALU.subtract, [gmax.b, W1.b], [W2.b])

        def moe_phase():
            precast(96)
            S.barrier()
            PSM = RV(PS.tiles + POR.tiles + [PL])
            o = [0]

            def af(n, name, nbuf=0):
                v = fview(o[0], n, name, nbuf)
                o[0] += n * 4
                return v

            def ab(n, name, nbuf=0):
                v = bview(o[0], n, name, nbuf)
                o[0] += n * 2
                return v
            DST1 = V(BIG[:, o[0] // 4:o[0] // 4 + 64].bitcast(I32), "DST1")
            o[0] += 256
            DST2 = V(BIG[:, o[0] // 4:o[0] // 4 + 64].bitcast(I32), "DST2")
            o[0] += 256
            WIDX = V(BIG[:, o[0] // 4:o[0] // 4 + NBLK].bitcast(I32), "WIDX")
            o[0] += NBLK * 4
            lnp2 = af(2 * D, "lnp2")
            g2b = af(D, "g2b")
            meta_end = o[0]
            SELb = ab(2048, "SELb")
            PRE = af(2048, "PRE")
            TOT = af(2048, "TOT")
            BASE = af(2048, "BASE")
            DD = af(2048, "DD")
            CMPB = af(NBLK * 32, "CMPB")
            triuF = af(128, "triuF")
            triuB = ab(128, "triuB")
            thr = af(64 + NBLK, "thr")
            iot = af(2, "iot")
            CNT = af(32, "CNT")
            NBK = af(32, "NBK")
            PEND = af(32, "PEND")
            PST = af(32, "PST")
            ones32 = af(32, "ones32")
            D1f = af(64, "D1f")
            D2f = af(64, "D2f")
            BLKE = af(NBLK, "BLKE")
            WIDXf = af(NBLK, "WIDXf")
            SAME = af(NBLK, "SAME")
            tmp_end = o[0]

            dma("sp", triuF[:], triu_d, [], [triuF.b])
            dma("sp", thr[:], thr_d, [], [thr.b])
            dma("sp", iot[:], iota_d, [], [iot.b])
            dma("sp", lnp2[:], lnp_d[:, 2 * D:4 * D], [], [lnp2.b])
            cp("dve", triuB[:], triuF[:], [triuF.b], [triuB.b])
            S.op("dve", lambda e: e.memset(ones32[:], 1.0), [], [ones32.b])
            tt("dve", SELb[:], OH1[:], OH2[:], ALU.add, [OH1.b, OH2.b], [SELb.b])
            for j in range(4):
                pp = PSM.get()
                mm(pp[:], triuB[:], SELb[:, j * 512:(j + 1) * 512], True, True, [triuB.b, SELb.b], [pp.b])
                cp("act", PRE[:, j * 512:(j + 1) * 512], pp[:], [pp.b], [PRE.b])
                pq = PSM.get()
                mm(pq[:], onesB[:], SELb[:, j * 512:(j + 1) * 512], True, True, [onesB.b, SELb.b], [pq.b])
                cp("dve", TOT[:, j * 512:(j + 1) * 512], pq[:], [pq.b], [TOT.b])
            S.op("dve", lambda e: e.memset(BASE[:, 0:32], 0.0), [], [BASE.b])
            for i in range(1, 64):
                tt("dve", BASE[:, i * 32:(i + 1) * 32], BASE[:, (i - 1) * 32:i * 32], TOT[:, (i - 1) * 32:i * 32], ALU.add,
                   [BASE.b, TOT.b], [BASE.b])
            tt("dve", CNT[:], BASE[:, 63 * 32:64 * 32], TOT[:, 63 * 32:64 * 32], ALU.add, [BASE.b, TOT.b], [CNT.b])
            cmpv = CMPB[:, 0:32 * 64].rearrange("p (e m) -> p e m", m=64)
            tt("dve", cmpv, CNT[:].unsqueeze(2).to_broadcast([128, 32, 64]), thr[:, 0:64].unsqueeze(1).to_broadcast([128, 32, 64]),
               ALU.is_gt, [CNT.b, thr.b], [CMPB.b])
            S.op("dve", lambda e: e.tensor_reduce(NBK[:], cmpv, AX.X, ALU.add), [CMPB.b], [NBK.b])
            S.op("dve", lambda e: e.tensor_tensor_scan(PEND[:], ones32[:], NBK[:], 0.0, ALU.mult, ALU.add), [ones32.b, NBK.b], [PEND.b])
            tt("dve", PST[:], PEND[:], NBK[:], ALU.subtract, [PEND.b, NBK.b], [PST.b])
            ts("dve", PST[:], PST[:], float(BLK), None, ALU.mult, None, [PST.b], [PST.b])
            ts("dve", PEND[:], PEND[:], float(BLK), None, ALU.mult, None, [PEND.b], [PEND.b])
            tt("dve", DD[:], PRE[:], BASE[:], ALU.add, [PRE.b, BASE.b], [DD.b])
            ddv = DD[:].rearrange("p (i e) -> p i e", e=32)
            tt("dve", ddv, ddv, PST[:].unsqueeze(1).to_broadcast([128, 64, 32]), ALU.add, [DD.b, PST.b], [DD.b])
            tmpv = PRE[:].rearrange("p (i e) -> p i e", e=32)
            tt("dve", PRE[:], DD[:], OH1[:], ALU.mult, [DD.b, OH1.b], [PRE.b])
            S.op("dve", lambda e: e.tensor_reduce(D1f[:], tmpv, AX.X, ALU.add), [PRE.b], [D1f.b])
            tt("dve", PRE[:], DD[:], OH2[:], ALU.mult, [DD.b, OH2.b], [PRE.b])
            S.op("dve", lambda e: e.tensor_reduce(D2f[:], tmpv, AX.X, ALU.add), [PRE.b], [D2f.b])
            cp("dve", DST1[:], D1f[:], [D1f.b], [DST1.b])
            cp("dve", DST2[:], D2f[:], [D2f.b], [DST2.b])
            cbv = CMPB[:].rearrange("p (j e) -> p j e", e=32)
            tt("dve", cbv, PEND[:].unsqueeze(1).to_broadcast([128, NBLK, 32]),
               thr[:, 64:64 + NBLK].unsqueeze(2).to_broadcast([128, NBLK, 32]), ALU.is_le, [PEND.b, thr.b], [CMPB.b])
            S.op("dve", lambda e: e.tensor_reduce(BLKE[:], cbv, AX.X, ALU.add), [CMPB.b], [BLKE.b])
            ts("dve", BLKE[:], BLKE[:], 31.0, None, ALU.min, None, [BLKE.b], [BLKE.b])
            ts("dve", WIDXf[:], BLKE[:], 128.0, iot[:, 0:1], ALU.mult, ALU.add, [BLKE.b, iot.b], [WIDXf.b])
            S.op("dve", lambda e: e.memset(SAME[:], 0.0), [], [SAME.b])
            tt("dve", SAME[:, NWB:NBLK], BLKE[:, NWB:NBLK], BLKE[:, 0:NBLK - NWB], ALU.is_equal, [BLKE.b], [SAME.b])
            stt("dve", WIDXf[:], SAME[:], 1.0e6, WIDXf[:], ALU.mult, ALU.add, [SAME.b, WIDXf.b], [WIDXf.b])
            cp("dve", WIDX[:], WIDXf[:], [WIDXf.b], [WIDX.b])

            if dbg == 2:
                dma("sp", dbgs_d[:, 0:64], W1[:], [W1.b], [])
                dma("sp", dbgs_d[:, 64:128], W2[:], [W2.b], [])
                dma("sp", dbgs_d[:, 128:192], D1f[:], [D1f.b], [])
                dma("sp", dbgs_d[:, 192:256], D2f[:], [D2f.b], [])
                dma("sp", dbgs_d[:, 256:256 + NBLK], BLKE[:], [BLKE.b], [])
                dma("sp", dbgs_d[:, 416:448], CNT[:], [CNT.b], [])
                dma("sp", dbgs_d[:, 448:480], PST[:], [PST.b], [])
            xrow = [bview(tmp_end + i * 2048, D, "xrow%d" % i) for i in range(4)]
            for i in range(64):
                xr = xrow[i % 4]
                dma("sp", xr[:], u2_scr[i * 128:(i + 1) * 128, :], [], [xr.b])
                for dst in (DST1, DST2):
                    S.dma("pool", (lambda x_, d_, i_: (lambda e: e.indirect_dma_start(
                        out=xs_scr, out_offset=bass.IndirectOffsetOnAxis(ap=d_[:, i_:i_ + 1], axis=0),
                        in_=x_[:], in_offset=None)))(xr, dst, i), [xr.b, dst.b], [])
            S.barrier()

            wo = meta_end
            WB = []
            for i in range(NWB):
                WB.append((bview(wo, 4096, "w1b%d" % i), bview(wo + 8192, 4096, "w3b%d" % i), bview(wo + 16384, 4096, "w2b%d" % i)))
                wo += 24576
            xbk = [bview(wo + i * 2048, D, "xbk%d" % i) for i in range(3)]
            wo += 3 * 2048
            xTk = [bview(wo + i * 2048, D, "xTk%d" % i) for i in range(2)]
            wo += 2 * 2048
            hidk = [bview(wo + i * 1024, 512, "hidk%d" % i) for i in range(2)]
            wo += 2 * 1024
            sak = [fview(wo + i * 2048, 512, "sak%d" % i) for i in range(2)]
            wo += 2 * 2048
            yk = [fview(wo + i * 4096, D, "yk%d" % i) for i in range(2)]
            wo += 2 * 4096
            assert wo <= ARENA_B, wo

            regs = {}

            def load_blk_w(j):
                wb = WB[j % NWB]
                for t_, d_ in zip(wb, wb_scr):
                    def mk(t__, d__, j_):
                        def f(e):
                            if "bc" not in regs:
                                regs["bc"] = e.to_reg(4095)
                            return e.indirect_dma_start(
                                out=t__[:], out_offset=None, in_=d__,
                                in_offset=bass.IndirectOffsetOnAxis(ap=WIDX[:, j_:j_ + 1], axis=0),
                                bounds_check=regs["bc"], oob_is_err=False)
                        return f
                    S.dma("pool", mk(t_, d_, j), [WIDX.b], [t_.b])

            def load_blk_x(j):
                xb_ = xbk[j % 3]
                dma("sp", xb_[:], xs_scr[j * BLK:(j + 1) * BLK, :], [], [xb_.b])

            load_blk_w(0)
            load_blk_x(0)
            load_blk_x(1)
            def blk_transpose(j_):
                xb__ = xbk[j_ % 3]
                xT_ = xTk[j_ % 2]
                pt = PSM.get()
                ptb = pt[:].bitcast(BF16)
                for k in range(8):
                    tr(ptb[:, k * 128:(k + 1) * 128], xb__[:, k * 128:(k + 1) * 128], identB[:], [xb__.b, identB.b], [pt.b])
                return pt, ptb, xT_

            pend = blk_transpose(0)
            cp("act", pend[2][:], pend[1][:, 0:1024], [pend[0].b], [pend[2].b])
            for j in range(NBLK):
                if j + 1 < NBLK:
                    load_blk_w(j + 1)
                if j + 2 < NBLK:
                    load_blk_x(j + 2)
                w1b, w3b, w2b = WB[j % NWB]
                w1v = w1b[:].rearrange("p (k n) -> p k n", k=8)
                w3v = w3b[:].rearrange("p (k n) -> p k n", k=8)
                w2v = w2b[:].rearrange("p (k n) -> p k n", k=4)
                xT = xTk[j % 2]
                xTv = xT[:].rearrange("p (k t) -> p k t", k=8)
                pa = PSM.get()
                pb_ = PSM.get()
                for ht in range(4):
                    for k in range(8):
                        mm(pa[:, ht * 128:(ht + 1) * 128], w1v[:, k, ht * 128:(ht + 1) * 128], xTv[:, k, :], k == 0, k == 7,
                           [w1b.b, xT.b], [pa.b])
                    for k in range(8):
                        mm(pb_[:, ht * 128:(ht + 1) * 128], w3v[:, k, ht * 128:(ht + 1) * 128], xTv[:, k, :], k == 0, k == 7,
                           [w3b.b, xT.b], [pb_.b])
                if j + 1 < NBLK:
                    pend = blk_transpose(j + 1)
                sa = sak[j % 2]
                act(sa[:], pa[:], AF.Silu, [pa.b], [sa.b])
                hid = hidk[j % 2]
                tt("dve", hid[:], sa[:], pb_[:], ALU.mult, [sa.b, pb_.b], [hid.b])
                if j + 1 < NBLK:
                    cp("act", pend[2][:], pend[1][:, 0:1024], [pend[0].b], [pend[2].b])
                hv = hid[:].rearrange("p (k t) -> p k t", k=4)
                yy = yk[j % 2]
                for hlf in range(2):
                    py = PSM.get()
                    for ht in range(4):
                        mm(py[:], hv[:, ht, :], w2v[:, ht, hlf * 512:(hlf + 1) * 512], ht == 0, ht == 3, [hid.b, w2b.b], [py.b])
                    if hlf == 0:
                        cp("act", yy[:, 0:512], py[:], [py.b], [yy.b])
                    else:
                        cp("dve", yy[:, 512:1024], py[:], [py.b], [yy.b])
                dma("sp", ys_scr[j * BLK:(j + 1) * BLK, :], yy[:], [yy.b], [])
            S.barrier()

            fo = meta_end
            NF = 4
            y1k = [fview(fo + i * 4096, D, "y1k%d" % i) for i in range(NF)]
            y2k = [fview(fo + NF * 4096 + i * 4096, D, "y2k%d" % i) for i in range(NF)]
            h1k = [fview(fo + 2 * NF * 4096 + i * 4096, D, "h1k%d" % i) for i in range(NF)]
            assert fo + 3 * NF * 4096 <= ARENA_B
            g2bs = [g2b, fview(fo + 3 * NF * 4096, D, "g2b1")]
            assert fo + 3 * NF * 4096 + 4096 <= ARENA_B
            for bb in range(2):
                dma("sp", g2bs[bb][:], m_scr[bb:bb + 1, 5120:6144].partition_broadcast(128), [], [g2bs[bb].b])

            def fin_loads(i):
                for yt, dst in ((y1k[i % NF], DST1), (y2k[i % NF], DST2)):
                    S.dma("pool", (lambda y_, d_, i_: (lambda e: e.indirect_dma_start(
                        out=y_[:], out_offset=None, in_=ys_scr,
                        in_offset=bass.IndirectOffsetOnAxis(ap=d_[:, i_:i_ + 1], axis=0))))(yt, dst, i), [dst.b], [yt.b])
                dma("sp", h1k[i % NF][:], h1_scr[i * 128:(i + 1) * 128, :], [], [h1k[i % NF].b])

            for i in range(NF - 1):
                fin_loads(i)
            for i in range(64):
                bb = i // 32
                if i + NF - 1 < 64:
                    fin_loads(i + NF - 1)
                y1 = y1k[i % NF]
                y2 = y2k[i % NF]
                hh = h1k[i % NF]
                act(y1[:], y1[:], AF.Copy, [y1.b, W1.b], [y1.b], scale=W1[:, i:i + 1])
                stt("dve", y1[:], y2[:], W2[:, i:i + 1], y1[:], ALU.mult, ALU.add, [y2.b, W2.b, y1.b], [y1.b])
                if dbg == 2:
                    dma("sp", dbg_d[i * 128:(i + 1) * 128, :], y1[:], [y1.b], [])
                tt("dve", y1[:], y1[:], g2bs[bb][:], ALU.mult, [y1.b, g2bs[bb].b], [y1.b])
                stt("dve", hh[:], hh[:], ALPHA, y1[:], ALU.mult, ALU.add, [hh.b, y1.b], [hh.b])
                mv = ln_stats(S, hh, stats, small, act)
                act(hh[:], hh[:], AF.Identity, [hh.b, mv.b], [hh.b], bias=mv[:, 3:4], scale=mv[:, 2:3])
                tt("dve", hh[:], hh[:], lnp2[:, 0:D], ALU.mult, [hh.b, lnp2.b], [hh.b])
                tt("dve", hh[:], hh[:], lnp2[:, D:2 * D], ALU.add, [hh.b, lnp2.b], [hh.b])
                dma("sp", out_d[bb, (i % 32) * 128:(i % 32 + 1) * 128, :], hh[:], [hh.b], [])

        if dbg != 1:
            moe_phase()
        S.finish("sp")
        S.emit()
    return nc


def ln_stats(S, z, stats, small, act):
    st = stats.get()
    for hlf in range(2):
        S.op("dve", (lambda o, i_: (lambda e: e.bn_stats(o, i_)))(st[:, hlf * 6:(hlf + 1) * 6], z[:, hlf * 512:(hlf + 1) * 512]),
             [z.b], [st.b])
    mv = small.get()
    S.op("dve", (lambda o, i_: (lambda e: e.bn_aggr(o, i_)))(mv[:, 0:2], st[:, 0:12]), [st.b], [mv.b])
    act(mv[:, 2:3], mv[:, 1:2], AF.Sqrt, [mv.b], [mv.b], bias=EPS, scale=1.0)
    S.op("dve", (lambda o, i_: (lambda e: e.reciprocal(o, i_)))(mv[:, 2:3], mv[:, 2:3]), [mv.b], [mv.b])
    S.op("dve", (lambda o, a_, b_: (lambda e: e.scalar_tensor_tensor(o, a_, -1.0, b_, ALU.mult, ALU.mult)))(
        mv[:, 3:4], mv[:, 0:1], mv[:, 2:3]), [mv.b], [mv.b])
    return mv


def _host_consts():
    identF = np.eye(128, dtype=np.float32)
    s = np.arange(128)[:, None]
    t = np.arange(128)[None, :]
    same = (s // 64) == (t // 64)
    maskF = (same & (s <= t)).astype(np.float32)
    maskB = (same & (s >= t)).astype(np.float32)
    resetm = np.ones((128, 512), np.float32)
    resetm[:, ::64] = 0.0
    triu = (s < t).astype(np.float32)
    thr = np.zeros((128, 64 + NBLK), np.float32)
    thr[:, :64] = (np.arange(64) * BLK)[None, :]
    thr[:, 64:] = (np.arange(NBLK) * BLK)[None, :]
    iota = np.zeros((128, 2), np.float32)
    iota[:, 0] = np.arange(128)
    return dict(identF=identF, maskF=maskF, maskB=maskB, resetm=resetm, triu=triu, thr=thr, iota=iota)


def _prep_shared(inp):
    f = lambda a: np.ascontiguousarray(np.asarray(a, dtype=np.float32))
    sh = {}
    sh["ada_w"] = f(inp["ada_w"][0])
    sh["ada_b"] = f(inp["ada_b"][0]).reshape(1, -1)
    sh["w_in"] = f(inp["w_in"][0])
    cw = f(inp["rg_conv_w"][0])
    sh["convw"] = f(cw.reshape(4, 4, 128).transpose(2, 1, 0).reshape(128, 16))
    sh["convb"] = f(f(inp["rg_conv_b"][0]).reshape(4, 128).T)
    gwt = f(inp["rg_gate_w"][0])
    gw = np.zeros((128, 16, 128), np.float32)
    for d_ in range(2):
        for g_ in range(2):
            for c in range(4):
                for h_ in range(2):
                    gw[h_ * 64:(h_ + 1) * 64, d_ * 8 + g_ * 4 + c, h_ * 64:(h_ + 1) * 64] = gwt[d_, g_, c * 2 + h_]
    sh["gw"] = gw.reshape(128, 2048)
    gbt = f(inp["rg_gate_b"][0])
    sh["gb"] = f(gbt.reshape(2, 2, 4, 128).transpose(3, 0, 1, 2).reshape(128, 16))
    sh["lam"] = f(f(inp["rg_lambda"][0]).reshape(2, 4, 128).transpose(2, 0, 1).reshape(128, 8))
    sh["lbl"] = f(f(inp["hg_lb_logits"]).reshape(2, 2, 4, 128).transpose(3, 0, 1, 2).reshape(128, 16))
    sh["ng"] = f(f(inp["hg_norm_g"][0]).reshape(4, 128).T)
    sh["w_out"] = f(inp["w_out"][0])
    lnp = np.concatenate([f(inp["ln1_g"][0]), f(inp["ln1_b"][0]), f(inp["ln2_g"][0]), f(inp["ln2_b"][0])])
    sh["lnp"] = f(np.broadcast_to(lnp[None, :], (128, 4 * D)))
    sh["rw"] = f(np.concatenate([f(inp["router_g_w"][0]), f(inp["router_e_w"][0])], axis=1))
    rb = np.concatenate([f(inp["router_g_b"][0]), f(inp["router_e_b"][0])])
    sh["rb"] = f(np.broadcast_to(rb[None, :], (128, 36)))
    sh["w1r"] = f(f(inp["exp_w1"][0]).reshape(32, 8, 128, 512).transpose(0, 2, 1, 3).reshape(32 * 128, 8 * 512))
    sh["w3r"] = f(f(inp["exp_w3"][0]).reshape(32, 8, 128, 512).transpose(0, 2, 1, 3).reshape(32 * 128, 8 * 512))
    sh["w2r"] = f(f(inp["exp_w2"][0]).reshape(32, 4, 128, 1024).transpose(0, 2, 1, 3).reshape(32 * 128, 4 * 1024))
    sh.update(_host_consts())
    return sh


def _in_maps(inp):
    sh = _prep_shared(inp)
    x = np.asarray(inp["x"], np.float32)
    c = np.asarray(inp["c"], np.float32)
    ctx = np.asarray(inp["ctx"], np.float32)
    c_ctx = np.asarray(inp["c_ctx"], np.float32)
    maps = []
    for k in range(NCORES):
        m = dict(sh)
        m["x"] = np.ascontiguousarray(x[2 * k:2 * k + 2])
        m["ctx"] = np.ascontiguousarray(ctx[2 * k:2 * k + 2])
        cv = np.stack([c[2 * k], c[2 * k + 1], c_ctx], axis=0)
        m["cT"] = np.ascontiguousarray(cv.reshape(3, 8, 128).transpose(2, 1, 0).reshape(128, 24))
        maps.append(m)
    return maps


_NC_CACHE = {}


def kernel(**inputs):
    if "nc" not in _NC_CACHE:
        _NC_CACHE["nc"] = build_program(0)
    nc = _NC_CACHE["nc"]
    maps = _in_maps(inputs)
    res = run_bass_kernel_spmd(nc, maps, core_ids=list(range(NCORES)))
    out = np.concatenate([r["out"] for r in res.results], axis=0)
    return out.astype(np.float32)
```
